# Optimizing a Trainium2 kernel written in Bass

```python
import jax, jax.numpy as jnp
from jax import lax
import numpy as np

D_MODEL = 2048
BATCH = 4
SEQ = 4096
DEPTH = 2

GRID_W = 64
CTX_LEN = 256
D_MIX = D_MODEL
REC_WIDTH = D_MIX // 2
REC_BLOCKS = 8
REC_BLOCK_W = REC_WIDTH // REC_BLOCKS
CONV_W = 4
LRU_C = 8.0
N_HEADS = 8
QK_NOPE_DIM = 128
QK_ROPE_DIM = 64
QK_HEAD_DIM = QK_NOPE_DIM + QK_ROPE_DIM
V_HEAD_DIM = 128
ATT_WIDTH = N_HEADS * V_HEAD_DIM
Q_RANK = 512
KV_RANK = 256
ROPE_AXIS_DIM = QK_ROPE_DIM // 2
ROPE_BASE = 10000.0
ATT_SCALE = QK_HEAD_DIM ** -0.5
Q_BLOCK = 128
N_EXPERTS = 16
EC_CAPACITY = 2
D_EXPERT = 2048
N_MOD = 6
IN_WIDTH = 2 * REC_WIDTH + Q_RANK + KV_RANK + QK_ROPE_DIM
IN_SPLITS = (REC_WIDTH, 2 * REC_WIDTH, 2 * REC_WIDTH + Q_RANK, 2 * REC_WIDTH + Q_RANK + KV_RANK)
EPS = 1e-6

kernel_name = "hybrid_rglru_mla_ecmoe_diffusion_block"


def rms_norm(x, g):
    xf = x.astype(jnp.float32)
    y = xf * lax.rsqrt(jnp.mean(xf * xf, axis=-1, keepdims=True) + EPS)
    return y.astype(x.dtype) * g


def modulate(h, shift, scale):
    return h * (1 + scale) + shift


def axial_rope_tables(rows):
    row = jnp.repeat(jnp.arange(rows, dtype=jnp.float32), GRID_W)
    col = jnp.tile(jnp.arange(GRID_W, dtype=jnp.float32), rows)
    half = ROPE_AXIS_DIM // 2
    inv = ROPE_BASE ** (-jnp.arange(half, dtype=jnp.float32) / half)
    ang_r = row[:, None] * inv
    ang_c = col[:, None] * inv
    return (jnp.cos(ang_r), jnp.sin(ang_r), jnp.cos(ang_c), jnp.sin(ang_c))


def rotate(x, cos, sin):
    cos = cos[:, None, :].astype(x.dtype)
    sin = sin[:, None, :].astype(x.dtype)
    x1, x2 = jnp.split(x, 2, axis=-1)
    return jnp.concatenate([x1 * cos - x2 * sin, x2 * cos + x1 * sin], axis=-1)


def rope_head(x, tabs):
    cos_r, sin_r, cos_c, sin_c = tabs
    nope, rp = jnp.split(x, [QK_NOPE_DIM], axis=-1)
    pr, pc = jnp.split(rp, 2, axis=-1)
    return jnp.concatenate([nope, rotate(pr, cos_r, sin_r), rotate(pc, cos_c, sin_c)], axis=-1)


def centred_dwconv(x, w, b):
    T = x.shape[1]
    left = CONV_W // 2
    xp = jnp.pad(x, ((0, 0), (left, CONV_W - 1 - left), (0, 0)))
    out = b
    for k in range(CONV_W):
        out = out + xp[:, k:k + T] * w[k]
    return out


def _lin_combine(e1, e2):
    a1, b1 = e1
    a2, b2 = e2
    return a1 * a2, a2 * b1 + b2


def block_diag(u, w):
    B, T, W = u.shape
    ub = u.reshape(B, T, REC_BLOCKS, REC_BLOCK_W)
    return jnp.einsum('btgi,gij->btgj', ub, w).reshape(B, T, W)


def rg_lru(u, wa, ba, wx, bx, lam, h0, reverse):
    T = u.shape[1]
    r = jax.nn.sigmoid((block_diag(u, wa) + ba).astype(jnp.float32))
    i = jax.nn.sigmoid((block_diag(u, wx) + bx).astype(jnp.float32))
    log_a = LRU_C * r * jax.nn.log_sigmoid(lam.astype(jnp.float32))
    a = jnp.exp(log_a)
    b = jnp.sqrt(-jnp.expm1(2.0 * log_a)) * (i * u.astype(jnp.float32))
    edge = T - 1 if reverse else 0
    b = b.at[:, edge].add(a[:, edge] * h0)
    _, h = lax.associative_scan(_lin_combine, (a, b), axis=1, reverse=reverse)
    return h


def mla_q(cq, p, tabs):
    B, T, _ = cq.shape
    q = (rms_norm(cq, p['q_a_norm']) @ p['w_uq']).reshape(B, T, N_HEADS, QK_HEAD_DIM)
    q = rms_norm(q, p['q_norm'])
    return q if tabs is None else rope_head(q, tabs)


def mla_kv(ckv, kr, p, tabs):
    B, T, _ = ckv.shape
    kv = (rms_norm(ckv, p['kv_a_norm']) @ p['w_ukv']).reshape(B, T, N_HEADS, QK_NOPE_DIM + V_HEAD_DIM)
    k_nope, v = jnp.split(kv, [QK_NOPE_DIM], axis=-1)
    k_rope = jnp.broadcast_to(kr[:, :, None, :], (B, T, N_HEADS, QK_ROPE_DIM))
    k = rms_norm(jnp.concatenate([k_nope, k_rope], axis=-1), p['k_norm'])
    k = k if tabs is None else rope_head(k, tabs)
    return k, v


def block_attention(q, k, v):
    B, T, H, Dq = q.shape
    nb = T // Q_BLOCK
    qb = q.reshape(B, nb, Q_BLOCK, H, Dq).transpose(1, 0, 2, 3, 4)

    def one(qi):
        s = jnp.einsum('bqhd,bkhd->bhqk', qi, k).astype(jnp.float32) * ATT_SCALE
        pr = jax.nn.softmax(s, axis=-1).astype(v.dtype)
        return jnp.einsum('bhqk,bkhd->bqhd', pr, v)

    o = lax.map(one, qb)
    return o.transpose(1, 0, 2, 3, 4).reshape(B, T, H * v.shape[-1])


def ec_moe(h, w_router, w_gate, w_up, w_down):
    B, T, D = h.shape
    cap = EC_CAPACITY * T // N_EXPERTS
    aff = jax.nn.softmax((h @ w_router).astype(jnp.float32), axis=-1)
    gate, idx = lax.top_k(aff.transpose(0, 2, 1), cap)
    b_ix = jnp.arange(B)[:, None, None]
    xg = h[b_ix, idx]
    a = jnp.einsum('becd,edf->becf', xg, w_gate)
    u = jnp.einsum('becd,edf->becf', xg, w_up)
    y = jnp.einsum('becf,efd->becd', jax.nn.silu(a) * u, w_down) * gate[..., None].astype(h.dtype)
    return jnp.zeros_like(h).at[b_ix, idx].add(y)


def mixer_out(y_rec_f32, gr, y_att, w_out, dtype):
    y_rec = y_rec_f32.astype(dtype) * jax.nn.gelu(gr)
    return jnp.concatenate([y_rec, y_att], axis=-1) @ w_out


def layer(x, cx, c, c_ctx, p, tabs, last):
    B = x.shape[0]
    mod = (jax.nn.silu(c) @ p['w_mod'] + p['b_mod'])[:, None, :]
    mod_c = jax.nn.silu(c_ctx) @ p['w_mod'] + p['b_mod']
    sh1, sc1, g1, sh2, sc2, g2 = jnp.split(mod, N_MOD, axis=-1)
    sh1c, sc1c, g1c, sh2c, sc2c, g2c = jnp.split(mod_c, N_MOD, axis=-1)

    z = modulate(rms_norm(x, p['norm1']), sh1, sc1) @ p['w_in']
    zc = modulate(rms_norm(cx, p['norm1']), sh1c, sc1c) @ p['w_in']
    xr, gr, cq, ckv, kr = jnp.split(z, IN_SPLITS, axis=-1)
    xrc, grc, cqc, ckvc, krc = jnp.split(zc, IN_SPLITS, axis=-1)

    uc = centred_dwconv(xrc, p['conv_w'], p['conv_b'])
    ul = centred_dwconv(xr, p['conv_w'], p['conv_b'])
    zero = jnp.zeros((B, REC_WIDTH), jnp.float32)
    fwd = (p['lru_wa'][0], p['lru_ba'][0], p['lru_wx'][0], p['lru_bx'][0], p['lru_lambda'][0])
    bwd = (p['lru_wa'][1], p['lru_ba'][1], p['lru_wx'][1], p['lru_bx'][1], p['lru_lambda'][1])
    hcf = rg_lru(uc, *fwd, zero, False)
    hcb = rg_lru(uc, *bwd, zero, True)
    hlf = rg_lru(ul, *fwd, hcf[:, -1], False)
    hlb = rg_lru(ul, *bwd, hcb[:, 0], True)

    kc, vc = mla_kv(ckvc, krc, p, None)
    kl, vl = mla_kv(ckv, kr, p, tabs)
    ql = mla_q(cq, p, tabs)
    k_all = jnp.concatenate([kc, kl], axis=1)
    v_all = jnp.concatenate([vc, vl], axis=1)
    y_att = block_attention(ql, k_all, v_all)

    x_new = x + g1 * mixer_out(hlf + hlb, gr, y_att, p['w_out'], x.dtype)
    h2 = modulate(rms_norm(x_new, p['norm2']), sh2, sc2)
    x_new = x_new + g2 * ec_moe(h2, p['router'], p['w_gate'], p['w_up'], p['w_down'])

    if last:
        return x_new, cx
    qc = mla_q(cqc, p, None)
    y_att_c = block_attention(qc, kc, vc)
    cx_new = cx + g1c * mixer_out(hcf + hcb, grc, y_att_c, p['w_out'], cx.dtype)
    h2c = modulate(rms_norm(cx_new, p['norm2']), sh2c, sc2c)
    cx_new = cx_new + g2c * ec_moe(h2c, p['router'], p['w_gate'], p['w_up'], p['w_down'])
    return x_new, cx_new


def setup_inputs(seed: int = 0) -> dict:
    key = jax.random.key(seed)
    ks = jax.random.split(key, 32)
    f32 = jnp.float32
    nrm = lambda k, shape, s: jax.random.normal(k, shape, f32) * s
    gain = lambda k, shape: 1.0 + 0.05 * jax.random.normal(k, shape, f32)
    D = D_MODEL
    a0 = jax.random.uniform(ks[14], (DEPTH, 2, REC_WIDTH), f32, 0.9, 0.999)
    return {
        'x': nrm(ks[0], (BATCH, SEQ, D), 1.0),
        'c': nrm(ks[1], (BATCH, D), 1.0),
        'ctx': nrm(ks[2], (BATCH, CTX_LEN, D), 1.0),
        'c_ctx': nrm(ks[3], (D,), 1.0),
        'w_mod': nrm(ks[4], (DEPTH, D, N_MOD * D), 0.5 * D ** -0.5),
        'b_mod': nrm(ks[5], (DEPTH, N_MOD * D), 0.02),
        'norm1': gain(ks[6], (DEPTH, D)),
        'w_in': nrm(ks[7], (DEPTH, D, IN_WIDTH), D ** -0.5),
        'conv_w': nrm(ks[8], (DEPTH, CONV_W, REC_WIDTH), CONV_W ** -0.5),
        'conv_b': nrm(ks[9], (DEPTH, REC_WIDTH), 0.02),
        'lru_wa': nrm(ks[10], (DEPTH, 2, REC_BLOCKS, REC_BLOCK_W, REC_BLOCK_W), REC_BLOCK_W ** -0.5),
        'lru_ba': nrm(ks[11], (DEPTH, 2, REC_WIDTH), 0.1),
        'lru_wx': nrm(ks[12], (DEPTH, 2, REC_BLOCKS, REC_BLOCK_W, REC_BLOCK_W), REC_BLOCK_W ** -0.5),
        'lru_bx': nrm(ks[13], (DEPTH, 2, REC_WIDTH), 0.1),
        'lru_lambda': jnp.log(a0) - jnp.log1p(-a0),
        'q_a_norm': gain(ks[15], (DEPTH, Q_RANK)),
        'w_uq': nrm(ks[16], (DEPTH, Q_RANK, N_HEADS * QK_HEAD_DIM), Q_RANK ** -0.5),
        'kv_a_norm': gain(ks[17], (DEPTH, KV_RANK)),
        'w_ukv': nrm(ks[18], (DEPTH, KV_RANK, N_HEADS * (QK_NOPE_DIM + V_HEAD_DIM)), KV_RANK ** -0.5),
        'q_norm': gain(ks[19], (DEPTH, QK_HEAD_DIM)),
        'k_norm': gain(ks[20], (DEPTH, QK_HEAD_DIM)),
        'w_out': nrm(ks[21], (DEPTH, D_MIX, D), D_MIX ** -0.5),
        'norm2': gain(ks[22], (DEPTH, D)),
        'router': nrm(ks[23], (DEPTH, D, N_EXPERTS), D ** -0.5),
        'w_gate': nrm(ks[24], (DEPTH, N_EXPERTS, D, D_EXPERT), D ** -0.5),
        'w_up': nrm(ks[25], (DEPTH, N_EXPERTS, D, D_EXPERT), D ** -0.5),
        'w_down': nrm(ks[26], (DEPTH, N_EXPERTS, D_EXPERT, D), D_EXPERT ** -0.5),
    }


def reference(x, c, ctx, c_ctx, w_mod, b_mod, norm1, w_in, conv_w, conv_b, lru_wa, lru_ba,
              lru_wx, lru_bx, lru_lambda, q_a_norm, w_uq, kv_a_norm, w_ukv, q_norm, k_norm,
              w_out, norm2, router, w_gate, w_up, w_down):
    rows = x.shape[1] // GRID_W
    tabs = axial_rope_tables(rows)
    cx = ctx
    for l in range(DEPTH):
        p = {
            'w_mod': w_mod[l], 'b_mod': b_mod[l], 'norm1': norm1[l], 'w_in': w_in[l],
            'conv_w': conv_w[l], 'conv_b': conv_b[l], 'lru_wa': lru_wa[l], 'lru_ba': lru_ba[l],
            'lru_wx': lru_wx[l], 'lru_bx': lru_bx[l], 'lru_lambda': lru_lambda[l],
            'q_a_norm': q_a_norm[l], 'w_uq': w_uq[l], 'kv_a_norm': kv_a_norm[l], 'w_ukv': w_ukv[l],
            'q_norm': q_norm[l], 'k_norm': k_norm[l], 'w_out': w_out[l], 'norm2': norm2[l],
            'router': router[l], 'w_gate': w_gate[l], 'w_up': w_up[l], 'w_down': w_down[l],
        }
        x, cx = layer(x, cx, c, c_ctx, p, tabs, l == DEPTH - 1)
    return x
```

```python
import numpy as np
import ml_dtypes
import concourse.bass as bass
import concourse.mybir as mybir
from concourse.bass_utils import run_bass_kernel_spmd

F32 = mybir.dt.float32
BF16 = mybir.dt.bfloat16
ALU = mybir.AluOpType
AF = mybir.ActivationFunctionType
AX = mybir.AxisListType
NPBF = ml_dtypes.bfloat16

D = 2048
KC = 16
NH = 8
NE = 16
EPS = 1e-6
ATT_SCALE = 192 ** -0.5
NCORES = 8


class Prog:
    ENG = ("pe", "dve", "act", "pool", "sp")

    def __init__(self):
        self.nc = bass.Bass("TRN2", target_bir_lowering=False)
        nc = self.nc
        self.eng = {"pe": nc.tensor, "dve": nc.vector, "act": nc.scalar,
                    "pool": nc.gpsimd, "sp": nc.sync}
        self._ctx = []
        self.csem, self.dsem, self.cnt = {}, {}, {}
        self.NDS = 20
        self.drr = {}
        for e in self.ENG:
            self.csem[e] = self._enter(nc.semaphore("c_" + e))
            self.cnt[("c", e)] = 0
        for e in ("sp", "pool"):
            self.drr[e] = 0
            for i in range(self.NDS):
                self.dsem[(e, i)] = self._enter(nc.semaphore("d_%s%d" % (e, i)))
                self.cnt[("d", e, i)] = 0
        self.waited = {e: {} for e in self.ENG}
        self.lastw, self.readers, self.tags = {}, {}, {}
        self.skip = set()
        self.n_ins = 0
        self.uid = 0

    def _enter(self, cm):
        v = cm.__enter__()
        self._ctx.append(cm)
        return v

    def sb(self, name, shape, dt=F32):
        self.uid += 1
        return self._enter(self.nc.sbuf_tensor("%s_u%d" % (name, self.uid), list(shape), dt))

    def ps(self, name, shape, dt=F32):
        return self._enter(self.nc.psum_tensor(name, list(shape), dt))

    def din(self, name, shape, dt=F32):
        self.skip.add(name)
        return self.nc.dram_tensor(name, list(shape), dt, kind="ExternalInput").ap()

    def dout(self, name, shape, dt=F32):
        self.skip.add(name)
        return self.nc.dram_tensor(name, list(shape), dt, kind="ExternalOutput").ap()

    def dscr(self, name, shape, dt=F32, track=False):
        if not track:
            self.skip.add(name)
        return self.nc.dram_tensor(name, list(shape), dt, kind="Internal").ap()

    @staticmethod
    def _nm(t):
        if isinstance(t, str):
            return t
        if hasattr(t, "tensor"):
            t = t.tensor
        return t.name

    def _norm(self, ks):
        out = []
        for k in ks:
            if k is None or isinstance(k, (int, float)):
                continue
            if isinstance(k, tuple):
                n = self._nm(k[0])
                if n not in self.skip:
                    out.append((n, k[1]))
            else:
                n = self._nm(k)
                if n not in self.skip:
                    out.append((n, None))
        return out

    def _conf(self, key):
        name, tag = key
        if tag is None:
            return [(name, t) for t in self.tags.get(name, ())] + [(name, None)]
        return [(name, tag), (name, None)]

    def _sem(self, sk):
        return self.csem[sk[1]] if sk[0] == "c" else self.dsem[(sk[1], sk[2])]

    def emit(self, e, build, reads=(), writes=(), dma=False):
        reads = self._norm(reads)
        writes = self._norm(writes)
        deps = {}

        def need(d):
            if d is not None and deps.get(d[0], 0) < d[1]:
                deps[d[0]] = d[1]

        for k in reads:
            for c in self._conf(k):
                need(self.lastw.get(c))
        for k in writes:
            for c in self._conf(k):
                need(self.lastw.get(c))
                for r in self.readers.get(c, ()):
                    need(r)
        engine = self.eng[e]
        for sk, v in deps.items():
            if sk == ("c", "pe") and e == "pe" and not dma:
                continue
            if self.waited[e].get(sk, 0) >= v:
                continue
            engine.wait_ge(self._sem(sk), v)
            self.waited[e][sk] = v
        if dma:
            sk = ("d", e, self.drr[e] % self.NDS)
            self.drr[e] += 1
            if self.cnt[sk] > self.waited[e].get(sk, 0):
                engine.wait_ge(self._sem(sk), self.cnt[sk])
                self.waited[e][sk] = self.cnt[sk]
        else:
            sk = ("c", e)
        ins = build(engine)
        self.cnt[sk] += 16 if dma else 1
        ins.then_inc(self._sem(sk), 16 if dma else 1)
        me = (sk, self.cnt[sk])
        for k in writes:
            if k[1] is None:
                for c in self._conf(k):
                    self.lastw.pop(c, None)
                    self.readers.pop(c, None)
            else:
                self.tags.setdefault(k[0], set()).add(k[1])
            self.lastw[k] = me
            self.readers[k] = []
        for k in reads:
            if k[1] is not None:
                self.tags.setdefault(k[0], set()).add(k[1])
            lst = self.readers.setdefault(k, [])
            lst.append(me)
            if len(lst) > 10:
                best = {}
                for s, v in lst:
                    if best.get(s, 0) < v:
                        best[s] = v
                self.readers[k] = list(best.items())
        self.n_ins += 1
        return me

    def dma(self, out, in_, q="sp", rd=None, wr=None, slow=False):
        kw = {"allow_slow_non_contiguous": True} if slow else {}
        return self.emit(q, lambda en: en.dma_start(out=out, in_=in_, **kw),
                         rd if rd is not None else [in_], wr if wr is not None else [out], dma=True)

    def mm(self, out, lhsT, rhs, start, stop):
        return self.emit("pe", lambda en: en.matmul(out, lhsT, rhs, start=start, stop=stop),
                         [lhsT, rhs], [out])

    def act(self, out, in_, func, bias=None, scale=None, e="act"):
        kw = {}
        if bias is not None:
            kw["bias"] = bias
        if scale is not None:
            kw["scale"] = scale
        return self.emit(e, lambda en: en.activation(out, in_, func, **kw), [in_, bias, scale], [out])

    def tt(self, out, a, b, op, e="dve"):
        return self.emit(e, lambda en: en.tensor_tensor(out, a, b, op), [a, b], [out])

    def ts(self, out, a, s1, s2, op0, op1=None, e="dve"):
        if op1 is None:
            return self.emit(e, lambda en: en.tensor_scalar(out, a, s1, None, op0), [a, s1], [out])
        return self.emit(e, lambda en: en.tensor_scalar(out, a, s1, s2, op0, op1), [a, s1, s2], [out])

    def stt(self, out, in0, scalar, in1, op0, op1):
        return self.emit("dve", lambda en: en.scalar_tensor_tensor(out, in0, scalar, in1, op0, op1),
                         [in0, scalar, in1], [out])

    def copy(self, out, in_, e="dve"):
        return self.emit(e, lambda en: en.tensor_copy(out, in_), [in_], [out])

    def memset(self, out, val, e="dve"):
        return self.emit(e, lambda en: en.memset(out, val), [], [out])

    def scan(self, out, d0, d1, init, op0, op1):
        return self.emit("dve", lambda en: en.tensor_tensor_scan(out, d0, d1, init, op0, op1),
                         [d0, d1, init], [out])

    def recip(self, out, in_):
        return self.emit("dve", lambda en: en.reciprocal(out, in_), [in_], [out])

    def rstd(self, out, ss, inv_n, epsb, tmp=None):
        self.act(out, ss, AF.Sqrt, bias=epsb, scale=inv_n)
        self.recip(out, out)

    def barrier(self):
        for e in self.ENG:
            engine = self.eng[e]
            for sk, v in self.cnt.items():
                if v > self.waited[e].get(sk, 0):
                    engine.wait_ge(self._sem(sk), v)
                    self.waited[e][sk] = v
        self.lastw.clear()
        self.readers.clear()
        self.tags.clear()

    def phase_begin(self):
        return len(self._ctx)

    def phase_end(self, mark):
        self.barrier()
        while len(self._ctx) > mark:
            self._ctx.pop().__exit__(None, None, None)

    def finish(self):
        for sk, v in self.cnt.items():
            if v > 0:
                self.eng["sp"].wait_ge(self._sem(sk), v)
        while self._ctx:
            self._ctx.pop().__exit__(None, None, None)
        return self.nc


def fm(v):
    v = np.asarray(v, np.float32)
    return np.ascontiguousarray(v.reshape(-1, 128).T)


def chunks(n, step=512):
    return [(i, min(step, n - i)) for i in range(0, n, step)]


_SW = np.array([f + 16 if (f % 32) < 16 else f - 16 for f in range(64)])


def rope_tables(t_idx):
    t_idx = np.asarray(t_idx)
    row = (t_idx // 64).astype(np.float32)
    col = (t_idx % 64).astype(np.float32)
    inv = (np.float32(10000.0) ** (-np.arange(16, dtype=np.float32) / np.float32(16))).astype(np.float32)
    cos = np.zeros((64, len(t_idx)), np.float32)
    sin = np.zeros((64, len(t_idx)), np.float32)
    for f in range(64):
        pos = row if f < 32 else col
        ang = (pos * inv[f % 16]).astype(np.float32)
        cos[f] = np.cos(ang)
        s = np.sin(ang)
        sin[f] = -s if (f % 32) < 16 else s
    return cos, sin


def pad128(v):
    o = np.zeros((128, 1), np.float32)
    o[:len(v), 0] = v
    return o


def a_weights(inp, l):
    w_in = inp["w_in"][l]
    w_in_ext = np.ascontiguousarray(np.concatenate([w_in, w_in[:, 2816 + _SW]], axis=1))
    wq = inp["w_uq"][l].reshape(512, NH, 192)
    w_uq_ext = np.ascontiguousarray(np.concatenate([wq, wq[:, :, 128 + _SW]], axis=2).reshape(512, NH * 256))
    wkv = inp["w_ukv"][l].reshape(256, NH, 256)
    w_uk = np.ascontiguousarray(wkv[:, :, :128].reshape(256, 1024))
    w_uv = np.ascontiguousarray(wkv[:, :, 128:].reshape(256, 1024))
    return w_in_ext, w_uq_ext, w_uk, w_uv


I32 = mybir.dt.int32
N_BISECT = 34
W_IN_EXT = 2944
CV_A = dict(g1=0, sh_l=16, sc_l=32, sh_c=48, sc_c=64, gqa=80, gkva=84,
            gq_n=86, gq_r=87, gq_s=88, gk_n=89, gk_r=90, gk_s=91, n=92)
CV_B = dict(g1_l=0, g1_c=16, n2=32, sh2_l=48, sc2_l=64, sh2_c=80, sc2_c=96, n=112)


def emit_M(P, pb, G):
    mk = P.phase_begin()
    s2 = P.sb("M_s2", [128, KC, 2])
    bm = P.sb("M_bm", [128, 2, 96])
    wt = [P.sb("M_wt%d" % i, [128, KC, 512]) for i in range(2)]
    P.dma(s2[:], G["sT"])
    P.dma(bm[:], G["bmod"])
    P.act(s2[:], s2[:], AF.Silu)
    i = 0
    for l in range(2):
        wv = G["w_mod"][l].rearrange("(kc p) n -> p kc n", p=128)
        for cg in range(24):
            t = wt[i % 2]
            P.dma(t[:], wv[:, :, cg * 512:(cg + 1) * 512], q="sp" if i % 2 == 0 else "pool")
            i += 1
            for ci in range(4):
                ch = cg * 4 + ci
                p_ = pb[ch % 2]
                for kc in range(KC):
                    P.mm(p_[:, 0:2], t[:, kc, ci * 128:(ci + 1) * 128], s2[:, kc, :], kc == 0, kc == KC - 1)
                P.ts(G["modfm"][:, l, ch, :], p_[:, 0:2], bm[:, l, ch:ch + 1], None, ALU.add)
    P.phase_end(mk)


def emit_A(P, pb, G, l, xT, NLC, NCX):
    NT = NLC + NCX
    mk = P.phase_begin()
    modfm = G["modfm"]
    cvt = P.sb("A_cvt", [128, CV_A["n"]])
    P.dma(cvt[:], G["cvA"][l])
    P.copy(cvt[:, 16:32], modfm[:, l, 0:16, 0])
    P.copy(cvt[:, 32:48], modfm[:, l, 16:32, 0])
    P.copy(cvt[:, 48:64], modfm[:, l, 0:16, 1])
    P.copy(cvt[:, 64:80], modfm[:, l, 16:32, 1])
    C = lambda name, j=0, w=1: cvt[:, CV_A[name] + j: CV_A[name] + j + w]
    epsb = P.sb("A_epsb", [128, 1])
    P.memset(epsb[:], EPS)
    ones = P.sb("A_ones", [128, 128])
    P.memset(ones[:], 1.0)
    Am = P.sb("A_Am", [128, 2, KC])
    for k, nm in enumerate(("sc_l", "sc_c")):
        P.ts(Am[:, k, :], C(nm, 0, KC), 1.0, None, ALU.add)
        P.tt(Am[:, k, :], Am[:, k, :], C("g1", 0, KC), ALU.mult)
    rc_t = P.sb("A_rc", [64, 512])
    rs_t = P.sb("A_rs", [64, 512])
    tch = [(c0, n, 0) for c0, n in chunks(NLC)] + [(NLC + c0, n, 1) for c0, n in chunks(NCX)]
    hm_d = G["hm_d"]
    xt = P.sb("A_xt", [128, KC, 512])
    sq = [P.sb("A_sq%d" % i, [128, 512]) for i in range(2)]
    rs = P.sb("A_rsd", [128, 512])
    hmc = [P.sb("A_hmc%d" % i, [128, KC, 512], BF16) for i in range(3)]
    hm = hmc[0]
    xv = xT.rearrange("(kc p) n -> p kc n", p=128)
    for (c0, n, kind) in tch:
        P.dma(xt[:, :, :n], xv[:, :, c0:c0 + n], q="pool")
        for kc in range(KC):
            s_ = sq[kc % 2]
            P.act(s_[:, :n], xt[:, kc, :n], AF.Square)
            P.mm(pb[0][:, :n], ones[:], s_[:, :n], kc == 0, kc == KC - 1)
        P.rstd(rs[:, :n], pb[0][:, :n], 1.0 / D, epsb[:])
        shn = "sh_l" if kind == 0 else "sh_c"
        for kc in range(KC):
            s_ = sq[kc % 2]
            P.tt(s_[:, :n], xt[:, kc, :n], rs[:, :n], ALU.mult)
            P.ts(hm[:, kc, :n], s_[:, :n], Am[:, kind, kc:kc + 1], C(shn, kc), ALU.mult, ALU.add)
        P.dma(hm_d[:, :, c0:c0 + n], hm[:, :, :n], wr=[(hm_d, c0)])

    wg = [P.sb("A_wg%d" % i, [128, KC, 512], BF16) for i in range(2)]
    wuq = P.sb("A_wuq", [128, 4, 2048], BF16)
    wuk = P.sb("A_wuk", [128, 2, 1024], BF16)
    wuv = P.sb("A_wuv", [128, 2, 1024], BF16)
    P.dma(wuq[:], G["w_uq"][l].rearrange("(kc p) n -> p kc n", p=128), q="pool")
    P.dma(wuk[:], G["w_uk"][l].rearrange("(kc p) n -> p kc n", p=128), q="pool")
    P.dma(wuv[:], G["w_uv"][l].rearrange("(kc p) n -> p kc n", p=128), q="pool")
    wv = G["w_in"][l].rearrange("(kc p) n -> p kc n", p=128)
    ev = [P.sb("A_ev%d" % i, [128, 512]) for i in range(2)]
    evb = [P.sb("A_evb%d" % i, [128, 512], BF16) for i in range(2)]
    cqt = P.sb("A_cqt", [128, 4, 512])
    cqn = P.sb("A_cqn", [128, 4, 512], BF16)
    ckt = P.sb("A_ckt", [128, 2, 512])
    ckn = P.sb("A_ckn", [128, 2, 512], BF16)
    krt = P.sb("A_krt", [64, 512])
    kst = P.sb("A_kst", [64, 512])
    krsq = P.sb("A_krsq", [64, 512])
    Rt = P.sb("A_Rt", [64, 512])
    sqn2 = [P.sb("A_sqn%d" % i, [128, 512]) for i in range(2)]
    sqr2 = [P.sb("A_sqr%d" % i, [64, 512]) for i in range(2)]
    rsh2 = [P.sb("A_rsh%d" % i, [128, 512]) for i in range(2)]
    t64a = P.sb("A_t64a", [64, 512])
    t64b = P.sb("A_t64b", [64, 512])
    ob64 = [P.sb("A_ob64_%d" % i, [64, 512], BF16) for i in range(2)]
    vb = [P.sb("A_vb%d" % i, [128, 1024], BF16) for i in range(2)]
    xrT, ggT, qn_o, qr_o, kn_o, kr_o, v_o = G["xrT"], G["ggT"], G["qn"], G["qr"], G["kn"], G["kr"], G["v"]

    def rope_mix(out_bf, a_f, b_f, n, kind):
        if kind == 0:
            P.tt(a_f, a_f, rc_t[:, :n], ALU.mult)
            P.tt(b_f, b_f, rs_t[:, :n], ALU.mult)
            P.tt(out_bf, a_f, b_f, ALU.add)
        else:
            P.copy(out_bf, a_f)

    groups = chunks(W_IN_EXT)
    it = 0
    for gi, (g0, gn) in enumerate(groups):
        w_ = wg[gi % 2]
        P.dma(w_[:, :, :gn], wv[:, :, g0:g0 + gn], q="pool")
        for (c0, n, kind) in tch:
            h_ = hmc[it % 3]
            it += 1
            P.dma(h_[:, :, :n], hm_d[:, :, c0:c0 + n], rd=[(hm_d, c0)], q="pool")
            if kind == 0 and g0 >= 2048:
                P.dma(rc_t[:, :n], G["ropec"][:, c0:c0 + n], q="pool")
                P.dma(rs_t[:, :n], G["ropes"][:, c0:c0 + n], q="pool")
            nm = (gn + 127) // 128
            for mi in range(nm):
                col = g0 + mi * 128
                mw = min(128, gn - mi * 128)
                p_ = pb[1 + (mi % 4)]
                if col < 2816:
                    for kc in range(KC):
                        P.mm(p_[:mw, :n], w_[:, kc, mi * 128: mi * 128 + mw], h_[:, kc, :n], kc == 0, kc == KC - 1)
                m = col // 128
                if m < 8:
                    e_ = ev[m % 2]
                    P.act(e_[:, :n], p_[:, :n], AF.Copy)
                    P.dma(xrT[m * 128:(m + 1) * 128, c0:c0 + n], e_[:, :n])
                elif m < 16:
                    e_ = evb[m % 2]
                    P.act(e_[:, :n], p_[:, :n], AF.Gelu_apprx_tanh)
                    P.dma(ggT[(m - 8) * 128:(m - 7) * 128, c0:c0 + n], e_[:, :n])
                elif m < 20:
                    P.copy(cqt[:, m - 16, :n], p_[:, :n])
                elif m < 22:
                    P.copy(ckt[:, m - 20, :n], p_[:, :n])
                else:
                    for kc in range(KC):
                        P.mm(pb[5][:64, :n], w_[:, kc, mi * 128: mi * 128 + 64], h_[:, kc, :n], kc == 0, kc == KC - 1)
                    for kc in range(KC):
                        P.mm(pb[6][:64, :n], w_[:, kc, mi * 128 + 64: mi * 128 + 128], h_[:, kc, :n], kc == 0, kc == KC - 1)
                    P.copy(krt[:, :n], pb[5][:64, :n])
                    P.copy(kst[:, :n], pb[6][:64, :n])
            if g0 == 2048:
                for kc in range(4):
                    s_ = sq[kc % 2]
                    P.act(s_[:, :n], cqt[:, kc, :n], AF.Square)
                    P.mm(pb[0][:, :n], ones[:], s_[:, :n], kc == 0, kc == 3)
                P.rstd(rs[:, :n], pb[0][:, :n], 1.0 / 512, epsb[:])
                for kc in range(4):
                    P.stt(cqn[:, kc, :n], cqt[:, kc, :n], C("gqa", kc), rs[:, :n], ALU.mult, ALU.mult)
                def qproj(h):
                    pn, pr, pS = pb[1 + (h % 2) * 3], pb[2 + (h % 2) * 3], pb[3 + (h % 2) * 3]
                    b0 = h * 256
                    for kc in range(4):
                        P.mm(pn[:, :n], wuq[:, kc, b0:b0 + 128], cqn[:, kc, :n], kc == 0, kc == 3)
                    for kc in range(4):
                        P.mm(pr[:64, :n], wuq[:, kc, b0 + 128:b0 + 192], cqn[:, kc, :n], kc == 0, kc == 3)
                    for kc in range(4):
                        P.mm(pS[:64, :n], wuq[:, kc, b0 + 192:b0 + 256], cqn[:, kc, :n], kc == 0, kc == 3)

                qproj(0)
                for h in range(NH):
                    pn, pr, pS = pb[1 + (h % 2) * 3], pb[2 + (h % 2) * 3], pb[3 + (h % 2) * 3]
                    sqn_, sqr_, rsh_ = sqn2[h % 2], sqr2[h % 2], rsh2[h % 2]
                    P.act(sqn_[:, :n], pn[:, :n], AF.Square)
                    P.act(sqr_[:, :n], pr[:64, :n], AF.Square)
                    if h + 1 < NH:
                        qproj(h + 1)
                    P.mm(pb[7][:, :n], ones[:], sqn_[:, :n], True, False)
                    P.mm(pb[7][:, :n], ones[:64, :], sqr_[:, :n], False, True)
                    P.rstd(rsh_[:, :n], pb[7][:, :n], 1.0 / 192, epsb[:])
                    o_ = evb[h % 2]
                    P.stt(o_[:, :n], pn[:, :n], C("gq_n"), rsh_[:, :n], ALU.mult, ALU.mult)
                    P.dma(qn_o[h, :, c0:c0 + n], o_[:, :n])
                    P.stt(t64a[:, :n], pr[:64, :n], cvt[:64, CV_A["gq_r"]:CV_A["gq_r"] + 1], rsh_[:64, :n], ALU.mult, ALU.mult)
                    P.stt(t64b[:, :n], pS[:64, :n], cvt[:64, CV_A["gq_s"]:CV_A["gq_s"] + 1], rsh_[:64, :n], ALU.mult, ALU.mult)
                    o6 = ob64[h % 2]
                    rope_mix(o6[:, :n], t64a[:, :n], t64b[:, :n], n, kind)
                    P.dma(qr_o[h, :, c0:c0 + n], o6[:, :n])
            if g0 == 2560:
                for kc in range(2):
                    s_ = sq[kc % 2]
                    P.act(s_[:, :n], ckt[:, kc, :n], AF.Square)
                    P.mm(pb[0][:, :n], ones[:], s_[:, :n], kc == 0, kc == 1)
                P.rstd(rs[:, :n], pb[0][:, :n], 1.0 / 256, epsb[:])
                for kc in range(2):
                    P.stt(ckn[:, kc, :n], ckt[:, kc, :n], C("gkva", kc), rs[:, :n], ALU.mult, ALU.mult)
                P.act(krsq[:, :n], krt[:, :n], AF.Square)
                P.ts(t64a[:, :n], krt[:, :n], cvt[:64, CV_A["gk_r"]:CV_A["gk_r"] + 1], None, ALU.mult)
                P.ts(t64b[:, :n], kst[:, :n], cvt[:64, CV_A["gk_s"]:CV_A["gk_s"] + 1], None, ALU.mult)
                if kind == 0:
                    P.tt(t64a[:, :n], t64a[:, :n], rc_t[:, :n], ALU.mult)
                    P.tt(t64b[:, :n], t64b[:, :n], rs_t[:, :n], ALU.mult)
                    P.tt(Rt[:, :n], t64a[:, :n], t64b[:, :n], ALU.add)
                else:
                    P.copy(Rt[:, :n], t64a[:, :n])
                def kproj(h):
                    pn = pb[1 + (h % 2)]
                    for kc in range(2):
                        P.mm(pn[:, :n], wuk[:, kc, h * 128:(h + 1) * 128], ckn[:, kc, :n], kc == 0, kc == 1)

                kproj(0)
                for h in range(NH):
                    pn = pb[1 + (h % 2)]
                    sqn_, rsh_ = sqn2[h % 2], rsh2[h % 2]
                    P.act(sqn_[:, :n], pn[:, :n], AF.Square)
                    if h + 1 < NH:
                        kproj(h + 1)
                    P.mm(pb[7][:, :n], ones[:], sqn_[:, :n], True, False)
                    P.mm(pb[7][:, :n], ones[:64, :], krsq[:, :n], False, True)
                    P.rstd(rsh_[:, :n], pb[7][:, :n], 1.0 / 192, epsb[:])
                    o_ = evb[h % 2]
                    P.stt(o_[:, :n], pn[:, :n], C("gk_n"), rsh_[:, :n], ALU.mult, ALU.mult)
                    P.dma(kn_o[h, :, c0:c0 + n], o_[:, :n])
                    o6 = ob64[h % 2]
                    P.tt(o6[:, :n], Rt[:, :n], rsh_[:64, :n], ALU.mult)
                    P.dma(kr_o[h, :, c0:c0 + n], o6[:, :n])
                for j in range(n // 128):
                    vt = vb[j % 2]
                    for hh in range(2):
                        pv = pb[3 + hh]
                        for kc in range(2):
                            P.mm(pv[:, :], ckn[:, kc, j * 128:(j + 1) * 128], wuv[:, kc, hh * 512:(hh + 1) * 512], kc == 0, kc == 1)
                        P.act(vt[:, hh * 512:(hh + 1) * 512], pv[:, :], AF.Copy)
                    P.dma(v_o[c0 + j * 128: c0 + (j + 1) * 128, :], vt[:])
    P.phase_end(mk)


def emit_A2(P, pb, G, l, T, NCX):
    mk = P.phase_begin()
    xr, gg, yr = G["xrT"], G["ggT"], G["yr"]
    cvt = P.sb("L_cvt", [128, 88])
    P.dma(cvt[:], G["cvl"][l])
    one = P.sb("L_one", [128, 1])
    P.memset(one[:], 1.0)
    cl = P.sb("L_cl", [128, 16])
    P.act(cl[:], cvt[:, 72:88], AF.Exp, scale=-1.0)
    P.act(cl[:], cl[:], AF.Ln, bias=one[:])
    P.ts(cl[:], cl[:], -8.0, None, ALU.mult)
    wab = P.sb("L_wab", [128, 2, 8, 128], BF16)
    wxb = P.sb("L_wxb", [128, 2, 8, 128], BF16)
    for d in range(2):
        P.dma(wab[:, d, :, :], G["lru_wa"][l, d].rearrange("g i j -> i g j"), q="pool")
        P.dma(wxb[:, d, :, :], G["lru_wx"][l, d].rearrange("g i j -> i g j"), q="pool")
    streams = [("C", T, NCX), ("L", 0, T)]
    tl = {}
    for s, _, n in streams:
        X = P.sb("L_X" + s, [128, n + 3])
        tl[s] = dict(X=X, u=P.sb("L_u" + s, [128, n]), ub=P.sb("L_ub" + s, [128, n], BF16),
                     ra=[P.sb("L_ra%d" % d + s, [128, n]) for d in range(2)],
                     ii=[P.sb("L_ii%d" % d + s, [128, n]) for d in range(2)],
                     sb=[P.sb("L_sb%d" % d + s, [128, n]) for d in range(2)],
                     hf=P.sb("L_hf" + s, [128, n]),
                     gg=P.sb("L_gg" + s, [128, n], BF16), yo=P.sb("L_yo" + s, [128, n], BF16))
    for g in range(8):
        rows = slice(g * 128, (g + 1) * 128)
        for s, s0, n in streams:
            t = tl[s]
            P.memset(t["X"][:, 0:2], 0.0)
            P.memset(t["X"][:, n + 2:n + 3], 0.0)
            P.dma(t["X"][:, 2:n + 2], xr[rows, s0:s0 + n])
            P.dma(t["gg"][:], gg[rows, s0:s0 + n])
            P.ts(t["u"][:], t["X"][:, 0:n], cvt[:, g * 4:g * 4 + 1], cvt[:, 32 + g:33 + g], ALU.mult, ALU.add)
            for k in range(1, 4):
                P.stt(t["u"][:], t["X"][:, k:k + n], cvt[:, g * 4 + k:g * 4 + k + 1], t["u"][:], ALU.mult, ALU.add)
            P.act(t["ub"][:], t["u"][:], AF.Copy)
        for d in range(2):
            for s, s0, n in streams:
                t = tl[s]
                ra, ii, sb = t["ra"][d], t["ii"][d], t["sb"][d]
                for ci, (c0, cn) in enumerate(chunks(n)):
                    pr, pi = pb[(ci % 2) * 2], pb[(ci % 2) * 2 + 1]
                    P.mm(pr[:, :cn], wab[:, d, g, :], t["ub"][:, c0:c0 + cn], True, True)
                    P.mm(pi[:, :cn], wxb[:, d, g, :], t["ub"][:, c0:c0 + cn], True, True)
                    P.act(ra[:, c0:c0 + cn], pr[:, :cn], AF.Sigmoid, bias=cvt[:, 40 + d * 8 + g:41 + d * 8 + g])
                    P.act(ii[:, c0:c0 + cn], pi[:, :cn], AF.Sigmoid, bias=cvt[:, 56 + d * 8 + g:57 + d * 8 + g])
                P.act(ra[:], ra[:], AF.Exp, scale=cl[:, d * 8 + g:d * 8 + g + 1])
                P.act(sb[:], ra[:], AF.Square)
                P.act(sb[:], sb[:], AF.Sqrt, bias=one[:], scale=-1.0)
        for d in range(2):
            for s, s0, n in streams:
                t = tl[s]
                ra, ii, sb = t["ra"][d], t["ii"][d], t["sb"][d]
                P.tt(ii[:], ii[:], t["u"][:], ALU.mult)
                P.tt(sb[:], sb[:], ii[:], ALU.mult)
                if d == 0:
                    init = 0.0 if s == "C" else tl["C"]["hf"][:, NCX - 1:NCX]
                    P.scan(t["hf"][:], ra[:], sb[:], init, ALU.mult, ALU.add)
                else:
                    hb = t["X"][:, 0:n]
                    init = 0.0 if s == "C" else tl["C"]["X"][:, 0:1]
                    P.scan(hb[:, ::-1], ra[:, ::-1], sb[:, ::-1], init, ALU.mult, ALU.add)
        for s, s0, n in streams:
            t = tl[s]
            P.tt(t["hf"][:], t["hf"][:], t["X"][:, 0:n], ALU.add)
            P.tt(t["yo"][:], t["hf"][:], t["gg"][:], ALU.mult)
            P.dma(yr[rows, s0:s0 + n], t["yo"][:])
    P.phase_end(mk)


def emit_B(P, pb, G, l, xT, T, NCX, do_ctx):
    NT = T + NCX
    NKT = NT // 128
    NI = T // 128
    modfm = G["modfm"]
    aff_sb = G["aff_sb"]
    HQ = min(2048, T)
    for qp in range(T // HQ):
        mk = P.phase_begin()
        qch = [(qp * HQ + c0, n, 0, c0) for c0, n in chunks(HQ)]
        NQP = HQ
        if do_ctx and qp == 0:
            qch += [(T + c0, n, 1, HQ + c0) for c0, n in chunks(NCX)]
            NQP = HQ + NCX
        cvt = P.sb("B_cvt", [128, CV_B["n"]])
        P.dma(cvt[:], G["cvB"][l])
        P.copy(cvt[:, 0:16], modfm[:, l, 32:48, 0])
        P.copy(cvt[:, 16:32], modfm[:, l, 32:48, 1])
        P.copy(cvt[:, 48:64], modfm[:, l, 48:64, 0])
        P.copy(cvt[:, 64:80], modfm[:, l, 64:80, 0])
        P.copy(cvt[:, 80:96], modfm[:, l, 48:64, 1])
        P.copy(cvt[:, 96:112], modfm[:, l, 64:80, 1])
        C = lambda name, j=0, w=1: cvt[:, CV_B[name] + j: CV_B[name] + j + w]
        epsb = P.sb("B_epsb", [128, 1])
        P.memset(epsb[:], EPS)
        ones = P.sb("B_ones", [128, 128])
        P.memset(ones[:], 1.0)
        onesb = P.sb("B_onesb", [128, 128], BF16)
        P.memset(onesb[:], 1.0)
        ident = P.sb("B_ident", [128, 128])
        P.dma(ident[:], G["ident"])
        A2m = P.sb("B_A2m", [128, 2, KC])
        for k, nm in enumerate(("sc2_l", "sc2_c")):
            P.ts(A2m[:, k, :], C(nm, 0, KC), 1.0, None, ALU.add)
            P.tt(A2m[:, k, :], A2m[:, k, :], C("n2", 0, KC), ALU.mult)
        rt = P.sb("B_rt", [128, KC, NE])
        P.dma(rt[:], G["router"][l].rearrange("(kc p) e -> p kc e", p=128))

        ymix = P.sb("B_ymix", [128, 16, NQP], BF16)
        yrv = G["yr"].rearrange("(c p) n -> p c n", p=128)
        P.dma(ymix[:, 0:8, 0:HQ], yrv[:, :, qp * HQ:(qp + 1) * HQ])
        if NQP > HQ:
            P.dma(ymix[:, 0:8, HQ:NQP], yrv[:, :, T:NT])
        knt = P.sb("B_knt", [128, NT], BF16)
        krt = P.sb("B_krt", [64, NT], BF16)
        vt = P.sb("B_vt", [128, NKT, 128], BF16)
        qnt = P.sb("B_qnt", [128, NQP], BF16)
        qrt = P.sb("B_qrt", [64, NQP], BF16)
        pt = [P.sb("B_pt%d" % i, [128, 512], BF16) for i in range(3)]
        rec = P.sb("B_rec", [128, 512])
        it = 0
        for h in range(NH):
            P.dma(knt[:], G["kn"][h])
            P.dma(krt[:], G["kr"][h])
            P.dma(vt[:], G["v"][:, h * 128:(h + 1) * 128].rearrange("(j p) d -> p j d", p=128))
            P.dma(qnt[:, 0:HQ], G["qn"][h, :, qp * HQ:(qp + 1) * HQ])
            P.dma(qrt[:, 0:HQ], G["qr"][h, :, qp * HQ:(qp + 1) * HQ])
            if NQP > HQ:
                P.dma(qnt[:, HQ:NQP], G["qn"][h, :, T:NT])
                P.dma(qrt[:, HQ:NQP], G["qr"][h, :, T:NT])
            for (ca, n, kind, c0) in qch:
                kts = list(range(NKT)) if kind == 0 else list(range(NI, NKT))
                SB = (pb[0], pb[1], pb[4], pb[5])

                def qk(ji):
                    S = SB[ji % 4]
                    j = kts[ji]
                    P.mm(S[:, :n], knt[:, j * 128:(j + 1) * 128], qnt[:, c0:c0 + n], True, False)
                    P.mm(S[:, :n], krt[:, j * 128:(j + 1) * 128], qrt[:, c0:c0 + n], False, True)

                for ji in range(min(2, len(kts))):
                    qk(ji)
                for ji, j in enumerate(kts):
                    if ji + 2 < len(kts):
                        qk(ji + 2)
                    p_ = pt[ji % 3]
                    P.act(p_[:, :n], SB[ji % 4][:, :n], AF.Exp, scale=ATT_SCALE)
                    P.mm(pb[2][:, :n], vt[:, j, :], p_[:, :n], ji == 0, ji == len(kts) - 1)
                    P.mm(pb[3][:, :n], onesb[:], p_[:, :n], ji == 0, ji == len(kts) - 1)
                P.recip(rec[:, :n], pb[3][:, :n])
                P.tt(ymix[:, 8 + h, c0:c0 + n], pb[2][:, :n], rec[:, :n], ALU.mult)

        xc = P.sb("B_xc", [128, KC, 512])
        wob = [P.sb("B_wob%d" % i, [128, KC, 256], BF16) for i in range(2)]
        sq = [P.sb("B_sq%d" % i, [128, 512]) for i in range(2)]
        htm = [P.sb("B_htm%d" % i, [128, D], BF16) for i in range(4)]
        rs = P.sb("B_rs", [128, 512])
        mx = P.sb("B_mx", [128, 1])
        sm = P.sb("B_sm", [128, 1])
        ex = P.sb("B_ex", [128, NE])
        xv = xT.rearrange("(kc p) n -> p kc n", p=128)
        xnv = G["xn"].rearrange("(kc p) n -> p kc n", p=128)
        wov = G["w_out"][l].rearrange("(kc p) n -> p kc n", p=128)
        it = 0
        for (ca, n, kind, c0) in qch:
            nj = n // 128
            P.dma(xc[:, :, :n], xv[:, :, ca:ca + n], q="pool")
            g1n = "g1_l" if kind == 0 else "g1_c"
            shn = "sh2_l" if kind == 0 else "sh2_c"
            for mg in range(8):
                wo = wob[it % 2]
                it += 1
                P.dma(wo[:], wov[:, :, mg * 256:(mg + 1) * 256], q="pool")
                for mi in range(2):
                    m = mg * 2 + mi
                    p_ = pb[m % 2]
                    for kc in range(KC):
                        P.mm(p_[:, :n], wo[:, kc, mi * 128:(mi + 1) * 128], ymix[:, kc, c0:c0 + n], kc == 0, kc == KC - 1)
                    P.stt(xc[:, m, :n], p_[:, :n], C(g1n, m), xc[:, m, :n], ALU.mult, ALU.add)
            P.dma(xnv[:, :, ca:ca + n], xc[:, :, :n])
            for m in range(KC):
                P.act(sq[m % 2][:, :n], xc[:, m, :n], AF.Square)
                P.mm(pb[2][:, :n], ones[:], sq[m % 2][:, :n], m == 0, m == KC - 1)
            P.rstd(rs[:, :n], pb[2][:, :n], 1.0 / D, epsb[:])
            for m in range(KC):
                P.tt(xc[:, m, :n], xc[:, m, :n], rs[:, :n], ALU.mult)
                P.ts(xc[:, m, :n], xc[:, m, :n], A2m[:, kind, m:m + 1], C(shn, m), ALU.mult, ALU.add)
                for j in range(nj):
                    P.emit("pe", lambda en, j=j, m=m: en.transpose(pb[4 + j][:, (m % 4) * 128:(m % 4 + 1) * 128],
                                                                 xc[:, m, j * 128:(j + 1) * 128], ident[:]),
                           [xc, ident], [pb[4 + j]])
                if m % 4 == 3:
                    for j in range(nj):
                        dst = htm[j][:, (m // 4) * 512:(m // 4 + 1) * 512]
                        if j % 2 == 0:
                            P.copy(dst, pb[4 + j][:, :])
                        else:
                            P.act(dst, pb[4 + j][:, :], AF.Copy)
            for j in range(nj):
                P.dma(G["h2tm"][ca + j * 128:ca + (j + 1) * 128, :], htm[j][:])
                for m in range(KC):
                    P.mm(pb[j][:, :NE], xc[:, m, j * 128:(j + 1) * 128], rt[:, m, :], m == 0, m == KC - 1)
                lg = pb[j][:, :NE]
                ti = (ca // 128) + j
                P.emit("dve", lambda en, lg=lg: en.tensor_reduce(mx[:], lg, AX.X, ALU.max), [lg], [mx])
                P.ts(mx[:], mx[:], -1.0, None, ALU.mult)
                P.act(ex[:], lg, AF.Exp, bias=mx[:])
                P.emit("dve", lambda en: en.tensor_reduce(sm[:], ex[:], AX.X, ALU.add), [ex], [sm])
                P.recip(sm[:], sm[:])
                P.ts(aff_sb[:, :, ti], ex[:], sm[:, 0:1], None, ALU.mult)
        P.phase_end(mk)


def emit_D(P, pb, G, l, T, NCX, do_ctx):
    mk = P.phase_begin()
    NI, NIC = T // 128, NCX // 128
    cap, capc = 2 * T // NE, 2 * NCX // NE
    NU = NE
    aff_sb = G["aff_sb"]
    ones = P.sb("D_ones", [128, 128])
    P.memset(ones[:], 1.0)
    onesb = P.sb("D_onesb", [128, 128], BF16)
    P.memset(onesb[:], 1.0)
    utf = P.sb("D_utf", [128, 128])
    P.dma(utf[:], G["ut"])
    utb = P.sb("D_utb", [128, 128], BF16)
    P.copy(utb[:], utf[:])
    iott = P.sb("D_iott", [128, 512])
    P.dma(iott[:], G["iot"])
    zer = P.sb("D_zer", [128, 64])
    P.memset(zer[:], 0.0)

    streams = [("L", aff_sb[:, :, 0:NI], G["keyL"], NI, cap)]
    if do_ctx:
        streams.append(("C", aff_sb[:, :, NI:NI + NIC], G["keyC"], NIC, capc))
    NS = len(streams)
    lo = P.sb("D_lo", [128, NS, NU])
    mid = P.sb("D_mid", [128, NS, NU])
    cnt = P.sb("D_cnt", [128, NS, NU])
    capt = P.sb("D_capt", [128, NS, NU])
    ge = P.sb("D_ge", [128, NS, NU], I32)
    cms = [P.sb("D_cm" + tg, [128, NU, ni]) for (tg, _, _, ni, _) in streams]
    P.memset(lo[:], 0.0)
    for si, (_, _, _, _, cv_) in enumerate(streams):
        P.memset(capt[:, si, :], float(cv_))
    lof = lo[:].rearrange("p s u -> p (s u)")
    midf = mid[:].rearrange("p s u -> p (s u)")
    for k in range(N_BISECT):
        P.ts(midf, lof, float(2.0 ** -(k + 1)), None, ALU.add)
        for si, (_, af, _, ni, _) in enumerate(streams):
            P.tt(cms[si][:], af, mid[:, si, :].unsqueeze(2).to_broadcast([128, NU, ni]), ALU.is_ge)
            P.emit("dve", lambda en, si=si: en.tensor_reduce(cnt[:, si, :], cms[si][:], AX.X, ALU.add), [cms[si]], [cnt])
        P.mm(pb[0][:, :NS * NU], ones[:], cnt[:].rearrange("p s u -> p (s u)"), True, True)
        P.tt(ge[:].rearrange("p s u -> p (s u)"), pb[0][:, :NS * NU], capt[:].rearrange("p s u -> p (s u)"), ALU.is_ge)
        P.emit("dve", lambda en: en.copy_predicated(lof, ge[:].rearrange("p s u -> p (s u)"), midf), [ge, mid, lo], [lo])
    for si, (tag, af, pos, ni, _) in enumerate(streams):
        n3 = [128, NU, ni]
        cm = cms[si]
        P.tt(cm[:], af, lo[:, si, :].unsqueeze(2).to_broadcast(n3), ALU.is_ge)
        mb = P.sb("D_mb" + tag, [128, NU * ni], BF16)
        P.copy(mb[:], cm[:].rearrange("p u i -> p (u i)"))
        tot = P.sb("D_tot" + tag, n3)
        inc = P.sb("D_inc" + tag, n3)
        wit = P.sb("D_wit" + tag, n3)
        for c0, cn in chunks(NU * ni):
            P.mm(pb[1][:, :cn], utb[:], mb[:, c0:c0 + cn], True, True)
            P.mm(pb[2][:, :cn], onesb[:], mb[:, c0:c0 + cn], True, True)
            P.copy(wit[:].rearrange("p u i -> p (u i)")[:, c0:c0 + cn], pb[1][:, :cn])
            P.copy(tot[:].rearrange("p u i -> p (u i)")[:, c0:c0 + cn], pb[2][:, :cn])
        for u in range(NU):
            P.scan(inc[:, u, :], tot[:, u, :], zer[:, :ni], 0.0, ALU.add, ALU.add)
        P.tt(inc[:], inc[:], tot[:], ALU.subtract)
        P.tt(pos[:], inc[:], wit[:], ALU.add)
        P.tt(pos[:], pos[:], cm[:], ALU.mult)
        P.ts(pos[:], pos[:], -1.0, None, ALU.add)
    keyL, keyC = G["keyL"], G["keyC"]
    NT = T + NCX
    for e in range(NE):
        P.dma(G["keyD"][e, 0:T].rearrange("(i p) -> p i", p=128), keyL[:, e, :], slow=True, q="sp" if e % 2 == 0 else "pool")
        P.dma(G["gateD"][e, :].rearrange("(i p) -> p i", p=128), aff_sb[:, e, :], slow=True, q="pool" if e % 2 == 0 else "sp")
        if do_ctx:
            P.dma(G["keyD"][e, T:NT].rearrange("(i p) -> p i", p=128), keyC[:, e, :], slow=True)

    xg = P.sb("D_xg", [128, KC, cap], BF16)
    at = P.sb("D_at", [128, KC, cap], BF16)
    if do_ctx:
        xgc = P.sb("D_xgc", [128, KC, capc], BF16)
        atc = P.sb("D_atc", [128, KC, capc], BF16)
    selr = [P.sb("D_sel%d" % i, [128, 512], BF16) for i in range(3)]
    h2t = [P.sb("D_h2t%d" % i, [128, 1024], BF16) for i in range(3)]
    WG = 512
    wbuf = [P.sb("D_wbuf%d" % i, [128, KC, WG], BF16) for i in range(4)]
    sg = [P.sb("D_sg%d" % i, [128, 512]) for i in range(2)]
    yo = [P.sb("D_yo%d" % i, [128, 512], BF16) for i in range(2)]
    cnt_it = [0, 0, 0]
    h2tm = G["h2tm"]

    def gather(row0, key_t, u, ni, capv, dst):
        for half in range(2):
            for i in range(ni):
                s_ = selr[cnt_it[0] % 3]
                h_ = h2t[cnt_it[0] % 3]
                cnt_it[0] += 1
                P.ts(s_[:, :capv], iott[:, :capv], key_t[:, u, i:i + 1], None, ALU.is_equal)
                P.dma(h_[:], h2tm[row0 + i * 128:row0 + (i + 1) * 128, half * 1024:(half + 1) * 1024])
                for dc in range(8):
                    P.mm(pb[dc][:, :capv], h_[:, dc * 128:(dc + 1) * 128], s_[:, :capv], i == 0, i == ni - 1)
            for dc in range(8):
                if dc % 2 == 0:
                    P.copy(dst[:, half * 8 + dc, :capv], pb[dc][:, :capv])
                else:
                    P.act(dst[:, half * 8 + dc, :capv], pb[dc][:, :capv], AF.Copy)

    for e in range(NE):
        gather(0, keyL, e, NI, cap, xg)
        cks = [(xg, at, cap, G["ysl"])]
        if do_ctx:
            gather(T, keyC, e, NIC, capc, xgc)
            cks.append((xgc, atc, capc, G["ycsl"]))
        wgv = G["w_gate"][l, e].rearrange("(kc p) n -> p kc n", p=128)
        wuv = G["w_up"][l, e].rearrange("(kc p) n -> p kc n", p=128)
        wdv = G["w_down"][l, e].rearrange("(kc p) n -> p kc n", p=128)
        for fg in range(D // WG):
            wg_t = wbuf[(cnt_it[1] * 2) % 4]
            wu_t = wbuf[(cnt_it[1] * 2 + 1) % 4]
            cnt_it[1] += 1
            P.dma(wg_t[:], wgv[:, :, fg * WG:(fg + 1) * WG], q="pool")
            P.dma(wu_t[:], wuv[:, :, fg * WG:(fg + 1) * WG], q="pool")
            for (xs, as_, n, _) in cks:
                for fi in range(WG // 128):
                    f = fg * (WG // 128) + fi
                    pg, pu = pb[(f % 2) * 2], pb[(f % 2) * 2 + 1]
                    for kc in range(KC):
                        P.mm(pg[:, :n], wg_t[:, kc, fi * 128:(fi + 1) * 128], xs[:, kc, :n], kc == 0, kc == KC - 1)
                    for kc in range(KC):
                        P.mm(pu[:, :n], wu_t[:, kc, fi * 128:(fi + 1) * 128], xs[:, kc, :n], kc == 0, kc == KC - 1)
                    s_ = sg[f % 2]
                    P.act(s_[:, :n], pg[:, :n], AF.Silu)
                    P.tt(as_[:, f, :n], s_[:, :n], pu[:, :n], ALU.mult)
        for dg in range(D // WG):
            wd_t = wbuf[cnt_it[2] % 4]
            cnt_it[2] += 1
            P.dma(wd_t[:], wdv[:, :, dg * WG:(dg + 1) * WG], q="pool")
            for (xs, as_, n, ydst) in cks:
                for st in range((n + 127) // 128):
                    sw = min(128, n - st * 128)
                    py = pb[4 + (st % 2)]
                    for fc in range(KC):
                        P.mm(py[:sw, :WG], as_[:, fc, st * 128:st * 128 + sw], wd_t[:, fc, :], fc == 0, fc == KC - 1)
                    y_ = yo[st % 2]
                    P.act(y_[:sw, :WG], py[:sw, :WG], AF.Copy)
                    P.dma(ydst[e, st * 128:st * 128 + sw, dg * WG:(dg + 1) * WG], y_[:sw, :WG])
    P.phase_end(mk)


def emit_E(P, pb, G, l, T, NCX, do_ctx, xo):
    mk = P.phase_begin()
    cap, capc = 2 * T // NE, 2 * NCX // NE
    KL = min(128, cap)
    S = cap // KL
    modfm = G["modfm"]
    sidt = P.sb("E_sid", [128, 4])
    P.dma(sidt[:], G["sid"])
    Yt = P.sb("E_Yt", [KL, NE, S, 1024], BF16)
    if do_ctx:
        Yc = P.sb("E_Yc", [capc, NE, 1024], BF16)
    kgb = [P.sb("E_kgb%d" % i, [128, 2, 512]) for i in range(2)]
    selT = [P.sb("E_selT%d" % i, [128, 512], BF16) for i in range(3)]
    xs = [P.sb("E_xs%d" % i, [128, 512]) for i in range(2)]
    ot = [P.sb("E_ot%d" % i, [128, 512]) for i in range(2)]
    qch = [(c0, n, 0) for c0, n in chunks(T)]
    if do_ctx:
        qch += [(T + c0, n, 1) for c0, n in chunks(NCX)]
    it = 0
    for half in range(2):
        hs = slice(half * 1024, (half + 1) * 1024)
        for e in range(NE):
            P.dma(Yt[:, e, :, :], G["ysl"][e].rearrange("(s p) d -> p s d", p=KL)[:, :, hs], q="sp" if e % 2 == 0 else "pool")
        if do_ctx:
            P.dma(Yc[:], G["ycsl"].rearrange("e s d -> s e d")[:, :, hs])
        for (c0, n, kind) in qch:
            for e in range(NE):
                k_ = kgb[e % 2]
                ns, K = (S, KL) if kind == 0 else (1, capc)
                P.dma(k_[:, 0, :n], G["keyD"][e, c0:c0 + n].partition_broadcast(128))
                P.dma(k_[:, 1, :n], G["gateD"][e, c0:c0 + n].partition_broadcast(128), q="pool")
                for s in range(ns):
                    st = selT[it % 3]
                    it += 1
                    P.stt(st[:K, :n], k_[:K, 0, :n], sidt[:K, s:s + 1], k_[:K, 1, :n], ALU.is_equal, ALU.mult)
                    first = (e == 0 and s == 0)
                    last = (e == NE - 1 and s == ns - 1)
                    for dc in range(8):
                        lhs = Yt[:KL, e, s, dc * 128:(dc + 1) * 128] if kind == 0 else Yc[:, e, dc * 128:(dc + 1) * 128]
                        P.mm(pb[dc][:, :n], lhs, st[:K, :n], first, last)
            for dc in range(8):
                m = half * 8 + dc
                x_ = xs[dc % 2]
                o_ = ot[dc % 2]
                P.dma(x_[:, :n], G["xn"][m * 128:(m + 1) * 128, c0:c0 + n])
                P.stt(o_[:, :n], pb[dc][:, :n], modfm[:, l, 80 + m, kind:kind + 1], x_[:, :n], ALU.mult, ALU.add)
                P.dma(xo[m * 128:(m + 1) * 128, c0:c0 + n], o_[:, :n])
    P.phase_end(mk)


_PROG_CACHE = {}


def build_fused(T, NCX):
    NT = T + NCX
    NI, NIC = T // 128, NCX // 128
    cap, capc = 2 * T // NE, 2 * NCX // NE
    P = Prog()
    G = {}
    f32in = dict(xT0=[D, NT], sT=[128, KC, 2], bmod=[128, 2, 96], w_mod=[2, D, 6 * D], cvA=[2, 128, CV_A["n"]],
                 w_in=[2, D, W_IN_EXT], w_uq=[2, 512, 2048], w_uk=[2, 256, 1024], w_uv=[2, 256, 1024],
                 ropec=[64, T], ropes=[64, T], cvl=[2, 128, 88], lru_wa=[2, 2, 8, 128, 128], lru_wx=[2, 2, 8, 128, 128],
                 cvB=[2, 128, CV_B["n"]], w_out=[2, D, D], router=[2, D, NE], ident=[128, 128], ut=[128, 128],
                 iot=[128, 512], sid=[128, 4], w_gate=[2, NE, D, D], w_up=[2, NE, D, D], w_down=[2, NE, D, D])
    for k, shp in f32in.items():
        G[k] = P.din(k, shp)
    out = P.dout("out", [D, T])
    G["hm_d"] = P.dscr("hm_d", [128, KC, NT], BF16, track=True)
    G["xrT"] = P.dscr("xrT", [1024, NT])
    G["ggT"] = P.dscr("ggT", [1024, NT], BF16)
    G["qn"] = P.dscr("qn", [NH, 128, NT], BF16)
    G["qr"] = P.dscr("qr", [NH, 64, NT], BF16)
    G["kn"] = P.dscr("kn", [NH, 128, NT], BF16)
    G["kr"] = P.dscr("kr", [NH, 64, NT], BF16)
    G["v"] = P.dscr("v", [NT, 1024], BF16)
    G["yr"] = P.dscr("yr", [1024, NT], BF16)
    G["xn"] = P.dscr("xn", [D, NT])
    G["h2tm"] = P.dscr("h2tm", [NT, D], BF16)
    G["ysl"] = P.dscr("ysl", [NE, cap, D], BF16)
    G["ycsl"] = P.dscr("ycsl", [NE, capc, D], BF16)
    G["keyD"] = P.dscr("keyD", [NE, NT])
    G["gateD"] = P.dscr("gateD", [NE, NT])
    x1 = P.dscr("x1", [D, NT])
    pb = [P.ps("pb%d" % i, [128, 512]) for i in range(8)]
    G["modfm"] = P.sb("modfm", [128, 2, 96, 2])
    G["aff_sb"] = P.sb("aff_sb", [128, NE, NI + NIC])
    G["keyL"] = P.sb("keyL", [128, NE, NI])
    G["keyC"] = P.sb("keyC", [128, NE, NIC])
    emit_M(P, pb, G)
    xT = G["xT0"]
    for l in range(2):
        do_ctx = (l == 0)
        emit_A(P, pb, G, l, xT, T, NCX)
        emit_A2(P, pb, G, l, T, NCX)
        emit_B(P, pb, G, l, xT, T, NCX, do_ctx)
        emit_D(P, pb, G, l, T, NCX, do_ctx)
        emit_E(P, pb, G, l, T, NCX, do_ctx, x1 if l == 0 else out)
        xT = x1
    print("[build_fused] instructions:", P.n_ins)
    return P.finish()


def host_inputs(inp, b):
    x, ctx = inp["x"], inp["ctx"]
    T = x.shape[1]
    m = {}
    m["xT0"] = np.ascontiguousarray(np.concatenate([x[b].T, ctx[b].T], axis=1), dtype=np.float32)
    C2 = np.stack([inp["c"][b], inp["c_ctx"]], axis=0).astype(np.float32)
    m["sT"] = np.ascontiguousarray(C2.T.reshape(KC, 128, 2).transpose(1, 0, 2))
    m["bmod"] = np.ascontiguousarray(np.stack([fm(inp["b_mod"][l]) for l in range(2)], axis=1))
    m["w_mod"] = inp["w_mod"]
    z16 = np.zeros((128, 16), np.float32)
    cvA, cvl, cvB, w_in, w_uq, w_uk, w_uv = [], [], [], [], [], [], []
    for l in range(2):
        qn, kn = inp["q_norm"][l], inp["k_norm"][l]
        cvA.append(np.concatenate([fm(inp["norm1"][l]), z16, z16, z16, z16, fm(inp["q_a_norm"][l]), fm(inp["kv_a_norm"][l]),
                                   pad128(qn[:128]), pad128(qn[128:]), pad128(qn[128 + _SW]),
                                   pad128(kn[:128]), pad128(kn[128:]), pad128(kn[128 + _SW])], axis=1))
        a, b_, c_, d_ = a_weights(inp, l)
        w_in.append(a); w_uq.append(b_); w_uk.append(c_); w_uv.append(d_)
        cw, cb = inp["conv_w"][l], inp["conv_b"][l]
        cols = [cw.reshape(4, 8, 128).transpose(2, 1, 0).reshape(128, 32), cb.reshape(8, 128).T]
        for nm in ("lru_ba", "lru_bx", "lru_lambda"):
            cols.append(inp[nm][l].reshape(2, 8, 128).transpose(2, 0, 1).reshape(128, 16))
        cvl.append(np.concatenate(cols, axis=1))
        cvB.append(np.concatenate([z16, z16, fm(inp["norm2"][l]), z16, z16, z16, z16], axis=1))
    m["cvA"] = np.ascontiguousarray(np.stack(cvA).astype(np.float32))
    m["cvl"] = np.ascontiguousarray(np.stack(cvl).astype(np.float32))
    m["cvB"] = np.ascontiguousarray(np.stack(cvB).astype(np.float32))
    m["w_in"] = np.stack(w_in); m["w_uq"] = np.stack(w_uq); m["w_uk"] = np.stack(w_uk); m["w_uv"] = np.stack(w_uv)
    cos, sin = rope_tables(np.arange(T))
    m["ropec"], m["ropes"] = cos, sin
    m["lru_wa"], m["lru_wx"] = inp["lru_wa"], inp["lru_wx"]
    m["w_out"], m["router"] = inp["w_out"], inp["router"]
    m["ident"] = np.eye(128, dtype=np.float32)
    m["ut"] = np.triu(np.ones((128, 128), np.float32))
    m["iot"] = np.tile(np.arange(512, dtype=np.float32), (128, 1))
    m["sid"] = (np.arange(128, dtype=np.float32)[:, None] + 128.0 * np.arange(4, dtype=np.float32)[None, :]).astype(np.float32)
    m["w_gate"], m["w_up"], m["w_down"] = inp["w_gate"], inp["w_up"], inp["w_down"]
    return {k: np.ascontiguousarray(v, dtype=np.float32) for k, v in m.items()}


def kernel(**inputs):
    inp = {k: np.asarray(v) for k, v in inputs.items()}
    Bn, T, _ = inp["x"].shape
    NCX = inp["ctx"].shape[1]
    key = (T, NCX)
    if key not in _PROG_CACHE:
        _PROG_CACHE[key] = build_fused(T, NCX)
    nc = _PROG_CACHE[key]
    maps = [host_inputs(inp, b) for b in range(Bn)]
    res = run_bass_kernel_spmd(nc, maps, core_ids=list(range(Bn)))
    out = np.stack([np.ascontiguousarray(r["out"].T) for r in res.results])
    return out.astype(np.float32, copy=False)
```

```python
import numpy as np
import ml_dtypes
import concourse.bass as bass
import concourse.mybir as mybir
from concourse.bass_utils import run_bass_kernel_spmd

F32 = mybir.dt.float32
BF16 = mybir.dt.bfloat16
ALU = mybir.AluOpType
AF = mybir.ActivationFunctionType
AX = mybir.AxisListType
NPBF = ml_dtypes.bfloat16

D = 2048
KC = 16
NH = 8
NE = 16
EPS = 1e-6
ATT_SCALE = 192 ** -0.5
NCORES = 8


class Prog:
    ENG = ("pe", "dve", "act", "pool", "sp")

    def __init__(self):
        self.nc = bass.Bass("TRN2", target_bir_lowering=False)
        nc = self.nc
        self.eng = {"pe": nc.tensor, "dve": nc.vector, "act": nc.scalar,
                    "pool": nc.gpsimd, "sp": nc.sync}
        self._ctx = []
        self.csem, self.dsem, self.cnt = {}, {}, {}
        self.NDS = 20
        self.drr = {}
        for e in self.ENG:
            self.csem[e] = self._enter(nc.semaphore("c_" + e))
            self.cnt[("c", e)] = 0
        for e in ("sp", "pool"):
            self.drr[e] = 0
            for i in range(self.NDS):
                self.dsem[(e, i)] = self._enter(nc.semaphore("d_%s%d" % (e, i)))
                self.cnt[("d", e, i)] = 0
        self.waited = {e: {} for e in self.ENG}
        self.lastw, self.readers, self.tags = {}, {}, {}
        self.skip = set()
        self.n_ins = 0
        self.uid = 0

    def _enter(self, cm):
        v = cm.__enter__()
        self._ctx.append(cm)
        return v

    def sb(self, name, shape, dt=F32):
        self.uid += 1
        return self._enter(self.nc.sbuf_tensor("%s_u%d" % (name, self.uid), list(shape), dt))

    def ps(self, name, shape, dt=F32):
        return self._enter(self.nc.psum_tensor(name, list(shape), dt))

    def din(self, name, shape, dt=F32):
        self.skip.add(name)
        return self.nc.dram_tensor(name, list(shape), dt, kind="ExternalInput").ap()

    def dout(self, name, shape, dt=F32):
        self.skip.add(name)
        return self.nc.dram_tensor(name, list(shape), dt, kind="ExternalOutput").ap()

    def dscr(self, name, shape, dt=F32, track=False):
        if not track:
            self.skip.add(name)
        return self.nc.dram_tensor(name, list(shape), dt, kind="Internal").ap()

    @staticmethod
    def _nm(t):
        if isinstance(t, str):
            return t
        if hasattr(t, "tensor"):
            t = t.tensor
        return t.name

    def _norm(self, ks):
        out = []
        for k in ks:
            if k is None or isinstance(k, (int, float)):
                continue
            if isinstance(k, tuple):
                n = self._nm(k[0])
                if n not in self.skip:
                    out.append((n, k[1]))
            else:
                n = self._nm(k)
                if n not in self.skip:
                    out.append((n, None))
        return out

    def _conf(self, key):
        name, tag = key
        if tag is None:
            return [(name, t) for t in self.tags.get(name, ())] + [(name, None)]
        return [(name, tag), (name, None)]

    def _sem(self, sk):
        return self.csem[sk[1]] if sk[0] == "c" else self.dsem[(sk[1], sk[2])]

    def emit(self, e, build, reads=(), writes=(), dma=False):
        reads = self._norm(reads)
        writes = self._norm(writes)
        deps = {}

        def need(d):
            if d is not None and deps.get(d[0], 0) < d[1]:
                deps[d[0]] = d[1]

        for k in reads:
            for c in self._conf(k):
                need(self.lastw.get(c))
        for k in writes:
            for c in self._conf(k):
                need(self.lastw.get(c))
                for r in self.readers.get(c, ()):
                    need(r)
        engine = self.eng[e]
        for sk, v in deps.items():
            if sk == ("c", "pe") and e == "pe" and not dma:
                continue
            if self.waited[e].get(sk, 0) >= v:
                continue
            engine.wait_ge(self._sem(sk), v)
            self.waited[e][sk] = v
        if dma:
            sk = ("d", e, self.drr[e] % self.NDS)
            self.drr[e] += 1
            if self.cnt[sk] > self.waited[e].get(sk, 0):
                engine.wait_ge(self._sem(sk), self.cnt[sk])
                self.waited[e][sk] = self.cnt[sk]
        else:
            sk = ("c", e)
        ins = build(engine)
        self.cnt[sk] += 16 if dma else 1
        ins.then_inc(self._sem(sk), 16 if dma else 1)
        me = (sk, self.cnt[sk])
        for k in writes:
            if k[1] is None:
                for c in self._conf(k):
                    self.lastw.pop(c, None)
                    self.readers.pop(c, None)
            else:
                self.tags.setdefault(k[0], set()).add(k[1])
            self.lastw[k] = me
            self.readers[k] = []
        for k in reads:
            if k[1] is not None:
                self.tags.setdefault(k[0], set()).add(k[1])
            lst = self.readers.setdefault(k, [])
            lst.append(me)
            if len(lst) > 10:
                best = {}
                for s, v in lst:
                    if best.get(s, 0) < v:
                        best[s] = v
                self.readers[k] = list(best.items())
        self.n_ins += 1
        return me

    def dma(self, out, in_, q="sp", rd=None, wr=None, slow=False):
        kw = {"allow_slow_non_contiguous": True} if slow else {}
        return self.emit(q, lambda en: en.dma_start(out=out, in_=in_, **kw),
                         rd if rd is not None else [in_], wr if wr is not None else [out], dma=True)

    def mm(self, out, lhsT, rhs, start, stop):
        return self.emit("pe", lambda en: en.matmul(out, lhsT, rhs, start=start, stop=stop),
                         [lhsT, rhs], [out])

    def act(self, out, in_, func, bias=None, scale=None, e="act"):
        kw = {}
        if bias is not None:
            kw["bias"] = bias
        if scale is not None:
            kw["scale"] = scale
        return self.emit(e, lambda en: en.activation(out, in_, func, **kw), [in_, bias, scale], [out])

    def tt(self, out, a, b, op, e="dve"):
        return self.emit(e, lambda en: en.tensor_tensor(out, a, b, op), [a, b], [out])

    def ts(self, out, a, s1, s2, op0, op1=None, e="dve"):
        if op1 is None:
            return self.emit(e, lambda en: en.tensor_scalar(out, a, s1, None, op0), [a, s1], [out])
        return self.emit(e, lambda en: en.tensor_scalar(out, a, s1, s2, op0, op1), [a, s1, s2], [out])

    def stt(self, out, in0, scalar, in1, op0, op1):
        return self.emit("dve", lambda en: en.scalar_tensor_tensor(out, in0, scalar, in1, op0, op1),
                         [in0, scalar, in1], [out])

    def copy(self, out, in_, e="dve"):
        return self.emit(e, lambda en: en.tensor_copy(out, in_), [in_], [out])

    def memset(self, out, val, e="dve"):
        return self.emit(e, lambda en: en.memset(out, val), [], [out])

    def scan(self, out, d0, d1, init, op0, op1):
        return self.emit("dve", lambda en: en.tensor_tensor_scan(out, d0, d1, init, op0, op1),
                         [d0, d1, init], [out])

    def recip(self, out, in_):
        return self.emit("dve", lambda en: en.reciprocal(out, in_), [in_], [out])

    def rstd(self, out, ss, inv_n, epsb, tmp=None):
        self.act(out, ss, AF.Sqrt, bias=epsb, scale=inv_n)
        self.recip(out, out)

    def barrier(self):
        for e in self.ENG:
            engine = self.eng[e]
            for sk, v in self.cnt.items():
                if v > self.waited[e].get(sk, 0):
                    engine.wait_ge(self._sem(sk), v)
                    self.waited[e][sk] = v
        self.lastw.clear()
        self.readers.clear()
        self.tags.clear()

    def phase_begin(self):
        return len(self._ctx)

    def phase_end(self, mark):
        self.barrier()
        while len(self._ctx) > mark:
            self._ctx.pop().__exit__(None, None, None)

    def finish(self):
        for sk, v in self.cnt.items():
            if v > 0:
                self.eng["sp"].wait_ge(self._sem(sk), v)
        while self._ctx:
            self._ctx.pop().__exit__(None, None, None)
        return self.nc


def fm(v):
    v = np.asarray(v, np.float32)
    return np.ascontiguousarray(v.reshape(-1, 128).T)


def chunks(n, step=512):
    return [(i, min(step, n - i)) for i in range(0, n, step)]


_SW = np.array([f + 16 if (f % 32) < 16 else f - 16 for f in range(64)])


def rope_tables(t_idx):
    t_idx = np.asarray(t_idx)
    row = (t_idx // 64).astype(np.float32)
    col = (t_idx % 64).astype(np.float32)
    inv = (np.float32(10000.0) ** (-np.arange(16, dtype=np.float32) / np.float32(16))).astype(np.float32)
    cos = np.zeros((64, len(t_idx)), np.float32)
    sin = np.zeros((64, len(t_idx)), np.float32)
    for f in range(64):
        pos = row if f < 32 else col
        ang = (pos * inv[f % 16]).astype(np.float32)
        cos[f] = np.cos(ang)
        s = np.sin(ang)
        sin[f] = -s if (f % 32) < 16 else s
    return cos, sin


def pad128(v):
    o = np.zeros((128, 1), np.float32)
    o[:len(v), 0] = v
    return o


def a_weights(inp, l):
    w_in = inp["w_in"][l]
    w_in_ext = np.ascontiguousarray(np.concatenate([w_in, w_in[:, 2816 + _SW]], axis=1))
    wq = inp["w_uq"][l].reshape(512, NH, 192)
    w_uq_ext = np.ascontiguousarray(np.concatenate([wq, wq[:, :, 128 + _SW]], axis=2).reshape(512, NH * 256))
    wkv = inp["w_ukv"][l].reshape(256, NH, 256)
    w_uk = np.ascontiguousarray(wkv[:, :, :128].reshape(256, 1024))
    w_uv = np.ascontiguousarray(wkv[:, :, 128:].reshape(256, 1024))
    return w_in_ext, w_uq_ext, w_uk, w_uv


I32 = mybir.dt.int32
N_BISECT = 34
W_IN_EXT = 2944
CV_A = dict(g1=0, sh_l=16, sc_l=32, sh_c=48, sc_c=64, gqa=80, gkva=84,
            gq_n=86, gq_r=87, gq_s=88, gk_n=89, gk_r=90, gk_s=91, n=92)
CV_B = dict(g1_l=0, g1_c=16, n2=32, sh2_l=48, sc2_l=64, sh2_c=80, sc2_c=96, n=112)


def emit_M(P, pb, G):
    mk = P.phase_begin()
    s2 = P.sb("M_s2", [128, KC, 2])
    bm = P.sb("M_bm", [128, 2, 96])
    wt = [P.sb("M_wt%d" % i, [128, KC, 512]) for i in range(2)]
    P.dma(s2[:], G["sT"])
    P.dma(bm[:], G["bmod"])
    P.act(s2[:], s2[:], AF.Silu)
    i = 0
    for l in range(2):
        wv = G["w_mod"][l].rearrange("(kc p) n -> p kc n", p=128)
        for cg in range(24):
            t = wt[i % 2]
            P.dma(t[:], wv[:, :, cg * 512:(cg + 1) * 512], q="sp" if i % 2 == 0 else "pool")
            i += 1
            for ci in range(4):
                ch = cg * 4 + ci
                p_ = pb[ch % 2]
                for kc in range(KC):
                    P.mm(p_[:, 0:2], t[:, kc, ci * 128:(ci + 1) * 128], s2[:, kc, :], kc == 0, kc == KC - 1)
                P.ts(G["modfm"][:, l, ch, :], p_[:, 0:2], bm[:, l, ch:ch + 1], None, ALU.add)
    P.phase_end(mk)


def emit_A(P, pb, G, l, xT, NLC, NCX):
    NT = NLC + NCX
    mk = P.phase_begin()
    modfm = G["modfm"]
    cvt = P.sb("A_cvt", [128, CV_A["n"]])
    P.dma(cvt[:], G["cvA"][l])
    P.copy(cvt[:, 16:32], modfm[:, l, 0:16, 0])
    P.copy(cvt[:, 32:48], modfm[:, l, 16:32, 0])
    P.copy(cvt[:, 48:64], modfm[:, l, 0:16, 1])
    P.copy(cvt[:, 64:80], modfm[:, l, 16:32, 1])
    C = lambda name, j=0, w=1: cvt[:, CV_A[name] + j: CV_A[name] + j + w]
    epsb = P.sb("A_epsb", [128, 1])
    P.memset(epsb[:], EPS)
    ones = P.sb("A_ones", [128, 128])
    P.memset(ones[:], 1.0)
    Am = P.sb("A_Am", [128, 2, KC])
    for k, nm in enumerate(("sc_l", "sc_c")):
        P.ts(Am[:, k, :], C(nm, 0, KC), 1.0, None, ALU.add)
        P.tt(Am[:, k, :], Am[:, k, :], C("g1", 0, KC), ALU.mult)
    rc_t = P.sb("A_rc", [64, 512])
    rs_t = P.sb("A_rs", [64, 512])
    tch = [(c0, n, 0) for c0, n in chunks(NLC)] + [(NLC + c0, n, 1) for c0, n in chunks(NCX)]
    hm_d = G["hm_d"]
    xt = P.sb("A_xt", [128, KC, 512])
    sq = [P.sb("A_sq%d" % i, [128, 512]) for i in range(2)]
    rs = P.sb("A_rsd", [128, 512])
    hmc = [P.sb("A_hmc%d" % i, [128, KC, 512], BF16) for i in range(3)]
    hm = hmc[0]
    xv = xT.rearrange("(kc p) n -> p kc n", p=128)
    for (c0, n, kind) in tch:
        P.dma(xt[:, :, :n], xv[:, :, c0:c0 + n], q="pool")
        for kc in range(KC):
            s_ = sq[kc % 2]
            P.act(s_[:, :n], xt[:, kc, :n], AF.Square)
            P.mm(pb[0][:, :n], ones[:], s_[:, :n], kc == 0, kc == KC - 1)
        P.rstd(rs[:, :n], pb[0][:, :n], 1.0 / D, epsb[:])
        shn = "sh_l" if kind == 0 else "sh_c"
        for kc in range(KC):
            s_ = sq[kc % 2]
            P.tt(s_[:, :n], xt[:, kc, :n], rs[:, :n], ALU.mult)
            P.ts(hm[:, kc, :n], s_[:, :n], Am[:, kind, kc:kc + 1], C(shn, kc), ALU.mult, ALU.add)
        P.dma(hm_d[:, :, c0:c0 + n], hm[:, :, :n], wr=[(hm_d, c0)])

    wg = [P.sb("A_wg%d" % i, [128, KC, 512], BF16) for i in range(2)]
    wuq = P.sb("A_wuq", [128, 4, 2048], BF16)
    wuk = P.sb("A_wuk", [128, 2, 1024], BF16)
    wuv = P.sb("A_wuv", [128, 2, 1024], BF16)
    P.dma(wuq[:], G["w_uq"][l].rearrange("(kc p) n -> p kc n", p=128), q="pool")
    P.dma(wuk[:], G["w_uk"][l].rearrange("(kc p) n -> p kc n", p=128), q="pool")
    P.dma(wuv[:], G["w_uv"][l].rearrange("(kc p) n -> p kc n", p=128), q="pool")
    wv = G["w_in"][l].rearrange("(kc p) n -> p kc n", p=128)
    ev = [P.sb("A_ev%d" % i, [128, 512]) for i in range(2)]
    evb = [P.sb("A_evb%d" % i, [128, 512], BF16) for i in range(2)]
    cqt = P.sb("A_cqt", [128, 4, 512])
    cqn = P.sb("A_cqn", [128, 4, 512], BF16)
    ckt = P.sb("A_ckt", [128, 2, 512])
    ckn = P.sb("A_ckn", [128, 2, 512], BF16)
    krt = P.sb("A_krt", [64, 512])
    kst = P.sb("A_kst", [64, 512])
    krsq = P.sb("A_krsq", [64, 512])
    Rt = P.sb("A_Rt", [64, 512])
    sqn2 = [P.sb("A_sqn%d" % i, [128, 512]) for i in range(2)]
    sqr2 = [P.sb("A_sqr%d" % i, [64, 512]) for i in range(2)]
    rsh2 = [P.sb("A_rsh%d" % i, [128, 512]) for i in range(2)]
    t64a = P.sb("A_t64a", [64, 512])
    t64b = P.sb("A_t64b", [64, 512])
    ob64 = [P.sb("A_ob64_%d" % i, [64, 512], BF16) for i in range(2)]
    vb = [P.sb("A_vb%d" % i, [128, 1024], BF16) for i in range(2)]
    xrT, ggT, qn_o, qr_o, kn_o, kr_o, v_o = G["xrT"], G["ggT"], G["qn"], G["qr"], G["kn"], G["kr"], G["v"]

    def rope_mix(out_bf, a_f, b_f, n, kind):
        if kind == 0:
            P.tt(a_f, a_f, rc_t[:, :n], ALU.mult)
            P.tt(b_f, b_f, rs_t[:, :n], ALU.mult)
            P.tt(out_bf, a_f, b_f, ALU.add)
        else:
            P.copy(out_bf, a_f)

    groups = chunks(W_IN_EXT)
    it = 0
    for gi, (g0, gn) in enumerate(groups):
        w_ = wg[gi % 2]
        P.dma(w_[:, :, :gn], wv[:, :, g0:g0 + gn], q="pool")
        for (c0, n, kind) in tch:
            h_ = hmc[it % 3]
            it += 1
            P.dma(h_[:, :, :n], hm_d[:, :, c0:c0 + n], rd=[(hm_d, c0)], q="pool")
            if kind == 0 and g0 >= 2048:
                P.dma(rc_t[:, :n], G["ropec"][:, c0:c0 + n], q="pool")
                P.dma(rs_t[:, :n], G["ropes"][:, c0:c0 + n], q="pool")
            nm = (gn + 127) // 128
            for mi in range(nm):
                col = g0 + mi * 128
                mw = min(128, gn - mi * 128)
                p_ = pb[1 + (mi % 4)]
                if col < 2816:
                    for kc in range(KC):
                        P.mm(p_[:mw, :n], w_[:, kc, mi * 128: mi * 128 + mw], h_[:, kc, :n], kc == 0, kc == KC - 1)
                m = col // 128
                if m < 8:
                    e_ = ev[m % 2]
                    P.act(e_[:, :n], p_[:, :n], AF.Copy)
                    P.dma(xrT[m * 128:(m + 1) * 128, c0:c0 + n], e_[:, :n])
                elif m < 16:
                    e_ = evb[m % 2]
                    P.act(e_[:, :n], p_[:, :n], AF.Gelu_apprx_tanh)
                    P.dma(ggT[(m - 8) * 128:(m - 7) * 128, c0:c0 + n], e_[:, :n])
                elif m < 20:
                    P.copy(cqt[:, m - 16, :n], p_[:, :n])
                elif m < 22:
                    P.copy(ckt[:, m - 20, :n], p_[:, :n])
                else:
                    for kc in range(KC):
                        P.mm(pb[5][:64, :n], w_[:, kc, mi * 128: mi * 128 + 64], h_[:, kc, :n], kc == 0, kc == KC - 1)
                    for kc in range(KC):
                        P.mm(pb[6][:64, :n], w_[:, kc, mi * 128 + 64: mi * 128 + 128], h_[:, kc, :n], kc == 0, kc == KC - 1)
                    P.copy(krt[:, :n], pb[5][:64, :n])
                    P.copy(kst[:, :n], pb[6][:64, :n])
            if g0 == 2048:
                for kc in range(4):
                    s_ = sq[kc % 2]
                    P.act(s_[:, :n], cqt[:, kc, :n], AF.Square)
                    P.mm(pb[0][:, :n], ones[:], s_[:, :n], kc == 0, kc == 3)
                P.rstd(rs[:, :n], pb[0][:, :n], 1.0 / 512, epsb[:])
                for kc in range(4):
                    P.stt(cqn[:, kc, :n], cqt[:, kc, :n], C("gqa", kc), rs[:, :n], ALU.mult, ALU.mult)
                def qproj(h):
                    pn, pr, pS = pb[1 + (h % 2) * 3], pb[2 + (h % 2) * 3], pb[3 + (h % 2) * 3]
                    b0 = h * 256
                    for kc in range(4):
                        P.mm(pn[:, :n], wuq[:, kc, b0:b0 + 128], cqn[:, kc, :n], kc == 0, kc == 3)
                    for kc in range(4):
                        P.mm(pr[:64, :n], wuq[:, kc, b0 + 128:b0 + 192], cqn[:, kc, :n], kc == 0, kc == 3)
                    for kc in range(4):
                        P.mm(pS[:64, :n], wuq[:, kc, b0 + 192:b0 + 256], cqn[:, kc, :n], kc == 0, kc == 3)

                qproj(0)
                for h in range(NH):
                    pn, pr, pS = pb[1 + (h % 2) * 3], pb[2 + (h % 2) * 3], pb[3 + (h % 2) * 3]
                    sqn_, sqr_, rsh_ = sqn2[h % 2], sqr2[h % 2], rsh2[h % 2]
                    P.act(sqn_[:, :n], pn[:, :n], AF.Square)
                    P.act(sqr_[:, :n], pr[:64, :n], AF.Square)
                    if h + 1 < NH:
                        qproj(h + 1)
                    P.mm(pb[7][:, :n], ones[:], sqn_[:, :n], True, False)
                    P.mm(pb[7][:, :n], ones[:64, :], sqr_[:, :n], False, True)
                    P.rstd(rsh_[:, :n], pb[7][:, :n], 1.0 / 192, epsb[:])
                    o_ = evb[h % 2]
                    P.stt(o_[:, :n], pn[:, :n], C("gq_n"), rsh_[:, :n], ALU.mult, ALU.mult)
                    P.dma(qn_o[h, :, c0:c0 + n], o_[:, :n])
                    P.stt(t64a[:, :n], pr[:64, :n], cvt[:64, CV_A["gq_r"]:CV_A["gq_r"] + 1], rsh_[:64, :n], ALU.mult, ALU.mult)
                    P.stt(t64b[:, :n], pS[:64, :n], cvt[:64, CV_A["gq_s"]:CV_A["gq_s"] + 1], rsh_[:64, :n], ALU.mult, ALU.mult)
                    o6 = ob64[h % 2]
                    rope_mix(o6[:, :n], t64a[:, :n], t64b[:, :n], n, kind)
                    P.dma(qr_o[h, :, c0:c0 + n], o6[:, :n])
            if g0 == 2560:
                for kc in range(2):
                    s_ = sq[kc % 2]
                    P.act(s_[:, :n], ckt[:, kc, :n], AF.Square)
                    P.mm(pb[0][:, :n], ones[:], s_[:, :n], kc == 0, kc == 1)
                P.rstd(rs[:, :n], pb[0][:, :n], 1.0 / 256, epsb[:])
                for kc in range(2):
                    P.stt(ckn[:, kc, :n], ckt[:, kc, :n], C("gkva", kc), rs[:, :n], ALU.mult, ALU.mult)
                P.act(krsq[:, :n], krt[:, :n], AF.Square)
                P.ts(t64a[:, :n], krt[:, :n], cvt[:64, CV_A["gk_r"]:CV_A["gk_r"] + 1], None, ALU.mult)
                P.ts(t64b[:, :n], kst[:, :n], cvt[:64, CV_A["gk_s"]:CV_A["gk_s"] + 1], None, ALU.mult)
                if kind == 0:
                    P.tt(t64a[:, :n], t64a[:, :n], rc_t[:, :n], ALU.mult)
                    P.tt(t64b[:, :n], t64b[:, :n], rs_t[:, :n], ALU.mult)
                    P.tt(Rt[:, :n], t64a[:, :n], t64b[:, :n], ALU.add)
                else:
                    P.copy(Rt[:, :n], t64a[:, :n])
                def kproj(h):
                    pn = pb[1 + (h % 2)]
                    for kc in range(2):
                        P.mm(pn[:, :n], wuk[:, kc, h * 128:(h + 1) * 128], ckn[:, kc, :n], kc == 0, kc == 1)

                kproj(0)
                for h in range(NH):
                    pn = pb[1 + (h % 2)]
                    sqn_, rsh_ = sqn2[h % 2], rsh2[h % 2]
                    P.act(sqn_[:, :n], pn[:, :n], AF.Square)
                    if h + 1 < NH:
                        kproj(h + 1)
                    P.mm(pb[7][:, :n], ones[:], sqn_[:, :n], True, False)
                    P.mm(pb[7][:, :n], ones[:64, :], krsq[:, :n], False, True)
                    P.rstd(rsh_[:, :n], pb[7][:, :n], 1.0 / 192, epsb[:])
                    o_ = evb[h % 2]
                    P.stt(o_[:, :n], pn[:, :n], C("gk_n"), rsh_[:, :n], ALU.mult, ALU.mult)
                    P.dma(kn_o[h, :, c0:c0 + n], o_[:, :n])
                    o6 = ob64[h % 2]
                    P.tt(o6[:, :n], Rt[:, :n], rsh_[:64, :n], ALU.mult)
                    P.dma(kr_o[h, :, c0:c0 + n], o6[:, :n])
                for j in range(n // 128):
                    vt = vb[j % 2]
                    for hh in range(2):
                        pv = pb[3 + hh]
                        for kc in range(2):
                            P.mm(pv[:, :], ckn[:, kc, j * 128:(j + 1) * 128], wuv[:, kc, hh * 512:(hh + 1) * 512], kc == 0, kc == 1)
                        P.act(vt[:, hh * 512:(hh + 1) * 512], pv[:, :], AF.Copy)
                    P.dma(v_o[c0 + j * 128: c0 + (j + 1) * 128, :], vt[:])
    P.phase_end(mk)


def emit_A2(P, pb, G, l, T, NCX):
    mk = P.phase_begin()
    xr, gg, yr = G["xrT"], G["ggT"], G["yr"]
    cvt = P.sb("L_cvt", [128, 88])
    P.dma(cvt[:], G["cvl"][l])
    one = P.sb("L_one", [128, 1])
    P.memset(one[:], 1.0)
    cl = P.sb("L_cl", [128, 16])
    P.act(cl[:], cvt[:, 72:88], AF.Exp, scale=-1.0)
    P.act(cl[:], cl[:], AF.Ln, bias=one[:])
    P.ts(cl[:], cl[:], -8.0, None, ALU.mult)
    wab = P.sb("L_wab", [128, 2, 8, 128], BF16)
    wxb = P.sb("L_wxb", [128, 2, 8, 128], BF16)
    for d in range(2):
        P.dma(wab[:, d, :, :], G["lru_wa"][l, d].rearrange("g i j -> i g j"), q="pool")
        P.dma(wxb[:, d, :, :], G["lru_wx"][l, d].rearrange("g i j -> i g j"), q="pool")
    streams = [("C", T, NCX), ("L", 0, T)]
    tl = {}
    for s, _, n in streams:
        X = P.sb("L_X" + s, [128, n + 3])
        tl[s] = dict(X=X, u=P.sb("L_u" + s, [128, n]), ub=P.sb("L_ub" + s, [128, n], BF16),
                     ra=[P.sb("L_ra%d" % d + s, [128, n]) for d in range(2)],
                     ii=[P.sb("L_ii%d" % d + s, [128, n]) for d in range(2)],
                     sb=[P.sb("L_sb%d" % d + s, [128, n]) for d in range(2)],
                     hf=P.sb("L_hf" + s, [128, n]),
                     gg=P.sb("L_gg" + s, [128, n], BF16), yo=P.sb("L_yo" + s, [128, n], BF16))
    for g in range(8):
        rows = slice(g * 128, (g + 1) * 128)
        for s, s0, n in streams:
            t = tl[s]
            P.memset(t["X"][:, 0:2], 0.0)
            P.memset(t["X"][:, n + 2:n + 3], 0.0)
            P.dma(t["X"][:, 2:n + 2], xr[rows, s0:s0 + n])
            P.dma(t["gg"][:], gg[rows, s0:s0 + n])
            P.ts(t["u"][:], t["X"][:, 0:n], cvt[:, g * 4:g * 4 + 1], cvt[:, 32 + g:33 + g], ALU.mult, ALU.add)
            for k in range(1, 4):
                P.stt(t["u"][:], t["X"][:, k:k + n], cvt[:, g * 4 + k:g * 4 + k + 1], t["u"][:], ALU.mult, ALU.add)
            P.act(t["ub"][:], t["u"][:], AF.Copy)
        for d in range(2):
            for s, s0, n in streams:
                t = tl[s]
                ra, ii, sb = t["ra"][d], t["ii"][d], t["sb"][d]
                for ci, (c0, cn) in enumerate(chunks(n)):
                    pr, pi = pb[(ci % 2) * 2], pb[(ci % 2) * 2 + 1]
                    P.mm(pr[:, :cn], wab[:, d, g, :], t["ub"][:, c0:c0 + cn], True, True)
                    P.mm(pi[:, :cn], wxb[:, d, g, :], t["ub"][:, c0:c0 + cn], True, True)
                    P.act(ra[:, c0:c0 + cn], pr[:, :cn], AF.Sigmoid, bias=cvt[:, 40 + d * 8 + g:41 + d * 8 + g])
                    P.act(ii[:, c0:c0 + cn], pi[:, :cn], AF.Sigmoid, bias=cvt[:, 56 + d * 8 + g:57 + d * 8 + g])
                P.act(ra[:], ra[:], AF.Exp, scale=cl[:, d * 8 + g:d * 8 + g + 1])
                P.act(sb[:], ra[:], AF.Square)
                P.act(sb[:], sb[:], AF.Sqrt, bias=one[:], scale=-1.0)
        for d in range(2):
            for s, s0, n in streams:
                t = tl[s]
                ra, ii, sb = t["ra"][d], t["ii"][d], t["sb"][d]
                P.tt(ii[:], ii[:], t["u"][:], ALU.mult)
                P.tt(sb[:], sb[:], ii[:], ALU.mult)
                if d == 0:
                    init = 0.0 if s == "C" else tl["C"]["hf"][:, NCX - 1:NCX]
                    P.scan(t["hf"][:], ra[:], sb[:], init, ALU.mult, ALU.add)
                else:
                    hb = t["X"][:, 0:n]
                    init = 0.0 if s == "C" else tl["C"]["X"][:, 0:1]
                    P.scan(hb[:, ::-1], ra[:, ::-1], sb[:, ::-1], init, ALU.mult, ALU.add)
        for s, s0, n in streams:
            t = tl[s]
            P.tt(t["hf"][:], t["hf"][:], t["X"][:, 0:n], ALU.add)
            P.tt(t["yo"][:], t["hf"][:], t["gg"][:], ALU.mult)
            P.dma(yr[rows, s0:s0 + n], t["yo"][:])
    P.phase_end(mk)


def emit_B(P, pb, G, l, xT, T, NCX, do_ctx):
    NT = T + NCX
    NKT = NT // 128
    NI = T // 128
    modfm = G["modfm"]
    aff_sb = G["aff_sb"]
    HQ = min(2048, T)
    for qp in range(T // HQ):
        mk = P.phase_begin()
        qch = [(qp * HQ + c0, n, 0, c0) for c0, n in chunks(HQ)]
        NQP = HQ
        if do_ctx and qp == 0:
            qch += [(T + c0, n, 1, HQ + c0) for c0, n in chunks(NCX)]
            NQP = HQ + NCX
        cvt = P.sb("B_cvt", [128, CV_B["n"]])
        P.dma(cvt[:], G["cvB"][l])
        P.copy(cvt[:, 0:16], modfm[:, l, 32:48, 0])
        P.copy(cvt[:, 16:32], modfm[:, l, 32:48, 1])
        P.copy(cvt[:, 48:64], modfm[:, l, 48:64, 0])
        P.copy(cvt[:, 64:80], modfm[:, l, 64:80, 0])
        P.copy(cvt[:, 80:96], modfm[:, l, 48:64, 1])
        P.copy(cvt[:, 96:112], modfm[:, l, 64:80, 1])
        C = lambda name, j=0, w=1: cvt[:, CV_B[name] + j: CV_B[name] + j + w]
        epsb = P.sb("B_epsb", [128, 1])
        P.memset(epsb[:], EPS)
        ones = P.sb("B_ones", [128, 128])
        P.memset(ones[:], 1.0)
        onesb = P.sb("B_onesb", [128, 128], BF16)
        P.memset(onesb[:], 1.0)
        ident = P.sb("B_ident", [128, 128])
        P.dma(ident[:], G["ident"])
        A2m = P.sb("B_A2m", [128, 2, KC])
        for k, nm in enumerate(("sc2_l", "sc2_c")):
            P.ts(A2m[:, k, :], C(nm, 0, KC), 1.0, None, ALU.add)
            P.tt(A2m[:, k, :], A2m[:, k, :], C("n2", 0, KC), ALU.mult)
        rt = P.sb("B_rt", [128, KC, NE])
        P.dma(rt[:], G["router"][l].rearrange("(kc p) e -> p kc e", p=128))

        yatt = P.sb("B_yatt", [128, 8, NQP], BF16)
        yrv = G["yr"].rearrange("(c p) n -> p c n", p=128)
        kvq = [dict(knt=P.sb("B_knt%d" % i, [128, NT], BF16), krt=P.sb("B_krt%d" % i, [64, NT], BF16),
                    vt=P.sb("B_vt%d" % i, [128, NKT, 128], BF16), qnt=P.sb("B_qnt%d" % i, [128, NQP], BF16),
                    qrt=P.sb("B_qrt%d" % i, [64, NQP], BF16)) for i in range(2)]
        pt = [P.sb("B_pt%d" % i, [128, 512], BF16) for i in range(3)]
        rec = P.sb("B_rec", [128, 512])
        it = 0
        for h in range(NH):
            kq = kvq[h % 2]
            knt, krt, vt, qnt, qrt = kq["knt"], kq["krt"], kq["vt"], kq["qnt"], kq["qrt"]
            P.dma(knt[:], G["kn"][h])
            P.dma(krt[:], G["kr"][h])
            P.dma(vt[:], G["v"][:, h * 128:(h + 1) * 128].rearrange("(j p) d -> p j d", p=128))
            P.dma(qnt[:, 0:HQ], G["qn"][h, :, qp * HQ:(qp + 1) * HQ])
            P.dma(qrt[:, 0:HQ], G["qr"][h, :, qp * HQ:(qp + 1) * HQ])
            if NQP > HQ:
                P.dma(qnt[:, HQ:NQP], G["qn"][h, :, T:NT])
                P.dma(qrt[:, HQ:NQP], G["qr"][h, :, T:NT])
            for (ca, n, kind, c0) in qch:
                kts = list(range(NKT)) if kind == 0 else list(range(NI, NKT))
                SB = (pb[0], pb[1], pb[4], pb[5])

                def qk(ji):
                    S = SB[ji % 4]
                    j = kts[ji]
                    P.mm(S[:, :n], knt[:, j * 128:(j + 1) * 128], qnt[:, c0:c0 + n], True, False)
                    P.mm(S[:, :n], krt[:, j * 128:(j + 1) * 128], qrt[:, c0:c0 + n], False, True)

                for ji in range(min(2, len(kts))):
                    qk(ji)
                for ji, j in enumerate(kts):
                    if ji + 2 < len(kts):
                        qk(ji + 2)
                    p_ = pt[ji % 3]
                    P.act(p_[:, :n], SB[ji % 4][:, :n], AF.Exp, scale=ATT_SCALE)
                    P.mm(pb[2][:, :n], vt[:, j, :], p_[:, :n], ji == 0, ji == len(kts) - 1)
                    P.mm(pb[3][:, :n], onesb[:], p_[:, :n], ji == 0, ji == len(kts) - 1)
                P.recip(rec[:, :n], pb[3][:, :n])
                P.tt(yatt[:, h, c0:c0 + n], pb[2][:, :n], rec[:, :n], ALU.mult)

        xc = P.sb("B_xc", [128, KC, 512])
        yrc = P.sb("B_yrc", [128, 8, 512], BF16)
        wob = [P.sb("B_wob%d" % i, [128, KC, 256], BF16) for i in range(2)]
        sq = [P.sb("B_sq%d" % i, [128, 512]) for i in range(2)]
        htm = [P.sb("B_htm%d" % i, [128, D], BF16) for i in range(4)]
        rs = P.sb("B_rs", [128, 512])
        mx = P.sb("B_mx", [128, 1])
        sm = P.sb("B_sm", [128, 1])
        ex = P.sb("B_ex", [128, NE])
        xv = xT.rearrange("(kc p) n -> p kc n", p=128)
        xnv = G["xn"].rearrange("(kc p) n -> p kc n", p=128)
        wov = G["w_out"][l].rearrange("(kc p) n -> p kc n", p=128)
        it = 0
        for (ca, n, kind, c0) in qch:
            nj = n // 128
            P.dma(xc[:, :, :n], xv[:, :, ca:ca + n], q="pool")
            P.dma(yrc[:, :, :n], yrv[:, :, ca:ca + n], q="pool")
            g1n = "g1_l" if kind == 0 else "g1_c"
            shn = "sh2_l" if kind == 0 else "sh2_c"
            for mg in range(8):
                wo = wob[it % 2]
                it += 1
                P.dma(wo[:], wov[:, :, mg * 256:(mg + 1) * 256], q="pool")
                for mi in range(2):
                    m = mg * 2 + mi
                    p_ = pb[m % 2]
                    for kc in range(KC):
                        rhs_ = yrc[:, kc, :n] if kc < 8 else yatt[:, kc - 8, c0:c0 + n]
                        P.mm(p_[:, :n], wo[:, kc, mi * 128:(mi + 1) * 128], rhs_, kc == 0, kc == KC - 1)
                    P.stt(xc[:, m, :n], p_[:, :n], C(g1n, m), xc[:, m, :n], ALU.mult, ALU.add)
            P.dma(xnv[:, :, ca:ca + n], xc[:, :, :n])
            for m in range(KC):
                P.act(sq[m % 2][:, :n], xc[:, m, :n], AF.Square)
                P.mm(pb[2][:, :n], ones[:], sq[m % 2][:, :n], m == 0, m == KC - 1)
            P.rstd(rs[:, :n], pb[2][:, :n], 1.0 / D, epsb[:])
            for m in range(KC):
                P.tt(xc[:, m, :n], xc[:, m, :n], rs[:, :n], ALU.mult)
                P.ts(xc[:, m, :n], xc[:, m, :n], A2m[:, kind, m:m + 1], C(shn, m), ALU.mult, ALU.add)
                for j in range(nj):
                    P.emit("pe", lambda en, j=j, m=m: en.transpose(pb[4 + j][:, (m % 4) * 128:(m % 4 + 1) * 128],
                                                                 xc[:, m, j * 128:(j + 1) * 128], ident[:]),
                           [xc, ident], [pb[4 + j]])
                if m % 4 == 3:
                    for j in range(nj):
                        dst = htm[j][:, (m // 4) * 512:(m // 4 + 1) * 512]
                        if j % 2 == 0:
                            P.copy(dst, pb[4 + j][:, :])
                        else:
                            P.act(dst, pb[4 + j][:, :], AF.Copy)
            for j in range(nj):
                P.dma(G["h2tm"][ca + j * 128:ca + (j + 1) * 128, :], htm[j][:])
                for m in range(KC):
                    P.mm(pb[j][:, :NE], xc[:, m, j * 128:(j + 1) * 128], rt[:, m, :], m == 0, m == KC - 1)
                lg = pb[j][:, :NE]
                ti = (ca // 128) + j
                P.emit("dve", lambda en, lg=lg: en.tensor_reduce(mx[:], lg, AX.X, ALU.max), [lg], [mx])
                P.ts(mx[:], mx[:], -1.0, None, ALU.mult)
                P.act(ex[:], lg, AF.Exp, bias=mx[:])
                P.emit("dve", lambda en: en.tensor_reduce(sm[:], ex[:], AX.X, ALU.add), [ex], [sm])
                P.recip(sm[:], sm[:])
                P.ts(aff_sb[:, :, ti], ex[:], sm[:, 0:1], None, ALU.mult)
        P.phase_end(mk)


def emit_D(P, pb, G, l, T, NCX, do_ctx):
    mk = P.phase_begin()
    NI, NIC = T // 128, NCX // 128
    cap, capc = 2 * T // NE, 2 * NCX // NE
    NU = NE
    aff_sb = G["aff_sb"]
    ones = P.sb("D_ones", [128, 128])
    P.memset(ones[:], 1.0)
    onesb = P.sb("D_onesb", [128, 128], BF16)
    P.memset(onesb[:], 1.0)
    utf = P.sb("D_utf", [128, 128])
    P.dma(utf[:], G["ut"])
    utb = P.sb("D_utb", [128, 128], BF16)
    P.copy(utb[:], utf[:])
    iott = P.sb("D_iott", [128, 512])
    P.dma(iott[:], G["iot"])
    zer = P.sb("D_zer", [128, 64])
    P.memset(zer[:], 0.0)

    streams = [("L", aff_sb[:, :, 0:NI], G["keyL"], NI, cap)]
    if do_ctx:
        streams.append(("C", aff_sb[:, :, NI:NI + NIC], G["keyC"], NIC, capc))
    NS = len(streams)
    lo = P.sb("D_lo", [128, NS, NU])
    mid = P.sb("D_mid", [128, NS, NU])
    cnt = P.sb("D_cnt", [128, NS, NU])
    capt = P.sb("D_capt", [128, NS, NU])
    ge = P.sb("D_ge", [128, NS, NU], I32)
    cms = [P.sb("D_cm" + tg, [128, NU, ni]) for (tg, _, _, ni, _) in streams]
    P.memset(lo[:], 0.0)
    for si, (_, _, _, _, cv_) in enumerate(streams):
        P.memset(capt[:, si, :], float(cv_))
    lof = lo[:].rearrange("p s u -> p (s u)")
    midf = mid[:].rearrange("p s u -> p (s u)")
    for k in range(N_BISECT):
        P.ts(midf, lof, float(2.0 ** -(k + 1)), None, ALU.add)
        for si, (_, af, _, ni, _) in enumerate(streams):
            P.tt(cms[si][:], af, mid[:, si, :].unsqueeze(2).to_broadcast([128, NU, ni]), ALU.is_ge)
            P.emit("dve", lambda en, si=si: en.tensor_reduce(cnt[:, si, :], cms[si][:], AX.X, ALU.add), [cms[si]], [cnt])
        P.mm(pb[0][:, :NS * NU], ones[:], cnt[:].rearrange("p s u -> p (s u)"), True, True)
        P.tt(ge[:].rearrange("p s u -> p (s u)"), pb[0][:, :NS * NU], capt[:].rearrange("p s u -> p (s u)"), ALU.is_ge)
        P.emit("dve", lambda en: en.copy_predicated(lof, ge[:].rearrange("p s u -> p (s u)"), midf), [ge, mid, lo], [lo])
    for si, (tag, af, pos, ni, _) in enumerate(streams):
        n3 = [128, NU, ni]
        cm = cms[si]
        P.tt(cm[:], af, lo[:, si, :].unsqueeze(2).to_broadcast(n3), ALU.is_ge)
        mb = P.sb("D_mb" + tag, [128, NU * ni], BF16)
        P.copy(mb[:], cm[:].rearrange("p u i -> p (u i)"))
        tot = P.sb("D_tot" + tag, n3)
        inc = P.sb("D_inc" + tag, n3)
        wit = P.sb("D_wit" + tag, n3)
        for c0, cn in chunks(NU * ni):
            P.mm(pb[1][:, :cn], utb[:], mb[:, c0:c0 + cn], True, True)
            P.mm(pb[2][:, :cn], onesb[:], mb[:, c0:c0 + cn], True, True)
            P.copy(wit[:].rearrange("p u i -> p (u i)")[:, c0:c0 + cn], pb[1][:, :cn])
            P.copy(tot[:].rearrange("p u i -> p (u i)")[:, c0:c0 + cn], pb[2][:, :cn])
        for u in range(NU):
            P.scan(inc[:, u, :], tot[:, u, :], zer[:, :ni], 0.0, ALU.add, ALU.add)
        P.tt(inc[:], inc[:], tot[:], ALU.subtract)
        P.tt(pos[:], inc[:], wit[:], ALU.add)
        P.tt(pos[:], pos[:], cm[:], ALU.mult)
        P.ts(pos[:], pos[:], -1.0, None, ALU.add)
    keyL, keyC = G["keyL"], G["keyC"]
    NT = T + NCX
    for e in range(NE):
        P.dma(G["keyD"][e, 0:T].rearrange("(i p) -> p i", p=128), keyL[:, e, :], slow=True, q="sp" if e % 2 == 0 else "pool")
        P.dma(G["gateD"][e, :].rearrange("(i p) -> p i", p=128), aff_sb[:, e, :], slow=True, q="pool" if e % 2 == 0 else "sp")
        if do_ctx:
            P.dma(G["keyD"][e, T:NT].rearrange("(i p) -> p i", p=128), keyC[:, e, :], slow=True)

    xg = P.sb("D_xg", [128, KC, cap], BF16)
    at = P.sb("D_at", [128, KC, cap], BF16)
    if do_ctx:
        xgc = P.sb("D_xgc", [128, KC, capc], BF16)
        atc = P.sb("D_atc", [128, KC, capc], BF16)
    selr = [P.sb("D_sel%d" % i, [128, 512], BF16) for i in range(3)]
    h2t = [P.sb("D_h2t%d" % i, [128, 1024], BF16) for i in range(3)]
    WG = 512
    wbuf = [P.sb("D_wbuf%d" % i, [128, KC, WG], BF16) for i in range(4)]
    sg = [P.sb("D_sg%d" % i, [128, 512]) for i in range(2)]
    yo = [P.sb("D_yo%d" % i, [128, 512], BF16) for i in range(2)]
    cnt_it = [0, 0, 0]
    h2tm = G["h2tm"]

    def gather(row0, key_t, u, ni, capv, dst):
        for half in range(2):
            for i in range(ni):
                s_ = selr[cnt_it[0] % 3]
                h_ = h2t[cnt_it[0] % 3]
                cnt_it[0] += 1
                P.ts(s_[:, :capv], iott[:, :capv], key_t[:, u, i:i + 1], None, ALU.is_equal)
                P.dma(h_[:], h2tm[row0 + i * 128:row0 + (i + 1) * 128, half * 1024:(half + 1) * 1024])
                for dc in range(8):
                    P.mm(pb[dc][:, :capv], h_[:, dc * 128:(dc + 1) * 128], s_[:, :capv], i == 0, i == ni - 1)
            for dc in range(8):
                if dc % 2 == 0:
                    P.copy(dst[:, half * 8 + dc, :capv], pb[dc][:, :capv])
                else:
                    P.act(dst[:, half * 8 + dc, :capv], pb[dc][:, :capv], AF.Copy)

    for e in range(NE):
        gather(0, keyL, e, NI, cap, xg)
        cks = [(xg, at, cap, G["ysl"])]
        if do_ctx:
            gather(T, keyC, e, NIC, capc, xgc)
            cks.append((xgc, atc, capc, G["ycsl"]))
        wgv = G["w_gate"][l, e].rearrange("(kc p) n -> p kc n", p=128)
        wuv = G["w_up"][l, e].rearrange("(kc p) n -> p kc n", p=128)
        wdv = G["w_down"][l, e].rearrange("(kc p) n -> p kc n", p=128)
        for fg in range(D // WG):
            wg_t = wbuf[(cnt_it[1] * 2) % 4]
            wu_t = wbuf[(cnt_it[1] * 2 + 1) % 4]
            cnt_it[1] += 1
            P.dma(wg_t[:], wgv[:, :, fg * WG:(fg + 1) * WG], q="pool")
            P.dma(wu_t[:], wuv[:, :, fg * WG:(fg + 1) * WG], q="pool")
            for (xs, as_, n, _) in cks:
                for fi in range(WG // 128):
                    f = fg * (WG // 128) + fi
                    pg, pu = pb[(f % 2) * 2], pb[(f % 2) * 2 + 1]
                    for kc in range(KC):
                        P.mm(pg[:, :n], wg_t[:, kc, fi * 128:(fi + 1) * 128], xs[:, kc, :n], kc == 0, kc == KC - 1)
                    for kc in range(KC):
                        P.mm(pu[:, :n], wu_t[:, kc, fi * 128:(fi + 1) * 128], xs[:, kc, :n], kc == 0, kc == KC - 1)
                    s_ = sg[f % 2]
                    P.act(s_[:, :n], pg[:, :n], AF.Silu)
                    P.tt(as_[:, f, :n], s_[:, :n], pu[:, :n], ALU.mult)
        for dg in range(D // WG):
            wd_t = wbuf[cnt_it[2] % 4]
            cnt_it[2] += 1
            P.dma(wd_t[:], wdv[:, :, dg * WG:(dg + 1) * WG], q="pool")
            for (xs, as_, n, ydst) in cks:
                for st in range((n + 127) // 128):
                    sw = min(128, n - st * 128)
                    py = pb[4 + (st % 2)]
                    for fc in range(KC):
                        P.mm(py[:sw, :WG], as_[:, fc, st * 128:st * 128 + sw], wd_t[:, fc, :], fc == 0, fc == KC - 1)
                    y_ = yo[st % 2]
                    P.act(y_[:sw, :WG], py[:sw, :WG], AF.Copy)
                    P.dma(ydst[e, st * 128:st * 128 + sw, dg * WG:(dg + 1) * WG], y_[:sw, :WG])
    P.phase_end(mk)


def emit_E(P, pb, G, l, T, NCX, do_ctx, xo):
    mk = P.phase_begin()
    cap, capc = 2 * T // NE, 2 * NCX // NE
    KL = min(128, cap)
    S = cap // KL
    modfm = G["modfm"]
    sidt = P.sb("E_sid", [128, 4])
    P.dma(sidt[:], G["sid"])
    Yt = P.sb("E_Yt", [KL, NE, S, 1024], BF16)
    if do_ctx:
        Yc = P.sb("E_Yc", [capc, NE, 1024], BF16)
    kgb = [P.sb("E_kgb%d" % i, [128, 2, 512]) for i in range(2)]
    selT = [P.sb("E_selT%d" % i, [128, 512], BF16) for i in range(3)]
    xs = [P.sb("E_xs%d" % i, [128, 512]) for i in range(2)]
    ot = [P.sb("E_ot%d" % i, [128, 512]) for i in range(2)]
    qch = [(c0, n, 0) for c0, n in chunks(T)]
    if do_ctx:
        qch += [(T + c0, n, 1) for c0, n in chunks(NCX)]
    it = 0
    for half in range(2):
        hs = slice(half * 1024, (half + 1) * 1024)
        for e in range(NE):
            P.dma(Yt[:, e, :, :], G["ysl"][e].rearrange("(s p) d -> p s d", p=KL)[:, :, hs], q="sp" if e % 2 == 0 else "pool")
        if do_ctx:
            P.dma(Yc[:], G["ycsl"].rearrange("e s d -> s e d")[:, :, hs])
        for (c0, n, kind) in qch:
            for e in range(NE):
                k_ = kgb[e % 2]
                ns, K = (S, KL) if kind == 0 else (1, capc)
                P.dma(k_[:, 0, :n], G["keyD"][e, c0:c0 + n].partition_broadcast(128))
                P.dma(k_[:, 1, :n], G["gateD"][e, c0:c0 + n].partition_broadcast(128), q="pool")
                for s in range(ns):
                    st = selT[it % 3]
                    it += 1
                    P.stt(st[:K, :n], k_[:K, 0, :n], sidt[:K, s:s + 1], k_[:K, 1, :n], ALU.is_equal, ALU.mult)
                    first = (e == 0 and s == 0)
                    last = (e == NE - 1 and s == ns - 1)
                    for dc in range(8):
                        lhs = Yt[:KL, e, s, dc * 128:(dc + 1) * 128] if kind == 0 else Yc[:, e, dc * 128:(dc + 1) * 128]
                        P.mm(pb[dc][:, :n], lhs, st[:K, :n], first, last)
            for dc in range(8):
                m = half * 8 + dc
                x_ = xs[dc % 2]
                o_ = ot[dc % 2]
                P.dma(x_[:, :n], G["xn"][m * 128:(m + 1) * 128, c0:c0 + n], q="pool")
                P.stt(o_[:, :n], pb[dc][:, :n], modfm[:, l, 80 + m, kind:kind + 1], x_[:, :n], ALU.mult, ALU.add)
                P.dma(xo[m * 128:(m + 1) * 128, c0:c0 + n], o_[:, :n])
    P.phase_end(mk)


_PROG_CACHE = {}


def build_fused(T, NCX):
    NT = T + NCX
    NI, NIC = T // 128, NCX // 128
    cap, capc = 2 * T // NE, 2 * NCX // NE
    P = Prog()
    G = {}
    f32in = dict(xT0=[D, NT], sT=[128, KC, 2], bmod=[128, 2, 96], w_mod=[2, D, 6 * D], cvA=[2, 128, CV_A["n"]],
                 w_in=[2, D, W_IN_EXT], w_uq=[2, 512, 2048], w_uk=[2, 256, 1024], w_uv=[2, 256, 1024],
                 ropec=[64, T], ropes=[64, T], cvl=[2, 128, 88], lru_wa=[2, 2, 8, 128, 128], lru_wx=[2, 2, 8, 128, 128],
                 cvB=[2, 128, CV_B["n"]], w_out=[2, D, D], router=[2, D, NE], ident=[128, 128], ut=[128, 128],
                 iot=[128, 512], sid=[128, 4], w_gate=[2, NE, D, D], w_up=[2, NE, D, D], w_down=[2, NE, D, D])
    for k, shp in f32in.items():
        G[k] = P.din(k, shp)
    out = P.dout("out", [D, T])
    G["hm_d"] = P.dscr("hm_d", [128, KC, NT], BF16, track=True)
    G["xrT"] = P.dscr("xrT", [1024, NT])
    G["ggT"] = P.dscr("ggT", [1024, NT], BF16)
    G["qn"] = P.dscr("qn", [NH, 128, NT], BF16)
    G["qr"] = P.dscr("qr", [NH, 64, NT], BF16)
    G["kn"] = P.dscr("kn", [NH, 128, NT], BF16)
    G["kr"] = P.dscr("kr", [NH, 64, NT], BF16)
    G["v"] = P.dscr("v", [NT, 1024], BF16)
    G["yr"] = P.dscr("yr", [1024, NT], BF16)
    G["xn"] = P.dscr("xn", [D, NT])
    G["h2tm"] = P.dscr("h2tm", [NT, D], BF16)
    G["ysl"] = P.dscr("ysl", [NE, cap, D], BF16)
    G["ycsl"] = P.dscr("ycsl", [NE, capc, D], BF16)
    G["keyD"] = P.dscr("keyD", [NE, NT])
    G["gateD"] = P.dscr("gateD", [NE, NT])
    x1 = P.dscr("x1", [D, NT])
    pb = [P.ps("pb%d" % i, [128, 512]) for i in range(8)]
    G["modfm"] = P.sb("modfm", [128, 2, 96, 2])
    G["aff_sb"] = P.sb("aff_sb", [128, NE, NI + NIC])
    G["keyL"] = P.sb("keyL", [128, NE, NI])
    G["keyC"] = P.sb("keyC", [128, NE, NIC])
    emit_M(P, pb, G)
    xT = G["xT0"]
    for l in range(2):
        do_ctx = (l == 0)
        emit_A(P, pb, G, l, xT, T, NCX)
        emit_A2(P, pb, G, l, T, NCX)
        emit_B(P, pb, G, l, xT, T, NCX, do_ctx)
        emit_D(P, pb, G, l, T, NCX, do_ctx)
        emit_E(P, pb, G, l, T, NCX, do_ctx, x1 if l == 0 else out)
        xT = x1
    print("[build_fused] instructions:", P.n_ins)
    return P.finish()


def host_inputs(inp, b):
    x, ctx = inp["x"], inp["ctx"]
    T = x.shape[1]
    m = {}
    m["xT0"] = np.ascontiguousarray(np.concatenate([x[b].T, ctx[b].T], axis=1), dtype=np.float32)
    C2 = np.stack([inp["c"][b], inp["c_ctx"]], axis=0).astype(np.float32)
    m["sT"] = np.ascontiguousarray(C2.T.reshape(KC, 128, 2).transpose(1, 0, 2))
    m["bmod"] = np.ascontiguousarray(np.stack([fm(inp["b_mod"][l]) for l in range(2)], axis=1))
    m["w_mod"] = inp["w_mod"]
    z16 = np.zeros((128, 16), np.float32)
    cvA, cvl, cvB, w_in, w_uq, w_uk, w_uv = [], [], [], [], [], [], []
    for l in range(2):
        qn, kn = inp["q_norm"][l], inp["k_norm"][l]
        cvA.append(np.concatenate([fm(inp["norm1"][l]), z16, z16, z16, z16, fm(inp["q_a_norm"][l]), fm(inp["kv_a_norm"][l]),
                                   pad128(qn[:128]), pad128(qn[128:]), pad128(qn[128 + _SW]),
                                   pad128(kn[:128]), pad128(kn[128:]), pad128(kn[128 + _SW])], axis=1))
        a, b_, c_, d_ = a_weights(inp, l)
        w_in.append(a); w_uq.append(b_); w_uk.append(c_); w_uv.append(d_)
        cw, cb = inp["conv_w"][l], inp["conv_b"][l]
        cols = [cw.reshape(4, 8, 128).transpose(2, 1, 0).reshape(128, 32), cb.reshape(8, 128).T]
        for nm in ("lru_ba", "lru_bx", "lru_lambda"):
            cols.append(inp[nm][l].reshape(2, 8, 128).transpose(2, 0, 1).reshape(128, 16))
        cvl.append(np.concatenate(cols, axis=1))
        cvB.append(np.concatenate([z16, z16, fm(inp["norm2"][l]), z16, z16, z16, z16], axis=1))
    m["cvA"] = np.ascontiguousarray(np.stack(cvA).astype(np.float32))
    m["cvl"] = np.ascontiguousarray(np.stack(cvl).astype(np.float32))
    m["cvB"] = np.ascontiguousarray(np.stack(cvB).astype(np.float32))
    m["w_in"] = np.stack(w_in); m["w_uq"] = np.stack(w_uq); m["w_uk"] = np.stack(w_uk); m["w_uv"] = np.stack(w_uv)
    cos, sin = rope_tables(np.arange(T))
    m["ropec"], m["ropes"] = cos, sin
    m["lru_wa"], m["lru_wx"] = inp["lru_wa"], inp["lru_wx"]
    m["w_out"], m["router"] = inp["w_out"], inp["router"]
    m["ident"] = np.eye(128, dtype=np.float32)
    m["ut"] = np.triu(np.ones((128, 128), np.float32))
    m["iot"] = np.tile(np.arange(512, dtype=np.float32), (128, 1))
    m["sid"] = (np.arange(128, dtype=np.float32)[:, None] + 128.0 * np.arange(4, dtype=np.float32)[None, :]).astype(np.float32)
    m["w_gate"], m["w_up"], m["w_down"] = inp["w_gate"], inp["w_up"], inp["w_down"]
    return {k: np.ascontiguousarray(v, dtype=np.float32) for k, v in m.items()}


def kernel(**inputs):
    inp = {k: np.asarray(v) for k, v in inputs.items()}
    Bn, T, _ = inp["x"].shape
    NCX = inp["ctx"].shape[1]
    key = (T, NCX)
    if key not in _PROG_CACHE:
        _PROG_CACHE[key] = build_fused(T, NCX)
    nc = _PROG_CACHE[key]
    maps = [host_inputs(inp, b) for b in range(Bn)]
    res = run_bass_kernel_spmd(nc, maps, core_ids=list(range(Bn)))
    out = np.stack([np.ascontiguousarray(r["out"].T) for r in res.results])
    return out.astype(np.float32, copy=False)
```

```python
import numpy as np
import ml_dtypes
import concourse.bass as bass
import concourse.mybir as mybir
from concourse.bass_utils import run_bass_kernel_spmd

F32 = mybir.dt.float32
BF16 = mybir.dt.bfloat16
ALU = mybir.AluOpType
AF = mybir.ActivationFunctionType
AX = mybir.AxisListType
NPBF = ml_dtypes.bfloat16

D = 2048
KC = 16
NH = 8
NE = 16
EPS = 1e-6
ATT_SCALE = 192 ** -0.5
NCORES = 8


class Prog:
    ENG = ("pe", "dve", "act", "pool", "sp")

    def __init__(self):
        self.nc = bass.Bass("TRN2", target_bir_lowering=False)
        nc = self.nc
        self.eng = {"pe": nc.tensor, "dve": nc.vector, "act": nc.scalar,
                    "pool": nc.gpsimd, "sp": nc.sync}
        self._ctx = []
        self.csem, self.dsem, self.cnt = {}, {}, {}
        self.NDS = 20
        self.drr = {}
        for e in self.ENG:
            self.csem[e] = self._enter(nc.semaphore("c_" + e))
            self.cnt[("c", e)] = 0
        for e in ("sp", "pool"):
            self.drr[e] = 0
            for i in range(self.NDS):
                self.dsem[(e, i)] = self._enter(nc.semaphore("d_%s%d" % (e, i)))
                self.cnt[("d", e, i)] = 0
        self.waited = {e: {} for e in self.ENG}
        self.lastw, self.readers, self.tags = {}, {}, {}
        self.skip = set()
        self.n_ins = 0
        self.uid = 0

    def _enter(self, cm):
        v = cm.__enter__()
        self._ctx.append(cm)
        return v

    def sb(self, name, shape, dt=F32):
        self.uid += 1
        return self._enter(self.nc.sbuf_tensor("%s_u%d" % (name, self.uid), list(shape), dt))

    def ps(self, name, shape, dt=F32):
        return self._enter(self.nc.psum_tensor(name, list(shape), dt))

    def din(self, name, shape, dt=F32):
        self.skip.add(name)
        return self.nc.dram_tensor(name, list(shape), dt, kind="ExternalInput").ap()

    def dout(self, name, shape, dt=F32):
        self.skip.add(name)
        return self.nc.dram_tensor(name, list(shape), dt, kind="ExternalOutput").ap()

    def dscr(self, name, shape, dt=F32, track=False):
        if not track:
            self.skip.add(name)
        return self.nc.dram_tensor(name, list(shape), dt, kind="Internal").ap()

    @staticmethod
    def _nm(t):
        if isinstance(t, str):
            return t
        if hasattr(t, "tensor"):
            t = t.tensor
        return t.name

    def _norm(self, ks):
        out = []
        for k in ks:
            if k is None or isinstance(k, (int, float)):
                continue
            if isinstance(k, tuple):
                n = self._nm(k[0])
                if n not in self.skip:
                    out.append((n, k[1]))
            else:
                n = self._nm(k)
                if n not in self.skip:
                    out.append((n, None))
        return out

    def _conf(self, key):
        name, tag = key
        if tag is None:
            return [(name, t) for t in self.tags.get(name, ())] + [(name, None)]
        return [(name, tag), (name, None)]

    def _sem(self, sk):
        return self.csem[sk[1]] if sk[0] == "c" else self.dsem[(sk[1], sk[2])]

    def emit(self, e, build, reads=(), writes=(), dma=False):
        reads = self._norm(reads)
        writes = self._norm(writes)
        deps = {}

        def need(d):
            if d is not None and deps.get(d[0], 0) < d[1]:
                deps[d[0]] = d[1]

        for k in reads:
            for c in self._conf(k):
                need(self.lastw.get(c))
        for k in writes:
            for c in self._conf(k):
                need(self.lastw.get(c))
                for r in self.readers.get(c, ()):
                    need(r)
        engine = self.eng[e]
        for sk, v in deps.items():
            if sk == ("c", "pe") and e == "pe" and not dma:
                continue
            if self.waited[e].get(sk, 0) >= v:
                continue
            engine.wait_ge(self._sem(sk), v)
            self.waited[e][sk] = v
        if dma:
            sk = ("d", e, self.drr[e] % self.NDS)
            self.drr[e] += 1
            if self.cnt[sk] > self.waited[e].get(sk, 0):
                engine.wait_ge(self._sem(sk), self.cnt[sk])
                self.waited[e][sk] = self.cnt[sk]
        else:
            sk = ("c", e)
        ins = build(engine)
        self.cnt[sk] += 16 if dma else 1
        ins.then_inc(self._sem(sk), 16 if dma else 1)
        me = (sk, self.cnt[sk])
        for k in writes:
            if k[1] is None:
                for c in self._conf(k):
                    self.lastw.pop(c, None)
                    self.readers.pop(c, None)
            else:
                self.tags.setdefault(k[0], set()).add(k[1])
            self.lastw[k] = me
            self.readers[k] = []
        for k in reads:
            if k[1] is not None:
                self.tags.setdefault(k[0], set()).add(k[1])
            lst = self.readers.setdefault(k, [])
            lst.append(me)
            if len(lst) > 10:
                best = {}
                for s, v in lst:
                    if best.get(s, 0) < v:
                        best[s] = v
                self.readers[k] = list(best.items())
        self.n_ins += 1
        return me

    def dma(self, out, in_, q="sp", rd=None, wr=None, slow=False):
        kw = {"allow_slow_non_contiguous": True} if slow else {}
        return self.emit(q, lambda en: en.dma_start(out=out, in_=in_, **kw),
                         rd if rd is not None else [in_], wr if wr is not None else [out], dma=True)

    def mm(self, out, lhsT, rhs, start, stop):
        return self.emit("pe", lambda en: en.matmul(out, lhsT, rhs, start=start, stop=stop),
                         [lhsT, rhs], [out])

    def act(self, out, in_, func, bias=None, scale=None, e="act"):
        kw = {}
        if bias is not None:
            kw["bias"] = bias
        if scale is not None:
            kw["scale"] = scale
        return self.emit(e, lambda en: en.activation(out, in_, func, **kw), [in_, bias, scale], [out])

    def tt(self, out, a, b, op, e="dve"):
        return self.emit(e, lambda en: en.tensor_tensor(out, a, b, op), [a, b], [out])

    def ts(self, out, a, s1, s2, op0, op1=None, e="dve"):
        if op1 is None:
            return self.emit(e, lambda en: en.tensor_scalar(out, a, s1, None, op0), [a, s1], [out])
        return self.emit(e, lambda en: en.tensor_scalar(out, a, s1, s2, op0, op1), [a, s1, s2], [out])

    def stt(self, out, in0, scalar, in1, op0, op1):
        return self.emit("dve", lambda en: en.scalar_tensor_tensor(out, in0, scalar, in1, op0, op1),
                         [in0, scalar, in1], [out])

    def copy(self, out, in_, e="dve"):
        return self.emit(e, lambda en: en.tensor_copy(out, in_), [in_], [out])

    def memset(self, out, val, e="dve"):
        return self.emit(e, lambda en: en.memset(out, val), [], [out])

    def scan(self, out, d0, d1, init, op0, op1):
        return self.emit("dve", lambda en: en.tensor_tensor_scan(out, d0, d1, init, op0, op1),
                         [d0, d1, init], [out])

    def recip(self, out, in_):
        return self.emit("dve", lambda en: en.reciprocal(out, in_), [in_], [out])

    def rstd(self, out, ss, inv_n, epsb, tmp=None):
        self.act(out, ss, AF.Sqrt, bias=epsb, scale=inv_n)
        self.recip(out, out)

    def barrier(self):
        for e in self.ENG:
            engine = self.eng[e]
            for sk, v in self.cnt.items():
                if v > self.waited[e].get(sk, 0):
                    engine.wait_ge(self._sem(sk), v)
                    self.waited[e][sk] = v
        self.lastw.clear()
        self.readers.clear()
        self.tags.clear()

    def phase_begin(self):
        return len(self._ctx)

    def phase_end(self, mark):
        self.barrier()
        while len(self._ctx) > mark:
            self._ctx.pop().__exit__(None, None, None)

    def finish(self):
        for sk, v in self.cnt.items():
            if v > 0:
                self.eng["sp"].wait_ge(self._sem(sk), v)
        while self._ctx:
            self._ctx.pop().__exit__(None, None, None)
        return self.nc


def fm(v):
    v = np.asarray(v, np.float32)
    return np.ascontiguousarray(v.reshape(-1, 128).T)


def chunks(n, step=512):
    return [(i, min(step, n - i)) for i in range(0, n, step)]


_SW = np.array([f + 16 if (f % 32) < 16 else f - 16 for f in range(64)])


def rope_tables(t_idx):
    t_idx = np.asarray(t_idx)
    row = (t_idx // 64).astype(np.float32)
    col = (t_idx % 64).astype(np.float32)
    inv = (np.float32(10000.0) ** (-np.arange(16, dtype=np.float32) / np.float32(16))).astype(np.float32)
    cos = np.zeros((64, len(t_idx)), np.float32)
    sin = np.zeros((64, len(t_idx)), np.float32)
    for f in range(64):
        pos = row if f < 32 else col
        ang = (pos * inv[f % 16]).astype(np.float32)
        cos[f] = np.cos(ang)
        s = np.sin(ang)
        sin[f] = -s if (f % 32) < 16 else s
    return cos, sin


def pad128(v):
    o = np.zeros((128, 1), np.float32)
    o[:len(v), 0] = v
    return o


def a_weights(inp, l):
    w_in = inp["w_in"][l]
    w_in_ext = np.ascontiguousarray(np.concatenate([w_in, w_in[:, 2816 + _SW]], axis=1))
    wq = inp["w_uq"][l].reshape(512, NH, 192)
    w_uq_ext = np.ascontiguousarray(np.concatenate([wq, wq[:, :, 128 + _SW]], axis=2).reshape(512, NH * 256))
    wkv = inp["w_ukv"][l].reshape(256, NH, 256)
    w_uk = np.ascontiguousarray(wkv[:, :, :128].reshape(256, 1024))
    w_uv = np.ascontiguousarray(wkv[:, :, 128:].reshape(256, 1024))
    return w_in_ext, w_uq_ext, w_uk, w_uv


I32 = mybir.dt.int32
N_BISECT = 34
W_IN_EXT = 2944
CV_A = dict(g1=0, sh_l=16, sc_l=32, sh_c=48, sc_c=64, gqa=80, gkva=84,
            gq_n=86, gq_r=87, gq_s=88, gk_n=89, gk_r=90, gk_s=91, n=92)
CV_B = dict(g1_l=0, g1_c=16, n2=32, sh2_l=48, sc2_l=64, sh2_c=80, sc2_c=96, n=112)


def emit_M(P, pb, G):
    mk = P.phase_begin()
    s2 = P.sb("M_s2", [128, KC, 2])
    bm = P.sb("M_bm", [128, 2, 96])
    wt = [P.sb("M_wt%d" % i, [128, KC, 512]) for i in range(2)]
    P.dma(s2[:], G["sT"])
    P.dma(bm[:], G["bmod"])
    P.act(s2[:], s2[:], AF.Silu)
    i = 0
    for l in range(2):
        wv = G["w_mod"][l].rearrange("(kc p) n -> p kc n", p=128)
        for cg in range(24):
            t = wt[i % 2]
            P.dma(t[:], wv[:, :, cg * 512:(cg + 1) * 512], q="sp" if i % 2 == 0 else "pool")
            i += 1
            for ci in range(4):
                ch = cg * 4 + ci
                p_ = pb[ch % 2]
                for kc in range(KC):
                    P.mm(p_[:, 0:2], t[:, kc, ci * 128:(ci + 1) * 128], s2[:, kc, :], kc == 0, kc == KC - 1)
                P.ts(G["modfm"][:, l, ch, :], p_[:, 0:2], bm[:, l, ch:ch + 1], None, ALU.add)
    P.phase_end(mk)


def emit_A(P, pb, G, l, xT, NLC, NCX):
    NT = NLC + NCX
    mk = P.phase_begin()
    modfm = G["modfm"]
    cvt = P.sb("A_cvt", [128, CV_A["n"]])
    P.dma(cvt[:], G["cvA"][l])
    P.copy(cvt[:, 16:32], modfm[:, l, 0:16, 0])
    P.copy(cvt[:, 32:48], modfm[:, l, 16:32, 0])
    P.copy(cvt[:, 48:64], modfm[:, l, 0:16, 1])
    P.copy(cvt[:, 64:80], modfm[:, l, 16:32, 1])
    C = lambda name, j=0, w=1: cvt[:, CV_A[name] + j: CV_A[name] + j + w]
    epsb = P.sb("A_epsb", [128, 1])
    P.memset(epsb[:], EPS)
    ones = P.sb("A_ones", [128, 128])
    P.memset(ones[:], 1.0)
    Am = P.sb("A_Am", [128, 2, KC])
    for k, nm in enumerate(("sc_l", "sc_c")):
        P.ts(Am[:, k, :], C(nm, 0, KC), 1.0, None, ALU.add)
        P.tt(Am[:, k, :], Am[:, k, :], C("g1", 0, KC), ALU.mult)
    rc_t = P.sb("A_rc", [64, 512])
    rs_t = P.sb("A_rs", [64, 512])
    tch = [(c0, n, 0) for c0, n in chunks(NLC)] + [(NLC + c0, n, 1) for c0, n in chunks(NCX)]
    hm_d = G["hm_d"]
    xt = P.sb("A_xt", [128, KC, 512])
    sq = [P.sb("A_sq%d" % i, [128, 512]) for i in range(2)]
    rs = P.sb("A_rsd", [128, 512])
    hmc = [P.sb("A_hmc%d" % i, [128, KC, 512], BF16) for i in range(3)]
    hm = hmc[0]
    xv = xT.rearrange("(kc p) n -> p kc n", p=128)
    for (c0, n, kind) in tch:
        P.dma(xt[:, :, :n], xv[:, :, c0:c0 + n], q="pool")
        for kc in range(KC):
            s_ = sq[kc % 2]
            P.act(s_[:, :n], xt[:, kc, :n], AF.Square)
            P.mm(pb[0][:, :n], ones[:], s_[:, :n], kc == 0, kc == KC - 1)
        P.rstd(rs[:, :n], pb[0][:, :n], 1.0 / D, epsb[:])
        shn = "sh_l" if kind == 0 else "sh_c"
        for kc in range(KC):
            s_ = sq[kc % 2]
            P.tt(s_[:, :n], xt[:, kc, :n], rs[:, :n], ALU.mult)
            P.ts(hm[:, kc, :n], s_[:, :n], Am[:, kind, kc:kc + 1], C(shn, kc), ALU.mult, ALU.add)
        P.dma(hm_d[:, :, c0:c0 + n], hm[:, :, :n], wr=[(hm_d, c0)])

    wg = [P.sb("A_wg%d" % i, [128, KC, 512], BF16) for i in range(2)]
    wuq = P.sb("A_wuq", [128, 4, 2048], BF16)
    wuk = P.sb("A_wuk", [128, 2, 1024], BF16)
    wuv = P.sb("A_wuv", [128, 2, 1024], BF16)
    P.dma(wuq[:], G["w_uq"][l].rearrange("(kc p) n -> p kc n", p=128), q="pool")
    P.dma(wuk[:], G["w_uk"][l].rearrange("(kc p) n -> p kc n", p=128), q="pool")
    P.dma(wuv[:], G["w_uv"][l].rearrange("(kc p) n -> p kc n", p=128), q="pool")
    wv = G["w_in"][l].rearrange("(kc p) n -> p kc n", p=128)
    ev = [P.sb("A_ev%d" % i, [128, 512]) for i in range(2)]
    evb = [P.sb("A_evb%d" % i, [128, 512], BF16) for i in range(2)]
    cqt = P.sb("A_cqt", [128, 4, 512])
    cqn = P.sb("A_cqn", [128, 4, 512], BF16)
    ckt = P.sb("A_ckt", [128, 2, 512])
    ckn = P.sb("A_ckn", [128, 2, 512], BF16)
    krt = P.sb("A_krt", [64, 512])
    kst = P.sb("A_kst", [64, 512])
    krsq = P.sb("A_krsq", [64, 512])
    Rt = P.sb("A_Rt", [64, 512])
    sqn2 = [P.sb("A_sqn%d" % i, [128, 512]) for i in range(2)]
    sqr2 = [P.sb("A_sqr%d" % i, [64, 512]) for i in range(2)]
    rsh2 = [P.sb("A_rsh%d" % i, [128, 512]) for i in range(2)]
    t64a = P.sb("A_t64a", [64, 512])
    t64b = P.sb("A_t64b", [64, 512])
    ob64 = [P.sb("A_ob64_%d" % i, [64, 512], BF16) for i in range(2)]
    vb = [P.sb("A_vb%d" % i, [128, 1024], BF16) for i in range(2)]
    xrT, ggT, qn_o, qr_o, kn_o, kr_o, v_o = G["xrT"], G["ggT"], G["qn"], G["qr"], G["kn"], G["kr"], G["v"]

    def rope_mix(out_bf, a_f, b_f, n, kind):
        if kind == 0:
            P.tt(a_f, a_f, rc_t[:, :n], ALU.mult)
            P.tt(b_f, b_f, rs_t[:, :n], ALU.mult)
            P.tt(out_bf, a_f, b_f, ALU.add)
        else:
            P.copy(out_bf, a_f)

    groups = chunks(W_IN_EXT)
    it = 0
    for gi, (g0, gn) in enumerate(groups):
        w_ = wg[gi % 2]
        P.dma(w_[:, :, :gn], wv[:, :, g0:g0 + gn], q="pool")
        for (c0, n, kind) in tch:
            h_ = hmc[it % 3]
            it += 1
            P.dma(h_[:, :, :n], hm_d[:, :, c0:c0 + n], rd=[(hm_d, c0)], q="pool")
            if kind == 0 and g0 >= 2048:
                P.dma(rc_t[:, :n], G["ropec"][:, c0:c0 + n], q="pool")
                P.dma(rs_t[:, :n], G["ropes"][:, c0:c0 + n], q="pool")
            nm = (gn + 127) // 128
            for mi in range(nm):
                col = g0 + mi * 128
                mw = min(128, gn - mi * 128)
                p_ = pb[1 + (mi % 4)]
                if col < 2816:
                    for kc in range(KC):
                        P.mm(p_[:mw, :n], w_[:, kc, mi * 128: mi * 128 + mw], h_[:, kc, :n], kc == 0, kc == KC - 1)
                m = col // 128
                if m < 8:
                    e_ = ev[m % 2]
                    P.act(e_[:, :n], p_[:, :n], AF.Copy)
                    P.dma(xrT[m * 128:(m + 1) * 128, c0:c0 + n], e_[:, :n])
                elif m < 16:
                    e_ = evb[m % 2]
                    P.act(e_[:, :n], p_[:, :n], AF.Gelu_apprx_tanh)
                    P.dma(ggT[(m - 8) * 128:(m - 7) * 128, c0:c0 + n], e_[:, :n])
                elif m < 20:
                    P.copy(cqt[:, m - 16, :n], p_[:, :n])
                elif m < 22:
                    P.copy(ckt[:, m - 20, :n], p_[:, :n])
                else:
                    for kc in range(KC):
                        P.mm(pb[5][:64, :n], w_[:, kc, mi * 128: mi * 128 + 64], h_[:, kc, :n], kc == 0, kc == KC - 1)
                    for kc in range(KC):
                        P.mm(pb[6][:64, :n], w_[:, kc, mi * 128 + 64: mi * 128 + 128], h_[:, kc, :n], kc == 0, kc == KC - 1)
                    P.copy(krt[:, :n], pb[5][:64, :n])
                    P.copy(kst[:, :n], pb[6][:64, :n])
            if g0 == 2048:
                for kc in range(4):
                    s_ = sq[kc % 2]
                    P.act(s_[:, :n], cqt[:, kc, :n], AF.Square)
                    P.mm(pb[0][:, :n], ones[:], s_[:, :n], kc == 0, kc == 3)
                P.rstd(rs[:, :n], pb[0][:, :n], 1.0 / 512, epsb[:])
                for kc in range(4):
                    P.stt(cqn[:, kc, :n], cqt[:, kc, :n], C("gqa", kc), rs[:, :n], ALU.mult, ALU.mult)
                def qproj(h):
                    pn, pr, pS = pb[1 + (h % 2) * 3], pb[2 + (h % 2) * 3], pb[3 + (h % 2) * 3]
                    b0 = h * 256
                    for kc in range(4):
                        P.mm(pn[:, :n], wuq[:, kc, b0:b0 + 128], cqn[:, kc, :n], kc == 0, kc == 3)
                    for kc in range(4):
                        P.mm(pr[:64, :n], wuq[:, kc, b0 + 128:b0 + 192], cqn[:, kc, :n], kc == 0, kc == 3)
                    for kc in range(4):
                        P.mm(pS[:64, :n], wuq[:, kc, b0 + 192:b0 + 256], cqn[:, kc, :n], kc == 0, kc == 3)

                qproj(0)
                for h in range(NH):
                    pn, pr, pS = pb[1 + (h % 2) * 3], pb[2 + (h % 2) * 3], pb[3 + (h % 2) * 3]
                    sqn_, sqr_, rsh_ = sqn2[h % 2], sqr2[h % 2], rsh2[h % 2]
                    P.act(sqn_[:, :n], pn[:, :n], AF.Square)
                    P.act(sqr_[:, :n], pr[:64, :n], AF.Square)
                    if h + 1 < NH:
                        qproj(h + 1)
                    P.mm(pb[7][:, :n], ones[:], sqn_[:, :n], True, False)
                    P.mm(pb[7][:, :n], ones[:64, :], sqr_[:, :n], False, True)
                    P.rstd(rsh_[:, :n], pb[7][:, :n], 1.0 / 192, epsb[:])
                    o_ = evb[h % 2]
                    P.stt(o_[:, :n], pn[:, :n], C("gq_n"), rsh_[:, :n], ALU.mult, ALU.mult)
                    P.dma(qn_o[h, :, c0:c0 + n], o_[:, :n])
                    P.stt(t64a[:, :n], pr[:64, :n], cvt[:64, CV_A["gq_r"]:CV_A["gq_r"] + 1], rsh_[:64, :n], ALU.mult, ALU.mult)
                    P.stt(t64b[:, :n], pS[:64, :n], cvt[:64, CV_A["gq_s"]:CV_A["gq_s"] + 1], rsh_[:64, :n], ALU.mult, ALU.mult)
                    o6 = ob64[h % 2]
                    rope_mix(o6[:, :n], t64a[:, :n], t64b[:, :n], n, kind)
                    P.dma(qr_o[h, :, c0:c0 + n], o6[:, :n])
            if g0 == 2560:
                for kc in range(2):
                    s_ = sq[kc % 2]
                    P.act(s_[:, :n], ckt[:, kc, :n], AF.Square)
                    P.mm(pb[0][:, :n], ones[:], s_[:, :n], kc == 0, kc == 1)
                P.rstd(rs[:, :n], pb[0][:, :n], 1.0 / 256, epsb[:])
                for kc in range(2):
                    P.stt(ckn[:, kc, :n], ckt[:, kc, :n], C("gkva", kc), rs[:, :n], ALU.mult, ALU.mult)
                P.act(krsq[:, :n], krt[:, :n], AF.Square)
                P.ts(t64a[:, :n], krt[:, :n], cvt[:64, CV_A["gk_r"]:CV_A["gk_r"] + 1], None, ALU.mult)
                P.ts(t64b[:, :n], kst[:, :n], cvt[:64, CV_A["gk_s"]:CV_A["gk_s"] + 1], None, ALU.mult)
                if kind == 0:
                    P.tt(t64a[:, :n], t64a[:, :n], rc_t[:, :n], ALU.mult)
                    P.tt(t64b[:, :n], t64b[:, :n], rs_t[:, :n], ALU.mult)
                    P.tt(Rt[:, :n], t64a[:, :n], t64b[:, :n], ALU.add)
                else:
                    P.copy(Rt[:, :n], t64a[:, :n])
                def kproj(h):
                    pn = pb[1 + (h % 2)]
                    for kc in range(2):
                        P.mm(pn[:, :n], wuk[:, kc, h * 128:(h + 1) * 128], ckn[:, kc, :n], kc == 0, kc == 1)

                kproj(0)
                for h in range(NH):
                    pn = pb[1 + (h % 2)]
                    sqn_, rsh_ = sqn2[h % 2], rsh2[h % 2]
                    P.act(sqn_[:, :n], pn[:, :n], AF.Square)
                    if h + 1 < NH:
                        kproj(h + 1)
                    P.mm(pb[7][:, :n], ones[:], sqn_[:, :n], True, False)
                    P.mm(pb[7][:, :n], ones[:64, :], krsq[:, :n], False, True)
                    P.rstd(rsh_[:, :n], pb[7][:, :n], 1.0 / 192, epsb[:])
                    o_ = evb[h % 2]
                    P.stt(o_[:, :n], pn[:, :n], C("gk_n"), rsh_[:, :n], ALU.mult, ALU.mult)
                    P.dma(kn_o[h, :, c0:c0 + n], o_[:, :n])
                    o6 = ob64[h % 2]
                    P.tt(o6[:, :n], Rt[:, :n], rsh_[:64, :n], ALU.mult)
                    P.dma(kr_o[h, :, c0:c0 + n], o6[:, :n])
                for j in range(n // 128):
                    vt = vb[j % 2]
                    for hh in range(2):
                        pv = pb[3 + hh]
                        for kc in range(2):
                            P.mm(pv[:, :], ckn[:, kc, j * 128:(j + 1) * 128], wuv[:, kc, hh * 512:(hh + 1) * 512], kc == 0, kc == 1)
                        P.act(vt[:, hh * 512:(hh + 1) * 512], pv[:, :], AF.Copy)
                    P.dma(v_o[c0 + j * 128: c0 + (j + 1) * 128, :], vt[:])
    P.phase_end(mk)


def emit_A2(P, pb, G, l, T, NCX):
    mk = P.phase_begin()
    xr, gg, yr = G["xrT"], G["ggT"], G["yr"]
    cvt = P.sb("L_cvt", [128, 88])
    P.dma(cvt[:], G["cvl"][l])
    one = P.sb("L_one", [128, 1])
    P.memset(one[:], 1.0)
    cl = P.sb("L_cl", [128, 16])
    P.act(cl[:], cvt[:, 72:88], AF.Exp, scale=-1.0)
    P.act(cl[:], cl[:], AF.Ln, bias=one[:])
    P.ts(cl[:], cl[:], -8.0, None, ALU.mult)
    wab = P.sb("L_wab", [128, 2, 8, 128], BF16)
    wxb = P.sb("L_wxb", [128, 2, 8, 128], BF16)
    for d in range(2):
        P.dma(wab[:, d, :, :], G["lru_wa"][l, d].rearrange("g i j -> i g j"), q="pool")
        P.dma(wxb[:, d, :, :], G["lru_wx"][l, d].rearrange("g i j -> i g j"), q="pool")
    streams = [("C", T, NCX), ("L", 0, T)]
    tl = {}
    for s, _, n in streams:
        X = P.sb("L_X" + s, [128, n + 3])
        tl[s] = dict(X=X, u=P.sb("L_u" + s, [128, n]), ub=P.sb("L_ub" + s, [128, n], BF16),
                     ra=[P.sb("L_ra%d" % d + s, [128, n]) for d in range(2)],
                     ii=[P.sb("L_ii%d" % d + s, [128, n]) for d in range(2)],
                     sb=[P.sb("L_sb%d" % d + s, [128, n]) for d in range(2)],
                     hf=P.sb("L_hf" + s, [128, n]),
                     gg=P.sb("L_gg" + s, [128, n], BF16), yo=P.sb("L_yo" + s, [128, n], BF16))
    for g in range(8):
        rows = slice(g * 128, (g + 1) * 128)
        for s, s0, n in streams:
            t = tl[s]
            P.memset(t["X"][:, 0:2], 0.0)
            P.memset(t["X"][:, n + 2:n + 3], 0.0)
            P.dma(t["X"][:, 2:n + 2], xr[rows, s0:s0 + n])
            P.dma(t["gg"][:], gg[rows, s0:s0 + n])
            P.ts(t["u"][:], t["X"][:, 0:n], cvt[:, g * 4:g * 4 + 1], cvt[:, 32 + g:33 + g], ALU.mult, ALU.add)
            for k in range(1, 4):
                P.stt(t["u"][:], t["X"][:, k:k + n], cvt[:, g * 4 + k:g * 4 + k + 1], t["u"][:], ALU.mult, ALU.add)
            P.act(t["ub"][:], t["u"][:], AF.Copy)
        for d in range(2):
            for s, s0, n in streams:
                t = tl[s]
                ra, ii, sb = t["ra"][d], t["ii"][d], t["sb"][d]
                for ci, (c0, cn) in enumerate(chunks(n)):
                    pr, pi = pb[(ci % 2) * 2], pb[(ci % 2) * 2 + 1]
                    P.mm(pr[:, :cn], wab[:, d, g, :], t["ub"][:, c0:c0 + cn], True, True)
                    P.mm(pi[:, :cn], wxb[:, d, g, :], t["ub"][:, c0:c0 + cn], True, True)
                    P.act(ra[:, c0:c0 + cn], pr[:, :cn], AF.Sigmoid, bias=cvt[:, 40 + d * 8 + g:41 + d * 8 + g])
                    P.act(ii[:, c0:c0 + cn], pi[:, :cn], AF.Sigmoid, bias=cvt[:, 56 + d * 8 + g:57 + d * 8 + g])
                P.act(ra[:], ra[:], AF.Exp, scale=cl[:, d * 8 + g:d * 8 + g + 1])
                P.act(sb[:], ra[:], AF.Square)
                P.act(sb[:], sb[:], AF.Sqrt, bias=one[:], scale=-1.0)
        for d in range(2):
            for s, s0, n in streams:
                t = tl[s]
                ra, ii, sb = t["ra"][d], t["ii"][d], t["sb"][d]
                P.tt(ii[:], ii[:], t["u"][:], ALU.mult)
                P.tt(sb[:], sb[:], ii[:], ALU.mult)
                if d == 0:
                    init = 0.0 if s == "C" else tl["C"]["hf"][:, NCX - 1:NCX]
                    P.scan(t["hf"][:], ra[:], sb[:], init, ALU.mult, ALU.add)
                else:
                    hb = t["X"][:, 0:n]
                    init = 0.0 if s == "C" else tl["C"]["X"][:, 0:1]
                    P.scan(hb[:, ::-1], ra[:, ::-1], sb[:, ::-1], init, ALU.mult, ALU.add)
        for s, s0, n in streams:
            t = tl[s]
            P.tt(t["hf"][:], t["hf"][:], t["X"][:, 0:n], ALU.add)
            P.tt(t["yo"][:], t["hf"][:], t["gg"][:], ALU.mult)
            P.dma(yr[rows, s0:s0 + n], t["yo"][:])
    P.phase_end(mk)


def emit_B(P, pb, G, l, xT, T, NCX, do_ctx):
    NT = T + NCX
    NKT = NT // 128
    NI = T // 128
    modfm = G["modfm"]
    aff_sb = G["aff_sb"]
    HQ = min(2048, T)
    for qp in range(T // HQ):
        mk = P.phase_begin()
        qch = [(qp * HQ + c0, n, 0, c0) for c0, n in chunks(HQ)]
        NQP = HQ
        if do_ctx and qp == 0:
            qch += [(T + c0, n, 1, HQ + c0) for c0, n in chunks(NCX)]
            NQP = HQ + NCX
        cvt = P.sb("B_cvt", [128, CV_B["n"]])
        P.dma(cvt[:], G["cvB"][l])
        P.copy(cvt[:, 0:16], modfm[:, l, 32:48, 0])
        P.copy(cvt[:, 16:32], modfm[:, l, 32:48, 1])
        P.copy(cvt[:, 48:64], modfm[:, l, 48:64, 0])
        P.copy(cvt[:, 64:80], modfm[:, l, 64:80, 0])
        P.copy(cvt[:, 80:96], modfm[:, l, 48:64, 1])
        P.copy(cvt[:, 96:112], modfm[:, l, 64:80, 1])
        C = lambda name, j=0, w=1: cvt[:, CV_B[name] + j: CV_B[name] + j + w]
        epsb = P.sb("B_epsb", [128, 1])
        P.memset(epsb[:], EPS)
        ones = P.sb("B_ones", [128, 128])
        P.memset(ones[:], 1.0)
        onesb = P.sb("B_onesb", [128, 128], BF16)
        P.memset(onesb[:], 1.0)
        ident = P.sb("B_ident", [128, 128])
        P.dma(ident[:], G["ident"])
        A2m = P.sb("B_A2m", [128, 2, KC])
        for k, nm in enumerate(("sc2_l", "sc2_c")):
            P.ts(A2m[:, k, :], C(nm, 0, KC), 1.0, None, ALU.add)
            P.tt(A2m[:, k, :], A2m[:, k, :], C("n2", 0, KC), ALU.mult)
        rt = P.sb("B_rt", [128, KC, NE])
        P.dma(rt[:], G["router"][l].rearrange("(kc p) e -> p kc e", p=128))

        yatt = P.sb("B_yatt", [128, 8, NQP], BF16)
        yrv = G["yr"].rearrange("(c p) n -> p c n", p=128)
        kvq = [dict(knt=P.sb("B_knt%d" % i, [128, NT], BF16), krt=P.sb("B_krt%d" % i, [64, NT], BF16),
                    vt=P.sb("B_vt%d" % i, [128, NKT, 128], BF16), qnt=P.sb("B_qnt%d" % i, [128, NQP], BF16),
                    qrt=P.sb("B_qrt%d" % i, [64, NQP], BF16)) for i in range(2)]
        pt = [P.sb("B_pt%d" % i, [128, 512], BF16) for i in range(3)]
        rec = P.sb("B_rec", [128, 512])
        it = 0
        for h in range(NH):
            kq = kvq[h % 2]
            knt, krt, vt, qnt, qrt = kq["knt"], kq["krt"], kq["vt"], kq["qnt"], kq["qrt"]
            P.dma(knt[:], G["kn"][h])
            P.dma(krt[:], G["kr"][h])
            P.dma(vt[:], G["v"][:, h * 128:(h + 1) * 128].rearrange("(j p) d -> p j d", p=128))
            P.dma(qnt[:, 0:HQ], G["qn"][h, :, qp * HQ:(qp + 1) * HQ])
            P.dma(qrt[:, 0:HQ], G["qr"][h, :, qp * HQ:(qp + 1) * HQ])
            if NQP > HQ:
                P.dma(qnt[:, HQ:NQP], G["qn"][h, :, T:NT])
                P.dma(qrt[:, HQ:NQP], G["qr"][h, :, T:NT])
            for (ca, n, kind, c0) in qch:
                kts = list(range(NKT)) if kind == 0 else list(range(NI, NKT))
                SB = (pb[0], pb[1], pb[4], pb[5])

                def qk(ji):
                    S = SB[ji % 4]
                    j = kts[ji]
                    P.mm(S[:, :n], knt[:, j * 128:(j + 1) * 128], qnt[:, c0:c0 + n], True, False)
                    P.mm(S[:, :n], krt[:, j * 128:(j + 1) * 128], qrt[:, c0:c0 + n], False, True)

                for ji in range(min(2, len(kts))):
                    qk(ji)
                for ji, j in enumerate(kts):
                    if ji + 2 < len(kts):
                        qk(ji + 2)
                    p_ = pt[ji % 3]
                    P.act(p_[:, :n], SB[ji % 4][:, :n], AF.Exp, scale=ATT_SCALE)
                    P.mm(pb[2][:, :n], vt[:, j, :], p_[:, :n], ji == 0, ji == len(kts) - 1)
                    P.mm(pb[3][:, :n], onesb[:], p_[:, :n], ji == 0, ji == len(kts) - 1)
                P.recip(rec[:, :n], pb[3][:, :n])
                P.tt(yatt[:, h, c0:c0 + n], pb[2][:, :n], rec[:, :n], ALU.mult)

        xc = P.sb("B_xc", [128, KC, 512])
        yrc = P.sb("B_yrc", [128, 8, 512], BF16)
        wob = [P.sb("B_wob%d" % i, [128, KC, 256], BF16) for i in range(2)]
        sq = [P.sb("B_sq%d" % i, [128, 512]) for i in range(2)]
        htm = [P.sb("B_htm%d" % i, [128, D], BF16) for i in range(4)]
        rs = P.sb("B_rs", [128, 512])
        mx = P.sb("B_mx", [128, 1])
        sm = P.sb("B_sm", [128, 1])
        ex = P.sb("B_ex", [128, NE])
        xv = xT.rearrange("(kc p) n -> p kc n", p=128)
        xnv = G["xn"].rearrange("(kc p) n -> p kc n", p=128)
        wov = G["w_out"][l].rearrange("(kc p) n -> p kc n", p=128)
        it = 0
        for (ca, n, kind, c0) in qch:
            nj = n // 128
            P.dma(xc[:, :, :n], xv[:, :, ca:ca + n], q="pool")
            P.dma(yrc[:, :, :n], yrv[:, :, ca:ca + n], q="pool")
            g1n = "g1_l" if kind == 0 else "g1_c"
            shn = "sh2_l" if kind == 0 else "sh2_c"
            for mg in range(8):
                wo = wob[it % 2]
                it += 1
                P.dma(wo[:], wov[:, :, mg * 256:(mg + 1) * 256], q="pool")
                for mi in range(2):
                    m = mg * 2 + mi
                    p_ = pb[m % 2]
                    for kc in range(KC):
                        rhs_ = yrc[:, kc, :n] if kc < 8 else yatt[:, kc - 8, c0:c0 + n]
                        P.mm(p_[:, :n], wo[:, kc, mi * 128:(mi + 1) * 128], rhs_, kc == 0, kc == KC - 1)
                    P.stt(xc[:, m, :n], p_[:, :n], C(g1n, m), xc[:, m, :n], ALU.mult, ALU.add)
            P.dma(xnv[:, :, ca:ca + n], xc[:, :, :n])
            for m in range(KC):
                P.act(sq[m % 2][:, :n], xc[:, m, :n], AF.Square)
                P.mm(pb[2][:, :n], ones[:], sq[m % 2][:, :n], m == 0, m == KC - 1)
            P.rstd(rs[:, :n], pb[2][:, :n], 1.0 / D, epsb[:])
            for m in range(KC):
                P.tt(xc[:, m, :n], xc[:, m, :n], rs[:, :n], ALU.mult)
                P.ts(xc[:, m, :n], xc[:, m, :n], A2m[:, kind, m:m + 1], C(shn, m), ALU.mult, ALU.add)
                for j in range(nj):
                    P.emit("pe", lambda en, j=j, m=m: en.transpose(pb[4 + j][:, (m % 4) * 128:(m % 4 + 1) * 128],
                                                                 xc[:, m, j * 128:(j + 1) * 128], ident[:]),
                           [xc, ident], [pb[4 + j]])
                if m % 4 == 3:
                    for j in range(nj):
                        dst = htm[j][:, (m // 4) * 512:(m // 4 + 1) * 512]
                        if j % 2 == 0:
                            P.copy(dst, pb[4 + j][:, :])
                        else:
                            P.act(dst, pb[4 + j][:, :], AF.Copy)
            for j in range(nj):
                P.dma(G["h2tm"][ca + j * 128:ca + (j + 1) * 128, :], htm[j][:])
                for m in range(KC):
                    P.mm(pb[j][:, :NE], xc[:, m, j * 128:(j + 1) * 128], rt[:, m, :], m == 0, m == KC - 1)
                lg = pb[j][:, :NE]
                ti = (ca // 128) + j
                P.emit("dve", lambda en, lg=lg: en.tensor_reduce(mx[:], lg, AX.X, ALU.max), [lg], [mx])
                P.ts(mx[:], mx[:], -1.0, None, ALU.mult)
                P.act(ex[:], lg, AF.Exp, bias=mx[:])
                P.emit("dve", lambda en: en.tensor_reduce(sm[:], ex[:], AX.X, ALU.add), [ex], [sm])
                P.recip(sm[:], sm[:])
                P.ts(aff_sb[:, :, ti], ex[:], sm[:, 0:1], None, ALU.mult)
        P.phase_end(mk)


def emit_D(P, pb, G, l, T, NCX, do_ctx):
    mk = P.phase_begin()
    NI, NIC = T // 128, NCX // 128
    cap, capc = 2 * T // NE, 2 * NCX // NE
    NU = NE
    aff_sb = G["aff_sb"]
    ones = P.sb("D_ones", [128, 128])
    P.memset(ones[:], 1.0)
    onesb = P.sb("D_onesb", [128, 128], BF16)
    P.memset(onesb[:], 1.0)
    utf = P.sb("D_utf", [128, 128])
    P.dma(utf[:], G["ut"])
    utb = P.sb("D_utb", [128, 128], BF16)
    P.copy(utb[:], utf[:])
    iott = P.sb("D_iott", [128, 512])
    P.dma(iott[:], G["iot"])
    zer = P.sb("D_zer", [128, 64])
    P.memset(zer[:], 0.0)

    key_sb = G["key_sb"]
    streams = [("L", aff_sb[:, :, 0:NI], key_sb[:, :, 0:NI], NI, cap)]
    if do_ctx:
        streams.append(("C", aff_sb[:, :, NI:NI + NIC], key_sb[:, :, NI:NI + NIC], NIC, capc))
    NS = len(streams)
    lo = P.sb("D_lo", [128, NS, NU])
    mid = P.sb("D_mid", [128, NS, NU])
    cnt = P.sb("D_cnt", [128, NS, NU])
    capt = P.sb("D_capt", [128, NS, NU])
    ge = P.sb("D_ge", [128, NS, NU], I32)
    cms = [P.sb("D_cm" + tg, [128, NU, ni]) for (tg, _, _, ni, _) in streams]
    P.memset(lo[:], 0.0)
    for si, (_, _, _, _, cv_) in enumerate(streams):
        P.memset(capt[:, si, :], float(cv_))
    lof = lo[:].rearrange("p s u -> p (s u)")
    midf = mid[:].rearrange("p s u -> p (s u)")
    for k in range(N_BISECT):
        P.ts(midf, lof, float(2.0 ** -(k + 1)), None, ALU.add)
        for si, (_, af, _, ni, _) in enumerate(streams):
            P.tt(cms[si][:], af, mid[:, si, :].unsqueeze(2).to_broadcast([128, NU, ni]), ALU.is_ge)
            P.emit("dve", lambda en, si=si: en.tensor_reduce(cnt[:, si, :], cms[si][:], AX.X, ALU.add), [cms[si]], [cnt])
        P.mm(pb[0][:, :NS * NU], ones[:], cnt[:].rearrange("p s u -> p (s u)"), True, True)
        P.tt(ge[:].rearrange("p s u -> p (s u)"), pb[0][:, :NS * NU], capt[:].rearrange("p s u -> p (s u)"), ALU.is_ge)
        P.emit("dve", lambda en: en.copy_predicated(lof, ge[:].rearrange("p s u -> p (s u)"), midf), [ge, mid, lo], [lo])
    for si, (tag, af, pos, ni, _) in enumerate(streams):
        n3 = [128, NU, ni]
        cm = cms[si]
        P.tt(cm[:], af, lo[:, si, :].unsqueeze(2).to_broadcast(n3), ALU.is_ge)
        mb = P.sb("D_mb" + tag, [128, NU * ni], BF16)
        P.copy(mb[:], cm[:].rearrange("p u i -> p (u i)"))
        tot = P.sb("D_tot" + tag, n3)
        inc = P.sb("D_inc" + tag, n3)
        wit = P.sb("D_wit" + tag, n3)
        for c0, cn in chunks(NU * ni):
            P.mm(pb[1][:, :cn], utb[:], mb[:, c0:c0 + cn], True, True)
            P.mm(pb[2][:, :cn], onesb[:], mb[:, c0:c0 + cn], True, True)
            P.copy(wit[:].rearrange("p u i -> p (u i)")[:, c0:c0 + cn], pb[1][:, :cn])
            P.copy(tot[:].rearrange("p u i -> p (u i)")[:, c0:c0 + cn], pb[2][:, :cn])
        for u in range(NU):
            P.scan(inc[:, u, :], tot[:, u, :], zer[:, :ni], 0.0, ALU.add, ALU.add)
        P.tt(inc[:], inc[:], tot[:], ALU.subtract)
        P.tt(pos, inc[:], wit[:], ALU.add)
        P.tt(pos, pos, cm[:], ALU.mult)
        P.ts(pos, pos, -1.0, None, ALU.add)
    keyL, keyC = key_sb[:, :, 0:NI], key_sb[:, :, NI:NI + NIC]
    NT = T + NCX

    identf = P.sb("D_ident", [128, 128])
    P.dma(identf[:], G["ident"])
    trT = [P.sb("D_trT%d" % i, [128, 128]) for i in range(2)]
    ncol = NI + NIC
    ti = 0
    for src, dstD in ((key_sb, G["keyD"]), (aff_sb, G["gateD"])):
        flat = src[:].rearrange("p e i -> p (e i)")
        dst2 = dstD.rearrange("e (i p) -> (e i) p", p=128)
        for c0 in range(0, NE * ncol, 128):
            w = min(128, NE * ncol - c0)
            t_ = trT[ti % 2]
            ti += 1
            P.emit("pe", lambda en, c0=c0, w=w, flat=flat: en.transpose(pb[1][:w, :128], flat[:, c0:c0 + w], identf[:]),
                   [src, identf], [pb[1]])
            P.copy(t_[:w, :], pb[1][:w, :128])
            P.dma(dst2[c0:c0 + w, :], t_[:w, :])

    xg = P.sb("D_xg", [128, KC, cap], BF16)
    at = P.sb("D_at", [128, KC, cap], BF16)
    if do_ctx:
        xgc = P.sb("D_xgc", [128, KC, capc], BF16)
        atc = P.sb("D_atc", [128, KC, capc], BF16)
    selr = [P.sb("D_sel%d" % i, [128, 512], BF16) for i in range(3)]
    h2t = [P.sb("D_h2t%d" % i, [128, 1024], BF16) for i in range(3)]
    WG = 512
    wbuf = [P.sb("D_wbuf%d" % i, [128, KC, WG], BF16) for i in range(4)]
    sg = [P.sb("D_sg%d" % i, [128, 512]) for i in range(2)]
    yo = [P.sb("D_yo%d" % i, [128, 512], BF16) for i in range(2)]
    cnt_it = [0, 0, 0]
    h2tm = G["h2tm"]

    def gather(row0, key_t, u, ni, capv, dst):
        for half in range(2):
            for i in range(ni):
                s_ = selr[cnt_it[0] % 3]
                h_ = h2t[cnt_it[0] % 3]
                cnt_it[0] += 1
                P.ts(s_[:, :capv], iott[:, :capv], key_t[:, u, i:i + 1], None, ALU.is_equal)
                P.dma(h_[:], h2tm[row0 + i * 128:row0 + (i + 1) * 128, half * 1024:(half + 1) * 1024])
                for dc in range(8):
                    P.mm(pb[dc][:, :capv], h_[:, dc * 128:(dc + 1) * 128], s_[:, :capv], i == 0, i == ni - 1)
            for dc in range(8):
                if dc % 2 == 0:
                    P.copy(dst[:, half * 8 + dc, :capv], pb[dc][:, :capv])
                else:
                    P.act(dst[:, half * 8 + dc, :capv], pb[dc][:, :capv], AF.Copy)

    for e in range(NE):
        gather(0, keyL, e, NI, cap, xg)
        cks = [(xg, at, cap, G["ysl"])]
        if do_ctx:
            gather(T, keyC, e, NIC, capc, xgc)
            cks.append((xgc, atc, capc, G["ycsl"]))
        wgv = G["w_gate"][l, e].rearrange("(kc p) n -> p kc n", p=128)
        wuv = G["w_up"][l, e].rearrange("(kc p) n -> p kc n", p=128)
        wdv = G["w_down"][l, e].rearrange("(kc p) n -> p kc n", p=128)
        for fg in range(D // WG):
            wg_t = wbuf[(cnt_it[1] * 2) % 4]
            wu_t = wbuf[(cnt_it[1] * 2 + 1) % 4]
            cnt_it[1] += 1
            P.dma(wg_t[:], wgv[:, :, fg * WG:(fg + 1) * WG], q="pool")
            P.dma(wu_t[:], wuv[:, :, fg * WG:(fg + 1) * WG], q="pool")
            for (xs, as_, n, _) in cks:
                for fi in range(WG // 128):
                    f = fg * (WG // 128) + fi
                    pg, pu = pb[(f % 2) * 2], pb[(f % 2) * 2 + 1]
                    for kc in range(KC):
                        P.mm(pg[:, :n], wg_t[:, kc, fi * 128:(fi + 1) * 128], xs[:, kc, :n], kc == 0, kc == KC - 1)
                    for kc in range(KC):
                        P.mm(pu[:, :n], wu_t[:, kc, fi * 128:(fi + 1) * 128], xs[:, kc, :n], kc == 0, kc == KC - 1)
                    s_ = sg[f % 2]
                    P.act(s_[:, :n], pg[:, :n], AF.Silu)
                    P.tt(as_[:, f, :n], s_[:, :n], pu[:, :n], ALU.mult)
        for dg in range(D // WG):
            wd_t = wbuf[cnt_it[2] % 4]
            cnt_it[2] += 1
            P.dma(wd_t[:], wdv[:, :, dg * WG:(dg + 1) * WG], q="pool")
            for (xs, as_, n, ydst) in cks:
                for st in range((n + 127) // 128):
                    sw = min(128, n - st * 128)
                    py = pb[4 + (st % 2)]
                    for fc in range(KC):
                        P.mm(py[:sw, :WG], as_[:, fc, st * 128:st * 128 + sw], wd_t[:, fc, :], fc == 0, fc == KC - 1)
                    y_ = yo[st % 2]
                    P.act(y_[:sw, :WG], py[:sw, :WG], AF.Copy)
                    P.dma(ydst[e, st * 128:st * 128 + sw, dg * WG:(dg + 1) * WG], y_[:sw, :WG])
    P.phase_end(mk)


def emit_E(P, pb, G, l, T, NCX, do_ctx, xo):
    mk = P.phase_begin()
    cap, capc = 2 * T // NE, 2 * NCX // NE
    KL = min(128, cap)
    S = cap // KL
    modfm = G["modfm"]
    sidt = P.sb("E_sid", [128, 4])
    P.dma(sidt[:], G["sid"])
    Yt = P.sb("E_Yt", [KL, NE, S, 1024], BF16)
    if do_ctx:
        Yc = P.sb("E_Yc", [capc, NE, 1024], BF16)
    kgb = [P.sb("E_kgb%d" % i, [128, 2, 512]) for i in range(2)]
    selT = [P.sb("E_selT%d" % i, [128, 512], BF16) for i in range(3)]
    xs = [P.sb("E_xs%d" % i, [128, 512]) for i in range(2)]
    ot = [P.sb("E_ot%d" % i, [128, 512]) for i in range(2)]
    qch = [(c0, n, 0) for c0, n in chunks(T)]
    if do_ctx:
        qch += [(T + c0, n, 1) for c0, n in chunks(NCX)]
    it = 0
    for half in range(2):
        hs = slice(half * 1024, (half + 1) * 1024)
        for e in range(NE):
            P.dma(Yt[:, e, :, :], G["ysl"][e].rearrange("(s p) d -> p s d", p=KL)[:, :, hs], q="sp" if e % 2 == 0 else "pool")
        if do_ctx:
            P.dma(Yc[:], G["ycsl"].rearrange("e s d -> s e d")[:, :, hs])
        for (c0, n, kind) in qch:
            for e in range(NE):
                k_ = kgb[e % 2]
                ns, K = (S, KL) if kind == 0 else (1, capc)
                P.dma(k_[:, 0, :n], G["keyD"][e, c0:c0 + n].partition_broadcast(128))
                P.dma(k_[:, 1, :n], G["gateD"][e, c0:c0 + n].partition_broadcast(128), q="pool")
                for s in range(ns):
                    st = selT[it % 3]
                    it += 1
                    P.stt(st[:K, :n], k_[:K, 0, :n], sidt[:K, s:s + 1], k_[:K, 1, :n], ALU.is_equal, ALU.mult)
                    first = (e == 0 and s == 0)
                    last = (e == NE - 1 and s == ns - 1)
                    for dc in range(8):
                        lhs = Yt[:KL, e, s, dc * 128:(dc + 1) * 128] if kind == 0 else Yc[:, e, dc * 128:(dc + 1) * 128]
                        P.mm(pb[dc][:, :n], lhs, st[:K, :n], first, last)
            for dc in range(8):
                m = half * 8 + dc
                x_ = xs[dc % 2]
                o_ = ot[dc % 2]
                P.dma(x_[:, :n], G["xn"][m * 128:(m + 1) * 128, c0:c0 + n], q="pool")
                P.stt(o_[:, :n], pb[dc][:, :n], modfm[:, l, 80 + m, kind:kind + 1], x_[:, :n], ALU.mult, ALU.add)
                P.dma(xo[m * 128:(m + 1) * 128, c0:c0 + n], o_[:, :n])
    P.phase_end(mk)


_PROG_CACHE = {}


def build_fused(T, NCX):
    NT = T + NCX
    NI, NIC = T // 128, NCX // 128
    cap, capc = 2 * T // NE, 2 * NCX // NE
    P = Prog()
    G = {}
    f32in = dict(xT0=[D, NT], sT=[128, KC, 2], bmod=[128, 2, 96], w_mod=[2, D, 6 * D], cvA=[2, 128, CV_A["n"]],
                 w_in=[2, D, W_IN_EXT], w_uq=[2, 512, 2048], w_uk=[2, 256, 1024], w_uv=[2, 256, 1024],
                 ropec=[64, T], ropes=[64, T], cvl=[2, 128, 88], lru_wa=[2, 2, 8, 128, 128], lru_wx=[2, 2, 8, 128, 128],
                 cvB=[2, 128, CV_B["n"]], w_out=[2, D, D], router=[2, D, NE], ident=[128, 128], ut=[128, 128],
                 iot=[128, 512], sid=[128, 4], w_gate=[2, NE, D, D], w_up=[2, NE, D, D], w_down=[2, NE, D, D])
    for k, shp in f32in.items():
        G[k] = P.din(k, shp)
    out = P.dout("out", [D, T])
    G["hm_d"] = P.dscr("hm_d", [128, KC, NT], BF16, track=True)
    G["xrT"] = P.dscr("xrT", [1024, NT])
    G["ggT"] = P.dscr("ggT", [1024, NT], BF16)
    G["qn"] = P.dscr("qn", [NH, 128, NT], BF16)
    G["qr"] = P.dscr("qr", [NH, 64, NT], BF16)
    G["kn"] = P.dscr("kn", [NH, 128, NT], BF16)
    G["kr"] = P.dscr("kr", [NH, 64, NT], BF16)
    G["v"] = P.dscr("v", [NT, 1024], BF16)
    G["yr"] = P.dscr("yr", [1024, NT], BF16)
    G["xn"] = P.dscr("xn", [D, NT])
    G["h2tm"] = P.dscr("h2tm", [NT, D], BF16)
    G["ysl"] = P.dscr("ysl", [NE, cap, D], BF16)
    G["ycsl"] = P.dscr("ycsl", [NE, capc, D], BF16)
    G["keyD"] = P.dscr("keyD", [NE, NT])
    G["gateD"] = P.dscr("gateD", [NE, NT])
    x1 = P.dscr("x1", [D, NT])
    pb = [P.ps("pb%d" % i, [128, 512]) for i in range(8)]
    G["modfm"] = P.sb("modfm", [128, 2, 96, 2])
    G["aff_sb"] = P.sb("aff_sb", [128, NE, NI + NIC])
    G["key_sb"] = P.sb("key_sb", [128, NE, NI + NIC])
    P.memset(G["key_sb"][:], -1.0)
    emit_M(P, pb, G)
    xT = G["xT0"]
    for l in range(2):
        do_ctx = (l == 0)
        emit_A(P, pb, G, l, xT, T, NCX)
        emit_A2(P, pb, G, l, T, NCX)
        emit_B(P, pb, G, l, xT, T, NCX, do_ctx)
        emit_D(P, pb, G, l, T, NCX, do_ctx)
        emit_E(P, pb, G, l, T, NCX, do_ctx, x1 if l == 0 else out)
        xT = x1
    print("[build_fused] instructions:", P.n_ins)
    return P.finish()


def host_inputs(inp, b):
    x, ctx = inp["x"], inp["ctx"]
    T = x.shape[1]
    m = {}
    m["xT0"] = np.ascontiguousarray(np.concatenate([x[b].T, ctx[b].T], axis=1), dtype=np.float32)
    C2 = np.stack([inp["c"][b], inp["c_ctx"]], axis=0).astype(np.float32)
    m["sT"] = np.ascontiguousarray(C2.T.reshape(KC, 128, 2).transpose(1, 0, 2))
    m["bmod"] = np.ascontiguousarray(np.stack([fm(inp["b_mod"][l]) for l in range(2)], axis=1))
    m["w_mod"] = inp["w_mod"]
    z16 = np.zeros((128, 16), np.float32)
    cvA, cvl, cvB, w_in, w_uq, w_uk, w_uv = [], [], [], [], [], [], []
    for l in range(2):
        qn, kn = inp["q_norm"][l], inp["k_norm"][l]
        cvA.append(np.concatenate([fm(inp["norm1"][l]), z16, z16, z16, z16, fm(inp["q_a_norm"][l]), fm(inp["kv_a_norm"][l]),
                                   pad128(qn[:128]), pad128(qn[128:]), pad128(qn[128 + _SW]),
                                   pad128(kn[:128]), pad128(kn[128:]), pad128(kn[128 + _SW])], axis=1))
        a, b_, c_, d_ = a_weights(inp, l)
        w_in.append(a); w_uq.append(b_); w_uk.append(c_); w_uv.append(d_)
        cw, cb = inp["conv_w"][l], inp["conv_b"][l]
        cols = [cw.reshape(4, 8, 128).transpose(2, 1, 0).reshape(128, 32), cb.reshape(8, 128).T]
        for nm in ("lru_ba", "lru_bx", "lru_lambda"):
            cols.append(inp[nm][l].reshape(2, 8, 128).transpose(2, 0, 1).reshape(128, 16))
        cvl.append(np.concatenate(cols, axis=1))
        cvB.append(np.concatenate([z16, z16, fm(inp["norm2"][l]), z16, z16, z16, z16], axis=1))
    m["cvA"] = np.ascontiguousarray(np.stack(cvA).astype(np.float32))
    m["cvl"] = np.ascontiguousarray(np.stack(cvl).astype(np.float32))
    m["cvB"] = np.ascontiguousarray(np.stack(cvB).astype(np.float32))
    m["w_in"] = np.stack(w_in); m["w_uq"] = np.stack(w_uq); m["w_uk"] = np.stack(w_uk); m["w_uv"] = np.stack(w_uv)
    cos, sin = rope_tables(np.arange(T))
    m["ropec"], m["ropes"] = cos, sin
    m["lru_wa"], m["lru_wx"] = inp["lru_wa"], inp["lru_wx"]
    m["w_out"], m["router"] = inp["w_out"], inp["router"]
    m["ident"] = np.eye(128, dtype=np.float32)
    m["ut"] = np.triu(np.ones((128, 128), np.float32))
    m["iot"] = np.tile(np.arange(512, dtype=np.float32), (128, 1))
    m["sid"] = (np.arange(128, dtype=np.float32)[:, None] + 128.0 * np.arange(4, dtype=np.float32)[None, :]).astype(np.float32)
    m["w_gate"], m["w_up"], m["w_down"] = inp["w_gate"], inp["w_up"], inp["w_down"]
    return {k: np.ascontiguousarray(v, dtype=np.float32) for k, v in m.items()}


def kernel(**inputs):
    inp = {k: np.asarray(v) for k, v in inputs.items()}
    Bn, T, _ = inp["x"].shape
    NCX = inp["ctx"].shape[1]
    key = (T, NCX)
    if key not in _PROG_CACHE:
        _PROG_CACHE[key] = build_fused(T, NCX)
    nc = _PROG_CACHE[key]
    maps = [host_inputs(inp, b) for b in range(Bn)]
    res = run_bass_kernel_spmd(nc, maps, core_ids=list(range(Bn)))
    out = np.stack([np.ascontiguousarray(r["out"].T) for r in res.results])
    return out.astype(np.float32, copy=False)
```

```python
import numpy as np
import ml_dtypes
import concourse.bass as bass
import concourse.mybir as mybir
from concourse.bass_utils import run_bass_kernel_spmd

F32 = mybir.dt.float32
BF16 = mybir.dt.bfloat16
ALU = mybir.AluOpType
AF = mybir.ActivationFunctionType
AX = mybir.AxisListType
NPBF = ml_dtypes.bfloat16

D = 2048
KC = 16
NH = 8
NE = 16
EPS = 1e-6
ATT_SCALE = 192 ** -0.5
NCORES = 8


class Prog:
    ENG = ("pe", "dve", "act", "pool", "sp")

    def __init__(self):
        self.nc = bass.Bass("TRN2", target_bir_lowering=False)
        nc = self.nc
        self.eng = {"pe": nc.tensor, "dve": nc.vector, "act": nc.scalar,
                    "pool": nc.gpsimd, "sp": nc.sync}
        self._ctx = []
        self.csem, self.dsem, self.cnt = {}, {}, {}
        self.NDS = 20
        self.drr = {}
        for e in self.ENG:
            self.csem[e] = self._enter(nc.semaphore("c_" + e))
            self.cnt[("c", e)] = 0
        for e in ("sp", "pool"):
            self.drr[e] = 0
            for i in range(self.NDS):
                self.dsem[(e, i)] = self._enter(nc.semaphore("d_%s%d" % (e, i)))
                self.cnt[("d", e, i)] = 0
        self.waited = {e: {} for e in self.ENG}
        self.lastw, self.readers, self.tags = {}, {}, {}
        self.skip = set()
        self.n_ins = 0
        self.uid = 0

    def _enter(self, cm):
        v = cm.__enter__()
        self._ctx.append(cm)
        return v

    def sb(self, name, shape, dt=F32):
        self.uid += 1
        return self._enter(self.nc.sbuf_tensor("%s_u%d" % (name, self.uid), list(shape), dt))

    def ps(self, name, shape, dt=F32):
        return self._enter(self.nc.psum_tensor(name, list(shape), dt))

    def din(self, name, shape, dt=F32):
        self.skip.add(name)
        return self.nc.dram_tensor(name, list(shape), dt, kind="ExternalInput").ap()

    def dout(self, name, shape, dt=F32):
        self.skip.add(name)
        return self.nc.dram_tensor(name, list(shape), dt, kind="ExternalOutput").ap()

    def dscr(self, name, shape, dt=F32, track=False):
        if not track:
            self.skip.add(name)
        return self.nc.dram_tensor(name, list(shape), dt, kind="Internal").ap()

    @staticmethod
    def _nm(t):
        if isinstance(t, str):
            return t
        if hasattr(t, "tensor"):
            t = t.tensor
        return t.name

    def _norm(self, ks):
        out = []
        for k in ks:
            if k is None or isinstance(k, (int, float)):
                continue
            if isinstance(k, tuple):
                n = self._nm(k[0])
                if n not in self.skip:
                    out.append((n, k[1]))
            else:
                n = self._nm(k)
                if n not in self.skip:
                    out.append((n, None))
        return out

    def _conf(self, key):
        name, tag = key
        if tag is None:
            return [(name, t) for t in self.tags.get(name, ())] + [(name, None)]
        return [(name, tag), (name, None)]

    def _sem(self, sk):
        return self.csem[sk[1]] if sk[0] == "c" else self.dsem[(sk[1], sk[2])]

    def emit(self, e, build, reads=(), writes=(), dma=False):
        reads = self._norm(reads)
        writes = self._norm(writes)
        deps = {}

        def need(d):
            if d is not None and deps.get(d[0], 0) < d[1]:
                deps[d[0]] = d[1]

        for k in reads:
            for c in self._conf(k):
                need(self.lastw.get(c))
        for k in writes:
            for c in self._conf(k):
                need(self.lastw.get(c))
                for r in self.readers.get(c, ()):
                    need(r)
        engine = self.eng[e]
        for sk, v in deps.items():
            if sk == ("c", "pe") and e == "pe" and not dma:
                continue
            if self.waited[e].get(sk, 0) >= v:
                continue
            engine.wait_ge(self._sem(sk), v)
            self.waited[e][sk] = v
        if dma:
            sk = ("d", e, self.drr[e] % self.NDS)
            self.drr[e] += 1
            if self.cnt[sk] > self.waited[e].get(sk, 0):
                engine.wait_ge(self._sem(sk), self.cnt[sk])
                self.waited[e][sk] = self.cnt[sk]
        else:
            sk = ("c", e)
        ins = build(engine)
        self.cnt[sk] += 16 if dma else 1
        ins.then_inc(self._sem(sk), 16 if dma else 1)
        me = (sk, self.cnt[sk])
        for k in writes:
            if k[1] is None:
                for c in self._conf(k):
                    self.lastw.pop(c, None)
                    self.readers.pop(c, None)
            else:
                self.tags.setdefault(k[0], set()).add(k[1])
            self.lastw[k] = me
            self.readers[k] = []
        for k in reads:
            if k[1] is not None:
                self.tags.setdefault(k[0], set()).add(k[1])
            lst = self.readers.setdefault(k, [])
            lst.append(me)
            if len(lst) > 10:
                best = {}
                for s, v in lst:
                    if best.get(s, 0) < v:
                        best[s] = v
                self.readers[k] = list(best.items())
        self.n_ins += 1
        return me

    def dma(self, out, in_, q="sp", rd=None, wr=None, slow=False):
        kw = {"allow_slow_non_contiguous": True} if slow else {}
        return self.emit(q, lambda en: en.dma_start(out=out, in_=in_, **kw),
                         rd if rd is not None else [in_], wr if wr is not None else [out], dma=True)

    def mm(self, out, lhsT, rhs, start, stop):
        return self.emit("pe", lambda en: en.matmul(out, lhsT, rhs, start=start, stop=stop),
                         [lhsT, rhs], [out])

    def act(self, out, in_, func, bias=None, scale=None, e="act"):
        kw = {}
        if bias is not None:
            kw["bias"] = bias
        if scale is not None:
            kw["scale"] = scale
        return self.emit(e, lambda en: en.activation(out, in_, func, **kw), [in_, bias, scale], [out])

    def tt(self, out, a, b, op, e="dve"):
        return self.emit(e, lambda en: en.tensor_tensor(out, a, b, op), [a, b], [out])

    def ts(self, out, a, s1, s2, op0, op1=None, e="dve"):
        if op1 is None:
            return self.emit(e, lambda en: en.tensor_scalar(out, a, s1, None, op0), [a, s1], [out])
        return self.emit(e, lambda en: en.tensor_scalar(out, a, s1, s2, op0, op1), [a, s1, s2], [out])

    def stt(self, out, in0, scalar, in1, op0, op1):
        return self.emit("dve", lambda en: en.scalar_tensor_tensor(out, in0, scalar, in1, op0, op1),
                         [in0, scalar, in1], [out])

    def copy(self, out, in_, e="dve"):
        return self.emit(e, lambda en: en.tensor_copy(out, in_), [in_], [out])

    def memset(self, out, val, e="dve"):
        return self.emit(e, lambda en: en.memset(out, val), [], [out])

    def scan(self, out, d0, d1, init, op0, op1):
        return self.emit("dve", lambda en: en.tensor_tensor_scan(out, d0, d1, init, op0, op1),
                         [d0, d1, init], [out])

    def recip(self, out, in_):
        return self.emit("dve", lambda en: en.reciprocal(out, in_), [in_], [out])

    def rstd(self, out, ss, inv_n, epsb, tmp=None):
        self.act(out, ss, AF.Sqrt, bias=epsb, scale=inv_n)
        self.recip(out, out)

    def barrier(self):
        for e in self.ENG:
            engine = self.eng[e]
            for sk, v in self.cnt.items():
                if v > self.waited[e].get(sk, 0):
                    engine.wait_ge(self._sem(sk), v)
                    self.waited[e][sk] = v
        self.lastw.clear()
        self.readers.clear()
        self.tags.clear()

    def phase_begin(self):
        return len(self._ctx)

    def phase_end(self, mark):
        self.barrier()
        while len(self._ctx) > mark:
            self._ctx.pop().__exit__(None, None, None)

    def finish(self):
        for sk, v in self.cnt.items():
            if v > 0:
                self.eng["sp"].wait_ge(self._sem(sk), v)
        while self._ctx:
            self._ctx.pop().__exit__(None, None, None)
        return self.nc


def fm(v):
    v = np.asarray(v, np.float32)
    return np.ascontiguousarray(v.reshape(-1, 128).T)


def chunks(n, step=512):
    return [(i, min(step, n - i)) for i in range(0, n, step)]


_SW = np.array([f + 16 if (f % 32) < 16 else f - 16 for f in range(64)])


def rope_tables(t_idx):
    t_idx = np.asarray(t_idx)
    row = (t_idx // 64).astype(np.float32)
    col = (t_idx % 64).astype(np.float32)
    inv = (np.float32(10000.0) ** (-np.arange(16, dtype=np.float32) / np.float32(16))).astype(np.float32)
    cos = np.zeros((64, len(t_idx)), np.float32)
    sin = np.zeros((64, len(t_idx)), np.float32)
    for f in range(64):
        pos = row if f < 32 else col
        ang = (pos * inv[f % 16]).astype(np.float32)
        cos[f] = np.cos(ang)
        s = np.sin(ang)
        sin[f] = -s if (f % 32) < 16 else s
    return cos, sin


def pad128(v):
    o = np.zeros((128, 1), np.float32)
    o[:len(v), 0] = v
    return o


def a_weights(inp, l):
    w_in = inp["w_in"][l]
    w_in_ext = np.ascontiguousarray(np.concatenate([w_in, w_in[:, 2816 + _SW]], axis=1))
    wq = inp["w_uq"][l].reshape(512, NH, 192)
    w_uq_ext = np.ascontiguousarray(np.concatenate([wq, wq[:, :, 128 + _SW]], axis=2).reshape(512, NH * 256))
    wkv = inp["w_ukv"][l].reshape(256, NH, 256)
    w_uk = np.ascontiguousarray(wkv[:, :, :128].reshape(256, 1024))
    w_uv = np.ascontiguousarray(wkv[:, :, 128:].reshape(256, 1024))
    return w_in_ext, w_uq_ext, w_uk, w_uv


I32 = mybir.dt.int32
N_BISECT = 34
W_IN_EXT = 2944
CV_A = dict(g1=0, sh_l=16, sc_l=32, sh_c=48, sc_c=64, gqa=80, gkva=84,
            gq_n=86, gq_r=87, gq_s=88, gk_n=89, gk_r=90, gk_s=91, n=92)
CV_B = dict(g1_l=0, g1_c=16, n2=32, sh2_l=48, sc2_l=64, sh2_c=80, sc2_c=96, n=112)


def emit_M(P, pb, G):
    mk = P.phase_begin()
    s2 = P.sb("M_s2", [128, KC, 2])
    bm = P.sb("M_bm", [128, 2, 96])
    wt = [P.sb("M_wt%d" % i, [128, KC, 512]) for i in range(2)]
    P.dma(s2[:], G["sT"])
    P.dma(bm[:], G["bmod"])
    P.act(s2[:], s2[:], AF.Silu)
    i = 0
    for l in range(2):
        wv = G["w_mod"][l].rearrange("(kc p) n -> p kc n", p=128)
        for cg in range(24):
            t = wt[i % 2]
            P.dma(t[:], wv[:, :, cg * 512:(cg + 1) * 512], q="sp" if i % 2 == 0 else "pool")
            i += 1
            for ci in range(4):
                ch = cg * 4 + ci
                p_ = pb[ch % 2]
                for kc in range(KC):
                    P.mm(p_[:, 0:2], t[:, kc, ci * 128:(ci + 1) * 128], s2[:, kc, :], kc == 0, kc == KC - 1)
                P.ts(G["modfm"][:, l, ch, :], p_[:, 0:2], bm[:, l, ch:ch + 1], None, ALU.add)
    P.phase_end(mk)


def emit_A(P, pb, G, l, xT, NLC, NCX):
    NT = NLC + NCX
    mk = P.phase_begin()
    modfm = G["modfm"]
    cvt = P.sb("A_cvt", [128, CV_A["n"]])
    P.dma(cvt[:], G["cvA"][l])
    P.copy(cvt[:, 16:32], modfm[:, l, 0:16, 0])
    P.copy(cvt[:, 32:48], modfm[:, l, 16:32, 0])
    P.copy(cvt[:, 48:64], modfm[:, l, 0:16, 1])
    P.copy(cvt[:, 64:80], modfm[:, l, 16:32, 1])
    C = lambda name, j=0, w=1: cvt[:, CV_A[name] + j: CV_A[name] + j + w]
    epsb = P.sb("A_epsb", [128, 1])
    P.memset(epsb[:], EPS)
    ones = P.sb("A_ones", [128, 128], BF16)
    P.memset(ones[:], 1.0)
    Am = P.sb("A_Am", [128, 2, KC])
    for k, nm in enumerate(("sc_l", "sc_c")):
        P.ts(Am[:, k, :], C(nm, 0, KC), 1.0, None, ALU.add)
        P.tt(Am[:, k, :], Am[:, k, :], C("g1", 0, KC), ALU.mult)
    rc_t = P.sb("A_rc", [64, 512])
    rs_t = P.sb("A_rs", [64, 512])
    tch = [(c0, n, 0) for c0, n in chunks(NLC)] + [(NLC + c0, n, 1) for c0, n in chunks(NCX)]
    hm_d = G["hm_d"]
    xt = P.sb("A_xt", [128, KC, 512])
    sq = [P.sb("A_sq%d" % i, [128, 512]) for i in range(2)]
    sqb = [P.sb("A_sqb%d" % i, [128, 512], BF16) for i in range(2)]
    rs = P.sb("A_rsd", [128, 512])
    hmc = [P.sb("A_hmc%d" % i, [128, KC, 512], BF16) for i in range(3)]
    hm = hmc[0]
    xv = xT.rearrange("(kc p) n -> p kc n", p=128)
    for (c0, n, kind) in tch:
        P.dma(xt[:, :, :n], xv[:, :, c0:c0 + n], q="pool")
        for kc in range(KC):
            s_ = sqb[kc % 2]
            P.act(s_[:, :n], xt[:, kc, :n], AF.Square)
            P.mm(pb[0][:, :n], ones[:], s_[:, :n], kc == 0, kc == KC - 1)
        P.rstd(rs[:, :n], pb[0][:, :n], 1.0 / D, epsb[:])
        shn = "sh_l" if kind == 0 else "sh_c"
        for kc in range(KC):
            s_ = sq[kc % 2]
            P.tt(s_[:, :n], xt[:, kc, :n], rs[:, :n], ALU.mult)
            P.ts(hm[:, kc, :n], s_[:, :n], Am[:, kind, kc:kc + 1], C(shn, kc), ALU.mult, ALU.add)
        P.dma(hm_d[:, :, c0:c0 + n], hm[:, :, :n], wr=[(hm_d, c0)])

    wg = [P.sb("A_wg%d" % i, [128, KC, 512], BF16) for i in range(2)]
    wuq = P.sb("A_wuq", [128, 4, 2048], BF16)
    wuk = P.sb("A_wuk", [128, 2, 1024], BF16)
    wuv = P.sb("A_wuv", [128, 2, 1024], BF16)
    P.dma(wuq[:], G["w_uq"][l].rearrange("(kc p) n -> p kc n", p=128), q="pool")
    P.dma(wuk[:], G["w_uk"][l].rearrange("(kc p) n -> p kc n", p=128), q="pool")
    P.dma(wuv[:], G["w_uv"][l].rearrange("(kc p) n -> p kc n", p=128), q="pool")
    wv = G["w_in"][l].rearrange("(kc p) n -> p kc n", p=128)
    ev = [P.sb("A_ev%d" % i, [128, 512]) for i in range(2)]
    evb = [P.sb("A_evb%d" % i, [128, 512], BF16) for i in range(2)]
    cqt = P.sb("A_cqt", [128, 4, 512])
    cqn = P.sb("A_cqn", [128, 4, 512], BF16)
    ckt = P.sb("A_ckt", [128, 2, 512])
    ckn = P.sb("A_ckn", [128, 2, 512], BF16)
    krt = P.sb("A_krt", [64, 512])
    kst = P.sb("A_kst", [64, 512])
    krsq = P.sb("A_krsq", [64, 512], BF16)
    Rt = P.sb("A_Rt", [64, 512])
    sqn2 = [P.sb("A_sqn%d" % i, [128, 512], BF16) for i in range(2)]
    sqr2 = [P.sb("A_sqr%d" % i, [64, 512], BF16) for i in range(2)]
    rsh2 = [P.sb("A_rsh%d" % i, [128, 512]) for i in range(2)]
    t64a = P.sb("A_t64a", [64, 512])
    t64b = P.sb("A_t64b", [64, 512])
    ob64 = [P.sb("A_ob64_%d" % i, [64, 512], BF16) for i in range(2)]
    vb = [P.sb("A_vb%d" % i, [128, 1024], BF16) for i in range(2)]
    xrT, ggT, qn_o, qr_o, kn_o, kr_o, v_o = G["xrT"], G["ggT"], G["qn"], G["qr"], G["kn"], G["kr"], G["v"]

    def rope_mix(out_bf, a_f, b_f, n, kind):
        if kind == 0:
            P.tt(a_f, a_f, rc_t[:, :n], ALU.mult)
            P.tt(b_f, b_f, rs_t[:, :n], ALU.mult)
            P.tt(out_bf, a_f, b_f, ALU.add)
        else:
            P.copy(out_bf, a_f)

    groups = chunks(W_IN_EXT)
    it = 0
    for gi, (g0, gn) in enumerate(groups):
        w_ = wg[gi % 2]
        P.dma(w_[:, :, :gn], wv[:, :, g0:g0 + gn], q="pool")
        for (c0, n, kind) in tch:
            h_ = hmc[it % 3]
            it += 1
            P.dma(h_[:, :, :n], hm_d[:, :, c0:c0 + n], rd=[(hm_d, c0)], q="pool")
            if kind == 0 and g0 >= 2048:
                P.dma(rc_t[:, :n], G["ropec"][:, c0:c0 + n], q="pool")
                P.dma(rs_t[:, :n], G["ropes"][:, c0:c0 + n], q="pool")
            nm = (gn + 127) // 128
            for mi in range(nm):
                col = g0 + mi * 128
                mw = min(128, gn - mi * 128)
                p_ = pb[1 + (mi % 4)]
                if col < 2816:
                    for kc in range(KC):
                        P.mm(p_[:mw, :n], w_[:, kc, mi * 128: mi * 128 + mw], h_[:, kc, :n], kc == 0, kc == KC - 1)
                m = col // 128
                if m < 8:
                    e_ = ev[m % 2]
                    P.act(e_[:, :n], p_[:, :n], AF.Copy)
                    P.dma(xrT[m * 128:(m + 1) * 128, c0:c0 + n], e_[:, :n])
                elif m < 16:
                    e_ = evb[m % 2]
                    P.act(e_[:, :n], p_[:, :n], AF.Gelu_apprx_tanh)
                    P.dma(ggT[(m - 8) * 128:(m - 7) * 128, c0:c0 + n], e_[:, :n])
                elif m < 20:
                    P.copy(cqt[:, m - 16, :n], p_[:, :n])
                elif m < 22:
                    P.copy(ckt[:, m - 20, :n], p_[:, :n])
                else:
                    for kc in range(KC):
                        P.mm(pb[5][:64, :n], w_[:, kc, mi * 128: mi * 128 + 64], h_[:, kc, :n], kc == 0, kc == KC - 1)
                    for kc in range(KC):
                        P.mm(pb[6][:64, :n], w_[:, kc, mi * 128 + 64: mi * 128 + 128], h_[:, kc, :n], kc == 0, kc == KC - 1)
                    P.copy(krt[:, :n], pb[5][:64, :n])
                    P.copy(kst[:, :n], pb[6][:64, :n])
            if g0 == 2048:
                for kc in range(4):
                    s_ = sqb[kc % 2]
                    P.act(s_[:, :n], cqt[:, kc, :n], AF.Square)
                    P.mm(pb[0][:, :n], ones[:], s_[:, :n], kc == 0, kc == 3)
                P.rstd(rs[:, :n], pb[0][:, :n], 1.0 / 512, epsb[:])
                for kc in range(4):
                    P.stt(cqn[:, kc, :n], cqt[:, kc, :n], C("gqa", kc), rs[:, :n], ALU.mult, ALU.mult)
                def qproj(h):
                    pn, pr, pS = pb[1 + (h % 2) * 3], pb[2 + (h % 2) * 3], pb[3 + (h % 2) * 3]
                    b0 = h * 256
                    for kc in range(4):
                        P.mm(pn[:, :n], wuq[:, kc, b0:b0 + 128], cqn[:, kc, :n], kc == 0, kc == 3)
                    for kc in range(4):
                        P.mm(pr[:64, :n], wuq[:, kc, b0 + 128:b0 + 192], cqn[:, kc, :n], kc == 0, kc == 3)
                    for kc in range(4):
                        P.mm(pS[:64, :n], wuq[:, kc, b0 + 192:b0 + 256], cqn[:, kc, :n], kc == 0, kc == 3)

                qproj(0)
                for h in range(NH):
                    pn, pr, pS = pb[1 + (h % 2) * 3], pb[2 + (h % 2) * 3], pb[3 + (h % 2) * 3]
                    sqn_, sqr_, rsh_ = sqn2[h % 2], sqr2[h % 2], rsh2[h % 2]
                    P.act(sqn_[:, :n], pn[:, :n], AF.Square)
                    P.act(sqr_[:, :n], pr[:64, :n], AF.Square)
                    if h + 1 < NH:
                        qproj(h + 1)
                    P.mm(pb[7][:, :n], ones[:], sqn_[:, :n], True, False)
                    P.mm(pb[7][:, :n], ones[:64, :], sqr_[:, :n], False, True)
                    P.rstd(rsh_[:, :n], pb[7][:, :n], 1.0 / 192, epsb[:])
                    o_ = evb[h % 2]
                    P.stt(o_[:, :n], pn[:, :n], C("gq_n"), rsh_[:, :n], ALU.mult, ALU.mult)
                    P.dma(qn_o[h, :, c0:c0 + n], o_[:, :n])
                    P.stt(t64a[:, :n], pr[:64, :n], cvt[:64, CV_A["gq_r"]:CV_A["gq_r"] + 1], rsh_[:64, :n], ALU.mult, ALU.mult)
                    P.stt(t64b[:, :n], pS[:64, :n], cvt[:64, CV_A["gq_s"]:CV_A["gq_s"] + 1], rsh_[:64, :n], ALU.mult, ALU.mult)
                    o6 = ob64[h % 2]
                    rope_mix(o6[:, :n], t64a[:, :n], t64b[:, :n], n, kind)
                    P.dma(qr_o[h, :, c0:c0 + n], o6[:, :n])
            if g0 == 2560:
                for kc in range(2):
                    s_ = sqb[kc % 2]
                    P.act(s_[:, :n], ckt[:, kc, :n], AF.Square)
                    P.mm(pb[0][:, :n], ones[:], s_[:, :n], kc == 0, kc == 1)
                P.rstd(rs[:, :n], pb[0][:, :n], 1.0 / 256, epsb[:])
                for kc in range(2):
                    P.stt(ckn[:, kc, :n], ckt[:, kc, :n], C("gkva", kc), rs[:, :n], ALU.mult, ALU.mult)
                P.act(krsq[:, :n], krt[:, :n], AF.Square)
                P.ts(t64a[:, :n], krt[:, :n], cvt[:64, CV_A["gk_r"]:CV_A["gk_r"] + 1], None, ALU.mult)
                P.ts(t64b[:, :n], kst[:, :n], cvt[:64, CV_A["gk_s"]:CV_A["gk_s"] + 1], None, ALU.mult)
                if kind == 0:
                    P.tt(t64a[:, :n], t64a[:, :n], rc_t[:, :n], ALU.mult)
                    P.tt(t64b[:, :n], t64b[:, :n], rs_t[:, :n], ALU.mult)
                    P.tt(Rt[:, :n], t64a[:, :n], t64b[:, :n], ALU.add)
                else:
                    P.copy(Rt[:, :n], t64a[:, :n])
                def kproj(h):
                    pn = pb[1 + (h % 2)]
                    for kc in range(2):
                        P.mm(pn[:, :n], wuk[:, kc, h * 128:(h + 1) * 128], ckn[:, kc, :n], kc == 0, kc == 1)

                kproj(0)
                for h in range(NH):
                    pn = pb[1 + (h % 2)]
                    sqn_, rsh_ = sqn2[h % 2], rsh2[h % 2]
                    P.act(sqn_[:, :n], pn[:, :n], AF.Square)
                    if h + 1 < NH:
                        kproj(h + 1)
                    P.mm(pb[7][:, :n], ones[:], sqn_[:, :n], True, False)
                    P.mm(pb[7][:, :n], ones[:64, :], krsq[:, :n], False, True)
                    P.rstd(rsh_[:, :n], pb[7][:, :n], 1.0 / 192, epsb[:])
                    o_ = evb[h % 2]
                    P.stt(o_[:, :n], pn[:, :n], C("gk_n"), rsh_[:, :n], ALU.mult, ALU.mult)
                    P.dma(kn_o[h, :, c0:c0 + n], o_[:, :n])
                    o6 = ob64[h % 2]
                    P.tt(o6[:, :n], Rt[:, :n], rsh_[:64, :n], ALU.mult)
                    P.dma(kr_o[h, :, c0:c0 + n], o6[:, :n])
                for j in range(n // 128):
                    vt = vb[j % 2]
                    for hh in range(2):
                        pv = pb[3 + hh]
                        for kc in range(2):
                            P.mm(pv[:, :], ckn[:, kc, j * 128:(j + 1) * 128], wuv[:, kc, hh * 512:(hh + 1) * 512], kc == 0, kc == 1)
                        P.act(vt[:, hh * 512:(hh + 1) * 512], pv[:, :], AF.Copy)
                    P.dma(v_o[c0 + j * 128: c0 + (j + 1) * 128, :], vt[:])
    P.phase_end(mk)


def emit_A2(P, pb, G, l, T, NCX):
    mk = P.phase_begin()
    xr, gg, yr = G["xrT"], G["ggT"], G["yr"]
    cvt = P.sb("L_cvt", [128, 88])
    P.dma(cvt[:], G["cvl"][l])
    one = P.sb("L_one", [128, 1])
    P.memset(one[:], 1.0)
    cl = P.sb("L_cl", [128, 16])
    P.act(cl[:], cvt[:, 72:88], AF.Exp, scale=-1.0)
    P.act(cl[:], cl[:], AF.Ln, bias=one[:])
    P.ts(cl[:], cl[:], -8.0, None, ALU.mult)
    wab = P.sb("L_wab", [128, 2, 8, 128], BF16)
    wxb = P.sb("L_wxb", [128, 2, 8, 128], BF16)
    for d in range(2):
        P.dma(wab[:, d, :, :], G["lru_wa"][l, d].rearrange("g i j -> i g j"), q="pool")
        P.dma(wxb[:, d, :, :], G["lru_wx"][l, d].rearrange("g i j -> i g j"), q="pool")
    streams = [("C", T, NCX), ("L", 0, T)]
    tl = {}
    for s, _, n in streams:
        X = P.sb("L_X" + s, [128, n + 3])
        tl[s] = dict(X=X, u=P.sb("L_u" + s, [128, n]), ub=P.sb("L_ub" + s, [128, n], BF16),
                     ra=[P.sb("L_ra%d" % d + s, [128, n]) for d in range(2)],
                     ii=[P.sb("L_ii%d" % d + s, [128, n]) for d in range(2)],
                     sb=[P.sb("L_sb%d" % d + s, [128, n]) for d in range(2)],
                     hf=P.sb("L_hf" + s, [128, n]),
                     gg=P.sb("L_gg" + s, [128, n], BF16), yo=P.sb("L_yo" + s, [128, n], BF16))
    for g in range(8):
        rows = slice(g * 128, (g + 1) * 128)
        for s, s0, n in streams:
            t = tl[s]
            P.memset(t["X"][:, 0:2], 0.0)
            P.memset(t["X"][:, n + 2:n + 3], 0.0)
            P.dma(t["X"][:, 2:n + 2], xr[rows, s0:s0 + n])
            P.dma(t["gg"][:], gg[rows, s0:s0 + n])
            P.ts(t["u"][:], t["X"][:, 0:n], cvt[:, g * 4:g * 4 + 1], cvt[:, 32 + g:33 + g], ALU.mult, ALU.add)
            for k in range(1, 4):
                P.stt(t["u"][:], t["X"][:, k:k + n], cvt[:, g * 4 + k:g * 4 + k + 1], t["u"][:], ALU.mult, ALU.add)
            P.act(t["ub"][:], t["u"][:], AF.Copy)
        for d in range(2):
            for s, s0, n in streams:
                t = tl[s]
                ra, ii, sb = t["ra"][d], t["ii"][d], t["sb"][d]
                for ci, (c0, cn) in enumerate(chunks(n)):
                    pr, pi = pb[(ci % 2) * 2], pb[(ci % 2) * 2 + 1]
                    P.mm(pr[:, :cn], wab[:, d, g, :], t["ub"][:, c0:c0 + cn], True, True)
                    P.mm(pi[:, :cn], wxb[:, d, g, :], t["ub"][:, c0:c0 + cn], True, True)
                    P.act(ra[:, c0:c0 + cn], pr[:, :cn], AF.Sigmoid, bias=cvt[:, 40 + d * 8 + g:41 + d * 8 + g])
                    P.act(ii[:, c0:c0 + cn], pi[:, :cn], AF.Sigmoid, bias=cvt[:, 56 + d * 8 + g:57 + d * 8 + g])
                P.act(ra[:], ra[:], AF.Exp, scale=cl[:, d * 8 + g:d * 8 + g + 1])
                P.act(sb[:], ra[:], AF.Square)
                P.act(sb[:], sb[:], AF.Sqrt, bias=one[:], scale=-1.0)
        for d in range(2):
            for s, s0, n in streams:
                t = tl[s]
                ra, ii, sb = t["ra"][d], t["ii"][d], t["sb"][d]
                P.tt(ii[:], ii[:], t["u"][:], ALU.mult)
                P.tt(sb[:], sb[:], ii[:], ALU.mult)
                if d == 0:
                    init = 0.0 if s == "C" else tl["C"]["hf"][:, NCX - 1:NCX]
                    P.scan(t["hf"][:], ra[:], sb[:], init, ALU.mult, ALU.add)
                else:
                    hb = t["X"][:, 0:n]
                    init = 0.0 if s == "C" else tl["C"]["X"][:, 0:1]
                    P.scan(hb[:, ::-1], ra[:, ::-1], sb[:, ::-1], init, ALU.mult, ALU.add)
        for s, s0, n in streams:
            t = tl[s]
            P.tt(t["hf"][:], t["hf"][:], t["X"][:, 0:n], ALU.add)
            P.tt(t["yo"][:], t["hf"][:], t["gg"][:], ALU.mult)
            P.dma(yr[rows, s0:s0 + n], t["yo"][:])
    P.phase_end(mk)


def emit_B(P, pb, G, l, xT, T, NCX, do_ctx):
    NT = T + NCX
    NKT = NT // 128
    NI = T // 128
    modfm = G["modfm"]
    aff_sb = G["aff_sb"]
    HQ = min(2048, T)
    for qp in range(T // HQ):
        mk = P.phase_begin()
        qch = [(qp * HQ + c0, n, 0, c0) for c0, n in chunks(HQ)]
        NQP = HQ
        if do_ctx and qp == 0:
            qch += [(T + c0, n, 1, HQ + c0) for c0, n in chunks(NCX)]
            NQP = HQ + NCX
        cvt = P.sb("B_cvt", [128, CV_B["n"]])
        P.dma(cvt[:], G["cvB"][l])
        P.copy(cvt[:, 0:16], modfm[:, l, 32:48, 0])
        P.copy(cvt[:, 16:32], modfm[:, l, 32:48, 1])
        P.copy(cvt[:, 48:64], modfm[:, l, 48:64, 0])
        P.copy(cvt[:, 64:80], modfm[:, l, 64:80, 0])
        P.copy(cvt[:, 80:96], modfm[:, l, 48:64, 1])
        P.copy(cvt[:, 96:112], modfm[:, l, 64:80, 1])
        C = lambda name, j=0, w=1: cvt[:, CV_B[name] + j: CV_B[name] + j + w]
        epsb = P.sb("B_epsb", [128, 1])
        P.memset(epsb[:], EPS)
        ones = P.sb("B_ones", [128, 128])
        P.memset(ones[:], 1.0)
        onesb = P.sb("B_onesb", [128, 128], BF16)
        P.memset(onesb[:], 1.0)
        ident = P.sb("B_ident", [128, 128])
        P.dma(ident[:], G["ident"])
        A2m = P.sb("B_A2m", [128, 2, KC])
        for k, nm in enumerate(("sc2_l", "sc2_c")):
            P.ts(A2m[:, k, :], C(nm, 0, KC), 1.0, None, ALU.add)
            P.tt(A2m[:, k, :], A2m[:, k, :], C("n2", 0, KC), ALU.mult)
        rt = P.sb("B_rt", [128, KC, NE])
        P.dma(rt[:], G["router"][l].rearrange("(kc p) e -> p kc e", p=128))

        yatt = P.sb("B_yatt", [128, 8, NQP], BF16)
        yrv = G["yr"].rearrange("(c p) n -> p c n", p=128)
        kvq = [dict(knt=P.sb("B_knt%d" % i, [128, NT], BF16), krt=P.sb("B_krt%d" % i, [64, NT], BF16),
                    vt=P.sb("B_vt%d" % i, [128, NKT, 128], BF16), qnt=P.sb("B_qnt%d" % i, [128, NQP], BF16),
                    qrt=P.sb("B_qrt%d" % i, [64, NQP], BF16)) for i in range(2)]
        pt = [P.sb("B_pt%d" % i, [128, 512], BF16) for i in range(3)]
        rec = P.sb("B_rec", [128, 512])
        it = 0
        for h in range(NH):
            kq = kvq[h % 2]
            knt, krt, vt, qnt, qrt = kq["knt"], kq["krt"], kq["vt"], kq["qnt"], kq["qrt"]
            P.dma(knt[:], G["kn"][h])
            P.dma(krt[:], G["kr"][h])
            P.dma(vt[:], G["v"][:, h * 128:(h + 1) * 128].rearrange("(j p) d -> p j d", p=128))
            P.dma(qnt[:, 0:HQ], G["qn"][h, :, qp * HQ:(qp + 1) * HQ])
            P.dma(qrt[:, 0:HQ], G["qr"][h, :, qp * HQ:(qp + 1) * HQ])
            if NQP > HQ:
                P.dma(qnt[:, HQ:NQP], G["qn"][h, :, T:NT])
                P.dma(qrt[:, HQ:NQP], G["qr"][h, :, T:NT])
            for (ca, n, kind, c0) in qch:
                kts = list(range(NKT)) if kind == 0 else list(range(NI, NKT))
                SB = (pb[0], pb[1], pb[4], pb[5])

                def qk(ji):
                    S = SB[ji % 4]
                    j = kts[ji]
                    P.mm(S[:, :n], knt[:, j * 128:(j + 1) * 128], qnt[:, c0:c0 + n], True, False)
                    P.mm(S[:, :n], krt[:, j * 128:(j + 1) * 128], qrt[:, c0:c0 + n], False, True)

                for ji in range(min(2, len(kts))):
                    qk(ji)
                for ji, j in enumerate(kts):
                    if ji + 2 < len(kts):
                        qk(ji + 2)
                    p_ = pt[ji % 3]
                    P.act(p_[:, :n], SB[ji % 4][:, :n], AF.Exp, scale=ATT_SCALE)
                    P.mm(pb[2][:, :n], vt[:, j, :], p_[:, :n], ji == 0, ji == len(kts) - 1)
                    P.mm(pb[3][:, :n], onesb[:], p_[:, :n], ji == 0, ji == len(kts) - 1)
                P.recip(rec[:, :n], pb[3][:, :n])
                P.tt(yatt[:, h, c0:c0 + n], pb[2][:, :n], rec[:, :n], ALU.mult)

        xc = P.sb("B_xc", [128, KC, 512])
        yrc = P.sb("B_yrc", [128, 8, 512], BF16)
        wob = [P.sb("B_wob%d" % i, [128, KC, 256], BF16) for i in range(2)]
        sq = [P.sb("B_sq%d" % i, [128, 512], BF16) for i in range(2)]
        htm = [P.sb("B_htm%d" % i, [128, D], BF16) for i in range(4)]
        rs = P.sb("B_rs", [128, 512])
        mx = P.sb("B_mx", [128, 1])
        sm = P.sb("B_sm", [128, 1])
        ex = P.sb("B_ex", [128, NE])
        xv = xT.rearrange("(kc p) n -> p kc n", p=128)
        xnv = G["xn"].rearrange("(kc p) n -> p kc n", p=128)
        wov = G["w_out"][l].rearrange("(kc p) n -> p kc n", p=128)
        it = 0
        for (ca, n, kind, c0) in qch:
            nj = n // 128
            P.dma(xc[:, :, :n], xv[:, :, ca:ca + n], q="pool")
            P.dma(yrc[:, :, :n], yrv[:, :, ca:ca + n], q="pool")
            g1n = "g1_l" if kind == 0 else "g1_c"
            shn = "sh2_l" if kind == 0 else "sh2_c"
            for mg in range(8):
                wo = wob[it % 2]
                it += 1
                P.dma(wo[:], wov[:, :, mg * 256:(mg + 1) * 256], q="pool")
                for mi in range(2):
                    m = mg * 2 + mi
                    p_ = pb[m % 2]
                    for kc in range(KC):
                        rhs_ = yrc[:, kc, :n] if kc < 8 else yatt[:, kc - 8, c0:c0 + n]
                        P.mm(p_[:, :n], wo[:, kc, mi * 128:(mi + 1) * 128], rhs_, kc == 0, kc == KC - 1)
                    P.stt(xc[:, m, :n], p_[:, :n], C(g1n, m), xc[:, m, :n], ALU.mult, ALU.add)
            P.dma(xnv[:, :, ca:ca + n], xc[:, :, :n])
            for m in range(KC):
                P.act(sq[m % 2][:, :n], xc[:, m, :n], AF.Square)
                P.mm(pb[2][:, :n], onesb[:], sq[m % 2][:, :n], m == 0, m == KC - 1)
            P.rstd(rs[:, :n], pb[2][:, :n], 1.0 / D, epsb[:])
            for m in range(KC):
                P.tt(xc[:, m, :n], xc[:, m, :n], rs[:, :n], ALU.mult)
                P.ts(xc[:, m, :n], xc[:, m, :n], A2m[:, kind, m:m + 1], C(shn, m), ALU.mult, ALU.add)
                for j in range(nj):
                    P.emit("pe", lambda en, j=j, m=m: en.transpose(pb[4 + j][:, (m % 4) * 128:(m % 4 + 1) * 128],
                                                                 xc[:, m, j * 128:(j + 1) * 128], ident[:]),
                           [xc, ident], [pb[4 + j]])
                if m % 4 == 3:
                    for j in range(nj):
                        dst = htm[j][:, (m // 4) * 512:(m // 4 + 1) * 512]
                        if j % 2 == 0:
                            P.copy(dst, pb[4 + j][:, :])
                        else:
                            P.act(dst, pb[4 + j][:, :], AF.Copy)
            for j in range(nj):
                P.dma(G["h2tm"][ca + j * 128:ca + (j + 1) * 128, :], htm[j][:])
                for m in range(KC):
                    P.mm(pb[j][:, :NE], xc[:, m, j * 128:(j + 1) * 128], rt[:, m, :], m == 0, m == KC - 1)
                lg = pb[j][:, :NE]
                ti = (ca // 128) + j
                P.emit("dve", lambda en, lg=lg: en.tensor_reduce(mx[:], lg, AX.X, ALU.max), [lg], [mx])
                P.ts(mx[:], mx[:], -1.0, None, ALU.mult)
                P.act(ex[:], lg, AF.Exp, bias=mx[:])
                P.emit("dve", lambda en: en.tensor_reduce(sm[:], ex[:], AX.X, ALU.add), [ex], [sm])
                P.recip(sm[:], sm[:])
                P.ts(aff_sb[:, :, ti], ex[:], sm[:, 0:1], None, ALU.mult)
        P.phase_end(mk)


def emit_D(P, pb, G, l, T, NCX, do_ctx):
    mk = P.phase_begin()
    NI, NIC = T // 128, NCX // 128
    cap, capc = 2 * T // NE, 2 * NCX // NE
    NU = NE
    aff_sb = G["aff_sb"]
    ones = P.sb("D_ones", [128, 128])
    P.memset(ones[:], 1.0)
    onesb = P.sb("D_onesb", [128, 128], BF16)
    P.memset(onesb[:], 1.0)
    utf = P.sb("D_utf", [128, 128])
    P.dma(utf[:], G["ut"])
    utb = P.sb("D_utb", [128, 128], BF16)
    P.copy(utb[:], utf[:])
    iott = P.sb("D_iott", [128, 512])
    P.dma(iott[:], G["iot"])
    zer = P.sb("D_zer", [128, 64])
    P.memset(zer[:], 0.0)

    key_sb = G["key_sb"]
    streams = [("L", aff_sb[:, :, 0:NI], key_sb[:, :, 0:NI], NI, cap)]
    if do_ctx:
        streams.append(("C", aff_sb[:, :, NI:NI + NIC], key_sb[:, :, NI:NI + NIC], NIC, capc))
    NS = len(streams)
    lo = P.sb("D_lo", [128, NS, NU])
    mid = P.sb("D_mid", [128, NS, NU])
    cnt = P.sb("D_cnt", [128, NS, NU])
    capt = P.sb("D_capt", [128, NS, NU])
    ge = P.sb("D_ge", [128, NS, NU], I32)
    cms = [P.sb("D_cm" + tg, [128, NU, ni]) for (tg, _, _, ni, _) in streams]
    P.memset(lo[:], 0.0)
    for si, (_, _, _, _, cv_) in enumerate(streams):
        P.memset(capt[:, si, :], float(cv_))
    lof = lo[:].rearrange("p s u -> p (s u)")
    midf = mid[:].rearrange("p s u -> p (s u)")
    for k in range(N_BISECT):
        P.ts(midf, lof, float(2.0 ** -(k + 1)), None, ALU.add)
        for si, (_, af, _, ni, _) in enumerate(streams):
            P.tt(cms[si][:], af, mid[:, si, :].unsqueeze(2).to_broadcast([128, NU, ni]), ALU.is_ge)
            P.emit("dve", lambda en, si=si: en.tensor_reduce(cnt[:, si, :], cms[si][:], AX.X, ALU.add), [cms[si]], [cnt])
        P.mm(pb[0][:, :NS * NU], ones[:], cnt[:].rearrange("p s u -> p (s u)"), True, True)
        P.tt(ge[:].rearrange("p s u -> p (s u)"), pb[0][:, :NS * NU], capt[:].rearrange("p s u -> p (s u)"), ALU.is_ge)
        P.emit("dve", lambda en: en.copy_predicated(lof, ge[:].rearrange("p s u -> p (s u)"), midf), [ge, mid, lo], [lo])
    for si, (tag, af, pos, ni, _) in enumerate(streams):
        n3 = [128, NU, ni]
        cm = cms[si]
        P.tt(cm[:], af, lo[:, si, :].unsqueeze(2).to_broadcast(n3), ALU.is_ge)
        mb = P.sb("D_mb" + tag, [128, NU * ni], BF16)
        P.copy(mb[:], cm[:].rearrange("p u i -> p (u i)"))
        tot = P.sb("D_tot" + tag, n3)
        inc = P.sb("D_inc" + tag, n3)
        wit = P.sb("D_wit" + tag, n3)
        for c0, cn in chunks(NU * ni):
            P.mm(pb[1][:, :cn], utb[:], mb[:, c0:c0 + cn], True, True)
            P.mm(pb[2][:, :cn], onesb[:], mb[:, c0:c0 + cn], True, True)
            P.copy(wit[:].rearrange("p u i -> p (u i)")[:, c0:c0 + cn], pb[1][:, :cn])
            P.copy(tot[:].rearrange("p u i -> p (u i)")[:, c0:c0 + cn], pb[2][:, :cn])
        for u in range(NU):
            P.scan(inc[:, u, :], tot[:, u, :], zer[:, :ni], 0.0, ALU.add, ALU.add)
        P.tt(inc[:], inc[:], tot[:], ALU.subtract)
        P.tt(pos, inc[:], wit[:], ALU.add)
        P.tt(pos, pos, cm[:], ALU.mult)
        P.ts(pos, pos, -1.0, None, ALU.add)
    keyL, keyC = key_sb[:, :, 0:NI], key_sb[:, :, NI:NI + NIC]
    NT = T + NCX

    identf = P.sb("D_ident", [128, 128])
    P.dma(identf[:], G["ident"])
    trT = [P.sb("D_trT%d" % i, [128, 128]) for i in range(2)]
    ncol = NI + NIC
    ti = 0
    for src, dstD in ((key_sb, G["keyD"]), (aff_sb, G["gateD"])):
        flat = src[:].rearrange("p e i -> p (e i)")
        dst2 = dstD.rearrange("e (i p) -> (e i) p", p=128)
        for c0 in range(0, NE * ncol, 128):
            w = min(128, NE * ncol - c0)
            t_ = trT[ti % 2]
            ti += 1
            P.emit("pe", lambda en, c0=c0, w=w, flat=flat: en.transpose(pb[1][:w, :128], flat[:, c0:c0 + w], identf[:]),
                   [src, identf], [pb[1]])
            P.copy(t_[:w, :], pb[1][:w, :128])
            P.dma(dst2[c0:c0 + w, :], t_[:w, :])

    xg = P.sb("D_xg", [128, KC, cap], BF16)
    at = P.sb("D_at", [128, KC, cap], BF16)
    if do_ctx:
        xgc = P.sb("D_xgc", [128, KC, capc], BF16)
        atc = P.sb("D_atc", [128, KC, capc], BF16)
    selr = [P.sb("D_sel%d" % i, [128, 512], BF16) for i in range(3)]
    h2t = [P.sb("D_h2t%d" % i, [128, 1024], BF16) for i in range(3)]
    WG = 512
    wbuf = [P.sb("D_wbuf%d" % i, [128, KC, WG], BF16) for i in range(4)]
    sg = [P.sb("D_sg%d" % i, [128, 512]) for i in range(2)]
    yo = [P.sb("D_yo%d" % i, [128, 512], BF16) for i in range(2)]
    cnt_it = [0, 0, 0]
    h2tm = G["h2tm"]

    def gather(row0, key_t, u, ni, capv, dst):
        for half in range(2):
            for i in range(ni):
                s_ = selr[cnt_it[0] % 3]
                h_ = h2t[cnt_it[0] % 3]
                cnt_it[0] += 1
                P.ts(s_[:, :capv], iott[:, :capv], key_t[:, u, i:i + 1], None, ALU.is_equal)
                P.dma(h_[:], h2tm[row0 + i * 128:row0 + (i + 1) * 128, half * 1024:(half + 1) * 1024])
                for dc in range(8):
                    P.mm(pb[dc][:, :capv], h_[:, dc * 128:(dc + 1) * 128], s_[:, :capv], i == 0, i == ni - 1)
            for dc in range(8):
                if dc % 2 == 0:
                    P.copy(dst[:, half * 8 + dc, :capv], pb[dc][:, :capv])
                else:
                    P.act(dst[:, half * 8 + dc, :capv], pb[dc][:, :capv], AF.Copy)

    for e in range(NE):
        gather(0, keyL, e, NI, cap, xg)
        cks = [(xg, at, cap, G["ysl"])]
        if do_ctx:
            gather(T, keyC, e, NIC, capc, xgc)
            cks.append((xgc, atc, capc, G["ycsl"]))
        wgv = G["w_gate"][l, e].rearrange("(kc p) n -> p kc n", p=128)
        wuv = G["w_up"][l, e].rearrange("(kc p) n -> p kc n", p=128)
        wdv = G["w_down"][l, e].rearrange("(kc p) n -> p kc n", p=128)
        for fg in range(D // WG):
            wg_t = wbuf[(cnt_it[1] * 2) % 4]
            wu_t = wbuf[(cnt_it[1] * 2 + 1) % 4]
            cnt_it[1] += 1
            P.dma(wg_t[:], wgv[:, :, fg * WG:(fg + 1) * WG], q="pool")
            P.dma(wu_t[:], wuv[:, :, fg * WG:(fg + 1) * WG], q="pool")
            for (xs, as_, n, _) in cks:
                for fi in range(WG // 128):
                    f = fg * (WG // 128) + fi
                    pg, pu = pb[(f % 2) * 2], pb[(f % 2) * 2 + 1]
                    for kc in range(KC):
                        P.mm(pg[:, :n], wg_t[:, kc, fi * 128:(fi + 1) * 128], xs[:, kc, :n], kc == 0, kc == KC - 1)
                    for kc in range(KC):
                        P.mm(pu[:, :n], wu_t[:, kc, fi * 128:(fi + 1) * 128], xs[:, kc, :n], kc == 0, kc == KC - 1)
                    s_ = sg[f % 2]
                    P.act(s_[:, :n], pg[:, :n], AF.Silu)
                    P.tt(as_[:, f, :n], s_[:, :n], pu[:, :n], ALU.mult)
        for dg in range(D // WG):
            wd_t = wbuf[cnt_it[2] % 4]
            cnt_it[2] += 1
            P.dma(wd_t[:], wdv[:, :, dg * WG:(dg + 1) * WG], q="pool")
            for (xs, as_, n, ydst) in cks:
                for st in range((n + 127) // 128):
                    sw = min(128, n - st * 128)
                    py = pb[4 + (st % 2)]
                    for fc in range(KC):
                        P.mm(py[:sw, :WG], as_[:, fc, st * 128:st * 128 + sw], wd_t[:, fc, :], fc == 0, fc == KC - 1)
                    y_ = yo[st % 2]
                    P.act(y_[:sw, :WG], py[:sw, :WG], AF.Copy)
                    P.dma(ydst[e, st * 128:st * 128 + sw, dg * WG:(dg + 1) * WG], y_[:sw, :WG])
    P.phase_end(mk)


def emit_E(P, pb, G, l, T, NCX, do_ctx, xo):
    mk = P.phase_begin()
    cap, capc = 2 * T // NE, 2 * NCX // NE
    KL = min(128, cap)
    S = cap // KL
    modfm = G["modfm"]
    sidt = P.sb("E_sid", [128, 4])
    P.dma(sidt[:], G["sid"])
    Yt = P.sb("E_Yt", [KL, NE, S, 1024], BF16)
    if do_ctx:
        Yc = P.sb("E_Yc", [capc, NE, 1024], BF16)
    kgb = [P.sb("E_kgb%d" % i, [128, 2, 512]) for i in range(2)]
    selT = [P.sb("E_selT%d" % i, [128, 512], BF16) for i in range(3)]
    xs = [P.sb("E_xs%d" % i, [128, 512]) for i in range(2)]
    ot = [P.sb("E_ot%d" % i, [128, 512]) for i in range(2)]
    qch = [(c0, n, 0) for c0, n in chunks(T)]
    if do_ctx:
        qch += [(T + c0, n, 1) for c0, n in chunks(NCX)]
    it = 0
    for half in range(2):
        hs = slice(half * 1024, (half + 1) * 1024)
        for e in range(NE):
            P.dma(Yt[:, e, :, :], G["ysl"][e].rearrange("(s p) d -> p s d", p=KL)[:, :, hs], q="sp" if e % 2 == 0 else "pool")
        if do_ctx:
            P.dma(Yc[:], G["ycsl"].rearrange("e s d -> s e d")[:, :, hs])
        for (c0, n, kind) in qch:
            for e in range(NE):
                k_ = kgb[e % 2]
                ns, K = (S, KL) if kind == 0 else (1, capc)
                P.dma(k_[:, 0, :n], G["keyD"][e, c0:c0 + n].partition_broadcast(128))
                P.dma(k_[:, 1, :n], G["gateD"][e, c0:c0 + n].partition_broadcast(128), q="pool")
                for s in range(ns):
                    st = selT[it % 3]
                    it += 1
                    P.stt(st[:K, :n], k_[:K, 0, :n], sidt[:K, s:s + 1], k_[:K, 1, :n], ALU.is_equal, ALU.mult)
                    first = (e == 0 and s == 0)
                    last = (e == NE - 1 and s == ns - 1)
                    for dc in range(8):
                        lhs = Yt[:KL, e, s, dc * 128:(dc + 1) * 128] if kind == 0 else Yc[:, e, dc * 128:(dc + 1) * 128]
                        P.mm(pb[dc][:, :n], lhs, st[:K, :n], first, last)
            for dc in range(8):
                m = half * 8 + dc
                x_ = xs[dc % 2]
                o_ = ot[dc % 2]
                P.dma(x_[:, :n], G["xn"][m * 128:(m + 1) * 128, c0:c0 + n], q="pool")
                P.stt(o_[:, :n], pb[dc][:, :n], modfm[:, l, 80 + m, kind:kind + 1], x_[:, :n], ALU.mult, ALU.add)
                P.dma(xo[m * 128:(m + 1) * 128, c0:c0 + n], o_[:, :n])
    P.phase_end(mk)


_PROG_CACHE = {}


def build_fused(T, NCX):
    NT = T + NCX
    NI, NIC = T // 128, NCX // 128
    cap, capc = 2 * T // NE, 2 * NCX // NE
    P = Prog()
    G = {}
    f32in = dict(xT0=[D, NT], sT=[128, KC, 2], bmod=[128, 2, 96], w_mod=[2, D, 6 * D], cvA=[2, 128, CV_A["n"]],
                 w_in=[2, D, W_IN_EXT], w_uq=[2, 512, 2048], w_uk=[2, 256, 1024], w_uv=[2, 256, 1024],
                 ropec=[64, T], ropes=[64, T], cvl=[2, 128, 88], lru_wa=[2, 2, 8, 128, 128], lru_wx=[2, 2, 8, 128, 128],
                 cvB=[2, 128, CV_B["n"]], w_out=[2, D, D], router=[2, D, NE], ident=[128, 128], ut=[128, 128],
                 iot=[128, 512], sid=[128, 4], w_gate=[2, NE, D, D], w_up=[2, NE, D, D], w_down=[2, NE, D, D])
    for k, shp in f32in.items():
        G[k] = P.din(k, shp)
    out = P.dout("out", [D, T])
    G["hm_d"] = P.dscr("hm_d", [128, KC, NT], BF16, track=True)
    G["xrT"] = P.dscr("xrT", [1024, NT])
    G["ggT"] = P.dscr("ggT", [1024, NT], BF16)
    G["qn"] = P.dscr("qn", [NH, 128, NT], BF16)
    G["qr"] = P.dscr("qr", [NH, 64, NT], BF16)
    G["kn"] = P.dscr("kn", [NH, 128, NT], BF16)
    G["kr"] = P.dscr("kr", [NH, 64, NT], BF16)
    G["v"] = P.dscr("v", [NT, 1024], BF16)
    G["yr"] = P.dscr("yr", [1024, NT], BF16)
    G["xn"] = P.dscr("xn", [D, NT])
    G["h2tm"] = P.dscr("h2tm", [NT, D], BF16)
    G["ysl"] = P.dscr("ysl", [NE, cap, D], BF16)
    G["ycsl"] = P.dscr("ycsl", [NE, capc, D], BF16)
    G["keyD"] = P.dscr("keyD", [NE, NT])
    G["gateD"] = P.dscr("gateD", [NE, NT])
    x1 = P.dscr("x1", [D, NT])
    pb = [P.ps("pb%d" % i, [128, 512]) for i in range(8)]
    G["modfm"] = P.sb("modfm", [128, 2, 96, 2])
    G["aff_sb"] = P.sb("aff_sb", [128, NE, NI + NIC])
    G["key_sb"] = P.sb("key_sb", [128, NE, NI + NIC])
    P.memset(G["key_sb"][:], -1.0)
    emit_M(P, pb, G)
    xT = G["xT0"]
    for l in range(2):
        do_ctx = (l == 0)
        emit_A(P, pb, G, l, xT, T, NCX)
        emit_A2(P, pb, G, l, T, NCX)
        emit_B(P, pb, G, l, xT, T, NCX, do_ctx)
        emit_D(P, pb, G, l, T, NCX, do_ctx)
        emit_E(P, pb, G, l, T, NCX, do_ctx, x1 if l == 0 else out)
        xT = x1
    print("[build_fused] instructions:", P.n_ins)
    return P.finish()


def host_inputs(inp, b):
    x, ctx = inp["x"], inp["ctx"]
    T = x.shape[1]
    m = {}
    m["xT0"] = np.ascontiguousarray(np.concatenate([x[b].T, ctx[b].T], axis=1), dtype=np.float32)
    C2 = np.stack([inp["c"][b], inp["c_ctx"]], axis=0).astype(np.float32)
    m["sT"] = np.ascontiguousarray(C2.T.reshape(KC, 128, 2).transpose(1, 0, 2))
    m["bmod"] = np.ascontiguousarray(np.stack([fm(inp["b_mod"][l]) for l in range(2)], axis=1))
    m["w_mod"] = inp["w_mod"]
    z16 = np.zeros((128, 16), np.float32)
    cvA, cvl, cvB, w_in, w_uq, w_uk, w_uv = [], [], [], [], [], [], []
    for l in range(2):
        qn, kn = inp["q_norm"][l], inp["k_norm"][l]
        cvA.append(np.concatenate([fm(inp["norm1"][l]), z16, z16, z16, z16, fm(inp["q_a_norm"][l]), fm(inp["kv_a_norm"][l]),
                                   pad128(qn[:128]), pad128(qn[128:]), pad128(qn[128 + _SW]),
                                   pad128(kn[:128]), pad128(kn[128:]), pad128(kn[128 + _SW])], axis=1))
        a, b_, c_, d_ = a_weights(inp, l)
        w_in.append(a); w_uq.append(b_); w_uk.append(c_); w_uv.append(d_)
        cw, cb = inp["conv_w"][l], inp["conv_b"][l]
        cols = [cw.reshape(4, 8, 128).transpose(2, 1, 0).reshape(128, 32), cb.reshape(8, 128).T]
        for nm in ("lru_ba", "lru_bx", "lru_lambda"):
            cols.append(inp[nm][l].reshape(2, 8, 128).transpose(2, 0, 1).reshape(128, 16))
        cvl.append(np.concatenate(cols, axis=1))
        cvB.append(np.concatenate([z16, z16, fm(inp["norm2"][l]), z16, z16, z16, z16], axis=1))
    m["cvA"] = np.ascontiguousarray(np.stack(cvA).astype(np.float32))
    m["cvl"] = np.ascontiguousarray(np.stack(cvl).astype(np.float32))
    m["cvB"] = np.ascontiguousarray(np.stack(cvB).astype(np.float32))
    m["w_in"] = np.stack(w_in); m["w_uq"] = np.stack(w_uq); m["w_uk"] = np.stack(w_uk); m["w_uv"] = np.stack(w_uv)
    cos, sin = rope_tables(np.arange(T))
    m["ropec"], m["ropes"] = cos, sin
    m["lru_wa"], m["lru_wx"] = inp["lru_wa"], inp["lru_wx"]
    m["w_out"], m["router"] = inp["w_out"], inp["router"]
    m["ident"] = np.eye(128, dtype=np.float32)
    m["ut"] = np.triu(np.ones((128, 128), np.float32))
    m["iot"] = np.tile(np.arange(512, dtype=np.float32), (128, 1))
    m["sid"] = (np.arange(128, dtype=np.float32)[:, None] + 128.0 * np.arange(4, dtype=np.float32)[None, :]).astype(np.float32)
    m["w_gate"], m["w_up"], m["w_down"] = inp["w_gate"], inp["w_up"], inp["w_down"]
    return {k: np.ascontiguousarray(v, dtype=np.float32) for k, v in m.items()}


def kernel(**inputs):
    inp = {k: np.asarray(v) for k, v in inputs.items()}
    Bn, T, _ = inp["x"].shape
    NCX = inp["ctx"].shape[1]
    key = (T, NCX)
    if key not in _PROG_CACHE:
        _PROG_CACHE[key] = build_fused(T, NCX)
    nc = _PROG_CACHE[key]
    maps = [host_inputs(inp, b) for b in range(Bn)]
    res = run_bass_kernel_spmd(nc, maps, core_ids=list(range(Bn)))
    out = np.stack([np.ascontiguousarray(r["out"].T) for r in res.results])
    return out.astype(np.float32, copy=False)
```

```python
import numpy as np
import ml_dtypes
import concourse.bass as bass
import concourse.mybir as mybir
from concourse.bass_utils import run_bass_kernel_spmd

F32 = mybir.dt.float32
BF16 = mybir.dt.bfloat16
ALU = mybir.AluOpType
AF = mybir.ActivationFunctionType
AX = mybir.AxisListType
NPBF = ml_dtypes.bfloat16

D = 2048
KC = 16
NH = 8
NE = 16
EPS = 1e-6
ATT_SCALE = 192 ** -0.5
NCORES = 8


class Prog:
    ENG = ("pe", "dve", "act", "pool", "sp")

    def __init__(self):
        self.nc = bass.Bass("TRN2", target_bir_lowering=False)
        nc = self.nc
        self.eng = {"pe": nc.tensor, "dve": nc.vector, "act": nc.scalar,
                    "pool": nc.gpsimd, "sp": nc.sync}
        self._ctx = []
        self.csem, self.dsem, self.cnt = {}, {}, {}
        self.NDS = 20
        self.drr = {}
        for e in self.ENG:
            self.csem[e] = self._enter(nc.semaphore("c_" + e))
            self.cnt[("c", e)] = 0
        for e in ("sp", "pool"):
            self.drr[e] = 0
            for i in range(self.NDS):
                self.dsem[(e, i)] = self._enter(nc.semaphore("d_%s%d" % (e, i)))
                self.cnt[("d", e, i)] = 0
        self.waited = {e: {} for e in self.ENG}
        self.lastw, self.readers, self.tags = {}, {}, {}
        self.skip = set()
        self.n_ins = 0
        self.uid = 0

    def _enter(self, cm):
        v = cm.__enter__()
        self._ctx.append(cm)
        return v

    def sb(self, name, shape, dt=F32):
        self.uid += 1
        return self._enter(self.nc.sbuf_tensor("%s_u%d" % (name, self.uid), list(shape), dt))

    def ps(self, name, shape, dt=F32):
        return self._enter(self.nc.psum_tensor(name, list(shape), dt))

    def din(self, name, shape, dt=F32):
        self.skip.add(name)
        return self.nc.dram_tensor(name, list(shape), dt, kind="ExternalInput").ap()

    def dout(self, name, shape, dt=F32):
        self.skip.add(name)
        return self.nc.dram_tensor(name, list(shape), dt, kind="ExternalOutput").ap()

    def dscr(self, name, shape, dt=F32, track=False):
        if not track:
            self.skip.add(name)
        return self.nc.dram_tensor(name, list(shape), dt, kind="Internal").ap()

    @staticmethod
    def _nm(t):
        if isinstance(t, str):
            return t
        if hasattr(t, "tensor"):
            t = t.tensor
        return t.name

    def _norm(self, ks):
        out = []
        for k in ks:
            if k is None or isinstance(k, (int, float)):
                continue
            if isinstance(k, tuple):
                n = self._nm(k[0])
                if n not in self.skip:
                    out.append((n, k[1]))
            else:
                n = self._nm(k)
                if n not in self.skip:
                    out.append((n, None))
        return out

    def _conf(self, key):
        name, tag = key
        if tag is None:
            return [(name, t) for t in self.tags.get(name, ())] + [(name, None)]
        return [(name, tag), (name, None)]

    def _sem(self, sk):
        return self.csem[sk[1]] if sk[0] == "c" else self.dsem[(sk[1], sk[2])]

    def emit(self, e, build, reads=(), writes=(), dma=False):
        reads = self._norm(reads)
        writes = self._norm(writes)
        deps = {}

        def need(d):
            if d is not None and deps.get(d[0], 0) < d[1]:
                deps[d[0]] = d[1]

        for k in reads:
            for c in self._conf(k):
                need(self.lastw.get(c))
        for k in writes:
            for c in self._conf(k):
                need(self.lastw.get(c))
                for r in self.readers.get(c, ()):
                    need(r)
        engine = self.eng[e]
        for sk, v in deps.items():
            if sk == ("c", "pe") and e == "pe" and not dma:
                continue
            if self.waited[e].get(sk, 0) >= v:
                continue
            engine.wait_ge(self._sem(sk), v)
            self.waited[e][sk] = v
        if dma:
            sk = ("d", e, self.drr[e] % self.NDS)
            self.drr[e] += 1
            if self.cnt[sk] > self.waited[e].get(sk, 0):
                engine.wait_ge(self._sem(sk), self.cnt[sk])
                self.waited[e][sk] = self.cnt[sk]
        else:
            sk = ("c", e)
        ins = build(engine)
        self.cnt[sk] += 16 if dma else 1
        ins.then_inc(self._sem(sk), 16 if dma else 1)
        me = (sk, self.cnt[sk])
        for k in writes:
            if k[1] is None:
                for c in self._conf(k):
                    self.lastw.pop(c, None)
                    self.readers.pop(c, None)
            else:
                self.tags.setdefault(k[0], set()).add(k[1])
            self.lastw[k] = me
            self.readers[k] = []
        for k in reads:
            if k[1] is not None:
                self.tags.setdefault(k[0], set()).add(k[1])
            lst = self.readers.setdefault(k, [])
            lst.append(me)
            if len(lst) > 10:
                best = {}
                for s, v in lst:
                    if best.get(s, 0) < v:
                        best[s] = v
                self.readers[k] = list(best.items())
        self.n_ins += 1
        return me

    def dma(self, out, in_, q="sp", rd=None, wr=None, slow=False):
        kw = {"allow_slow_non_contiguous": True} if slow else {}
        return self.emit(q, lambda en: en.dma_start(out=out, in_=in_, **kw),
                         rd if rd is not None else [in_], wr if wr is not None else [out], dma=True)

    def mm(self, out, lhsT, rhs, start, stop):
        return self.emit("pe", lambda en: en.matmul(out, lhsT, rhs, start=start, stop=stop),
                         [lhsT, rhs], [out])

    def act(self, out, in_, func, bias=None, scale=None, e="act"):
        kw = {}
        if bias is not None:
            kw["bias"] = bias
        if scale is not None:
            kw["scale"] = scale
        return self.emit(e, lambda en: en.activation(out, in_, func, **kw), [in_, bias, scale], [out])

    def tt(self, out, a, b, op, e="dve"):
        return self.emit(e, lambda en: en.tensor_tensor(out, a, b, op), [a, b], [out])

    def ts(self, out, a, s1, s2, op0, op1=None, e="dve"):
        if op1 is None:
            return self.emit(e, lambda en: en.tensor_scalar(out, a, s1, None, op0), [a, s1], [out])
        return self.emit(e, lambda en: en.tensor_scalar(out, a, s1, s2, op0, op1), [a, s1, s2], [out])

    def stt(self, out, in0, scalar, in1, op0, op1):
        return self.emit("dve", lambda en: en.scalar_tensor_tensor(out, in0, scalar, in1, op0, op1),
                         [in0, scalar, in1], [out])

    def copy(self, out, in_, e="dve"):
        return self.emit(e, lambda en: en.tensor_copy(out, in_), [in_], [out])

    def memset(self, out, val, e="dve"):
        return self.emit(e, lambda en: en.memset(out, val), [], [out])

    def scan(self, out, d0, d1, init, op0, op1):
        return self.emit("dve", lambda en: en.tensor_tensor_scan(out, d0, d1, init, op0, op1),
                         [d0, d1, init], [out])

    def recip(self, out, in_):
        return self.emit("dve", lambda en: en.reciprocal(out, in_), [in_], [out])

    def rstd(self, out, ss, inv_n, epsb, tmp=None):
        self.act(out, ss, AF.Sqrt, bias=epsb, scale=inv_n)
        self.recip(out, out)

    def barrier(self):
        for e in self.ENG:
            engine = self.eng[e]
            for sk, v in self.cnt.items():
                if v > self.waited[e].get(sk, 0):
                    engine.wait_ge(self._sem(sk), v)
                    self.waited[e][sk] = v
        self.lastw.clear()
        self.readers.clear()
        self.tags.clear()

    def phase_begin(self):
        return len(self._ctx)

    def phase_end(self, mark):
        self.barrier()
        while len(self._ctx) > mark:
            self._ctx.pop().__exit__(None, None, None)

    def finish(self):
        for sk, v in self.cnt.items():
            if v > 0:
                self.eng["sp"].wait_ge(self._sem(sk), v)
        while self._ctx:
            self._ctx.pop().__exit__(None, None, None)
        return self.nc


def fm(v):
    v = np.asarray(v, np.float32)
    return np.ascontiguousarray(v.reshape(-1, 128).T)


def chunks(n, step=512):
    return [(i, min(step, n - i)) for i in range(0, n, step)]


_SW = np.array([f + 16 if (f % 32) < 16 else f - 16 for f in range(64)])


def rope_tables(t_idx):
    t_idx = np.asarray(t_idx)
    row = (t_idx // 64).astype(np.float32)
    col = (t_idx % 64).astype(np.float32)
    inv = (np.float32(10000.0) ** (-np.arange(16, dtype=np.float32) / np.float32(16))).astype(np.float32)
    cos = np.zeros((64, len(t_idx)), np.float32)
    sin = np.zeros((64, len(t_idx)), np.float32)
    for f in range(64):
        pos = row if f < 32 else col
        ang = (pos * inv[f % 16]).astype(np.float32)
        cos[f] = np.cos(ang)
        s = np.sin(ang)
        sin[f] = -s if (f % 32) < 16 else s
    return cos, sin


def pad128(v):
    o = np.zeros((128, 1), np.float32)
    o[:len(v), 0] = v
    return o


def a_weights(inp, l):
    w_in = inp["w_in"][l]
    w_in_ext = np.ascontiguousarray(np.concatenate([w_in, w_in[:, 2816 + _SW]], axis=1))
    wq = inp["w_uq"][l].reshape(512, NH, 192)
    w_uq_ext = np.ascontiguousarray(np.concatenate([wq, wq[:, :, 128 + _SW]], axis=2).reshape(512, NH * 256))
    wkv = inp["w_ukv"][l].reshape(256, NH, 256)
    w_uk = np.ascontiguousarray(wkv[:, :, :128].reshape(256, 1024))
    w_uv = np.ascontiguousarray(wkv[:, :, 128:].reshape(256, 1024))
    return w_in_ext, w_uq_ext, w_uk, w_uv


I32 = mybir.dt.int32
N_BISECT = 34
W_IN_EXT = 2944
CV_A = dict(g1=0, sh_l=16, sc_l=32, sh_c=48, sc_c=64, gqa=80, gkva=84,
            gq_n=86, gq_r=87, gq_s=88, gk_n=89, gk_r=90, gk_s=91, n=92)
CV_B = dict(g1_l=0, g1_c=16, n2=32, sh2_l=48, sc2_l=64, sh2_c=80, sc2_c=96, n=112)


def emit_M(P, pb, G):
    mk = P.phase_begin()
    s2 = P.sb("M_s2", [128, KC, 2])
    s2b = P.sb("M_s2b", [128, KC, 2], BF16)
    bm = P.sb("M_bm", [128, 2, 96])
    wt = [P.sb("M_wt%d" % i, [128, KC, 512], BF16) for i in range(3)]
    P.dma(s2[:], G["sT"])
    P.dma(bm[:], G["bmod"])
    P.act(s2[:], s2[:], AF.Silu)
    P.copy(s2b[:], s2[:])
    i = 0
    for l in range(2):
        wv = G["w_mod"][l].rearrange("(kc p) n -> p kc n", p=128)
        for cg in range(24):
            t = wt[i % 3]
            P.dma(t[:], wv[:, :, cg * 512:(cg + 1) * 512], q="pool")
            i += 1
            for ci in range(4):
                ch = cg * 4 + ci
                p_ = pb[ch % 4]
                for kc in range(KC):
                    P.mm(p_[:, 0:2], t[:, kc, ci * 128:(ci + 1) * 128], s2b[:, kc, :], kc == 0, kc == KC - 1)
                P.ts(G["modfm"][:, l, ch, :], p_[:, 0:2], bm[:, l, ch:ch + 1], None, ALU.add)
    P.phase_end(mk)


def emit_A(P, pb, G, l, xT, NLC, NCX):
    NT = NLC + NCX
    mk = P.phase_begin()
    modfm = G["modfm"]
    cvt = P.sb("A_cvt", [128, CV_A["n"]])
    P.dma(cvt[:], G["cvA"][l])
    P.copy(cvt[:, 16:32], modfm[:, l, 0:16, 0])
    P.copy(cvt[:, 32:48], modfm[:, l, 16:32, 0])
    P.copy(cvt[:, 48:64], modfm[:, l, 0:16, 1])
    P.copy(cvt[:, 64:80], modfm[:, l, 16:32, 1])
    C = lambda name, j=0, w=1: cvt[:, CV_A[name] + j: CV_A[name] + j + w]
    epsb = P.sb("A_epsb", [128, 1])
    P.memset(epsb[:], EPS)
    ones = P.sb("A_ones", [128, 128], BF16)
    P.memset(ones[:], 1.0)
    Am = P.sb("A_Am", [128, 2, KC])
    for k, nm in enumerate(("sc_l", "sc_c")):
        P.ts(Am[:, k, :], C(nm, 0, KC), 1.0, None, ALU.add)
        P.tt(Am[:, k, :], Am[:, k, :], C("g1", 0, KC), ALU.mult)
    rc_t = P.sb("A_rc", [64, 512])
    rs_t = P.sb("A_rs", [64, 512])
    tch = [(c0, n, 0) for c0, n in chunks(NLC)] + [(NLC + c0, n, 1) for c0, n in chunks(NCX)]
    hm_d = G["hm_d"]
    xt = P.sb("A_xt", [128, KC, 512])
    sq = [P.sb("A_sq%d" % i, [128, 512]) for i in range(2)]
    sqb = [P.sb("A_sqb%d" % i, [128, 512], BF16) for i in range(2)]
    rs = P.sb("A_rsd", [128, 512])
    hmc = [P.sb("A_hmc%d" % i, [128, KC, 512], BF16) for i in range(3)]
    hm = hmc[0]
    xv = xT.rearrange("(kc p) n -> p kc n", p=128)
    for (c0, n, kind) in tch:
        P.dma(xt[:, :, :n], xv[:, :, c0:c0 + n], q="pool")
        for kc in range(KC):
            s_ = sqb[kc % 2]
            P.act(s_[:, :n], xt[:, kc, :n], AF.Square)
            P.mm(pb[0][:, :n], ones[:], s_[:, :n], kc == 0, kc == KC - 1)
        P.rstd(rs[:, :n], pb[0][:, :n], 1.0 / D, epsb[:])
        shn = "sh_l" if kind == 0 else "sh_c"
        for kc in range(KC):
            s_ = sq[kc % 2]
            P.tt(s_[:, :n], xt[:, kc, :n], rs[:, :n], ALU.mult)
            P.ts(hm[:, kc, :n], s_[:, :n], Am[:, kind, kc:kc + 1], C(shn, kc), ALU.mult, ALU.add)
        P.dma(hm_d[:, :, c0:c0 + n], hm[:, :, :n], wr=[(hm_d, c0)])

    wg = [P.sb("A_wg%d" % i, [128, KC, 512], BF16) for i in range(2)]
    wuq = P.sb("A_wuq", [128, 4, 2048], BF16)
    wuk = P.sb("A_wuk", [128, 2, 1024], BF16)
    wuv = P.sb("A_wuv", [128, 2, 1024], BF16)
    P.dma(wuq[:], G["w_uq"][l].rearrange("(kc p) n -> p kc n", p=128), q="pool")
    P.dma(wuk[:], G["w_uk"][l].rearrange("(kc p) n -> p kc n", p=128), q="pool")
    P.dma(wuv[:], G["w_uv"][l].rearrange("(kc p) n -> p kc n", p=128), q="pool")
    wv = G["w_in"][l].rearrange("(kc p) n -> p kc n", p=128)
    ev = [P.sb("A_ev%d" % i, [128, 512]) for i in range(2)]
    evb = [P.sb("A_evb%d" % i, [128, 512], BF16) for i in range(2)]
    cqt = P.sb("A_cqt", [128, 4, 512])
    cqn = P.sb("A_cqn", [128, 4, 512], BF16)
    ckt = P.sb("A_ckt", [128, 2, 512])
    ckn = P.sb("A_ckn", [128, 2, 512], BF16)
    krt = P.sb("A_krt", [64, 512])
    kst = P.sb("A_kst", [64, 512])
    krsq = P.sb("A_krsq", [64, 512], BF16)
    Rt = P.sb("A_Rt", [64, 512])
    sqn2 = [P.sb("A_sqn%d" % i, [128, 512], BF16) for i in range(2)]
    sqr2 = [P.sb("A_sqr%d" % i, [64, 512], BF16) for i in range(2)]
    rsh2 = [P.sb("A_rsh%d" % i, [128, 512]) for i in range(2)]
    t64a = P.sb("A_t64a", [64, 512])
    t64b = P.sb("A_t64b", [64, 512])
    ob64 = [P.sb("A_ob64_%d" % i, [64, 512], BF16) for i in range(2)]
    vb = [P.sb("A_vb%d" % i, [128, 1024], BF16) for i in range(2)]
    xrT, ggT, qn_o, qr_o, kn_o, kr_o, v_o = G["xrT"], G["ggT"], G["qn"], G["qr"], G["kn"], G["kr"], G["v"]

    def rope_mix(out_bf, a_f, b_f, n, kind):
        if kind == 0:
            P.tt(a_f, a_f, rc_t[:, :n], ALU.mult)
            P.tt(b_f, b_f, rs_t[:, :n], ALU.mult)
            P.tt(out_bf, a_f, b_f, ALU.add)
        else:
            P.copy(out_bf, a_f)

    groups = chunks(W_IN_EXT)
    it = 0
    for gi, (g0, gn) in enumerate(groups):
        w_ = wg[gi % 2]
        P.dma(w_[:, :, :gn], wv[:, :, g0:g0 + gn], q="pool")
        for (c0, n, kind) in tch:
            h_ = hmc[it % 3]
            it += 1
            P.dma(h_[:, :, :n], hm_d[:, :, c0:c0 + n], rd=[(hm_d, c0)], q="pool")
            if kind == 0 and g0 >= 2048:
                P.dma(rc_t[:, :n], G["ropec"][:, c0:c0 + n], q="pool")
                P.dma(rs_t[:, :n], G["ropes"][:, c0:c0 + n], q="pool")
            nm = (gn + 127) // 128
            for mi in range(nm):
                col = g0 + mi * 128
                mw = min(128, gn - mi * 128)
                p_ = pb[1 + (mi % 4)]
                if col < 2816:
                    for kc in range(KC):
                        P.mm(p_[:mw, :n], w_[:, kc, mi * 128: mi * 128 + mw], h_[:, kc, :n], kc == 0, kc == KC - 1)
                m = col // 128
                if m < 8:
                    e_ = ev[m % 2]
                    P.act(e_[:, :n], p_[:, :n], AF.Copy)
                    P.dma(xrT[m * 128:(m + 1) * 128, c0:c0 + n], e_[:, :n])
                elif m < 16:
                    e_ = evb[m % 2]
                    P.act(e_[:, :n], p_[:, :n], AF.Gelu_apprx_tanh)
                    P.dma(ggT[(m - 8) * 128:(m - 7) * 128, c0:c0 + n], e_[:, :n])
                elif m < 20:
                    P.copy(cqt[:, m - 16, :n], p_[:, :n])
                elif m < 22:
                    P.copy(ckt[:, m - 20, :n], p_[:, :n])
                else:
                    for kc in range(KC):
                        P.mm(pb[5][:64, :n], w_[:, kc, mi * 128: mi * 128 + 64], h_[:, kc, :n], kc == 0, kc == KC - 1)
                    for kc in range(KC):
                        P.mm(pb[6][:64, :n], w_[:, kc, mi * 128 + 64: mi * 128 + 128], h_[:, kc, :n], kc == 0, kc == KC - 1)
                    P.copy(krt[:, :n], pb[5][:64, :n])
                    P.copy(kst[:, :n], pb[6][:64, :n])
            if g0 == 2048:
                for kc in range(4):
                    s_ = sqb[kc % 2]
                    P.act(s_[:, :n], cqt[:, kc, :n], AF.Square)
                    P.mm(pb[0][:, :n], ones[:], s_[:, :n], kc == 0, kc == 3)
                P.rstd(rs[:, :n], pb[0][:, :n], 1.0 / 512, epsb[:])
                for kc in range(4):
                    P.stt(cqn[:, kc, :n], cqt[:, kc, :n], C("gqa", kc), rs[:, :n], ALU.mult, ALU.mult)
                def qproj(h):
                    pn, pr, pS = pb[1 + (h % 2) * 3], pb[2 + (h % 2) * 3], pb[3 + (h % 2) * 3]
                    b0 = h * 256
                    for kc in range(4):
                        P.mm(pn[:, :n], wuq[:, kc, b0:b0 + 128], cqn[:, kc, :n], kc == 0, kc == 3)
                    for kc in range(4):
                        P.mm(pr[:64, :n], wuq[:, kc, b0 + 128:b0 + 192], cqn[:, kc, :n], kc == 0, kc == 3)
                    for kc in range(4):
                        P.mm(pS[:64, :n], wuq[:, kc, b0 + 192:b0 + 256], cqn[:, kc, :n], kc == 0, kc == 3)

                qproj(0)
                for h in range(NH):
                    pn, pr, pS = pb[1 + (h % 2) * 3], pb[2 + (h % 2) * 3], pb[3 + (h % 2) * 3]
                    sqn_, sqr_, rsh_ = sqn2[h % 2], sqr2[h % 2], rsh2[h % 2]
                    P.act(sqn_[:, :n], pn[:, :n], AF.Square)
                    P.act(sqr_[:, :n], pr[:64, :n], AF.Square)
                    if h + 1 < NH:
                        qproj(h + 1)
                    P.mm(pb[7][:, :n], ones[:], sqn_[:, :n], True, False)
                    P.mm(pb[7][:, :n], ones[:64, :], sqr_[:, :n], False, True)
                    P.rstd(rsh_[:, :n], pb[7][:, :n], 1.0 / 192, epsb[:])
                    o_ = evb[h % 2]
                    P.stt(o_[:, :n], pn[:, :n], C("gq_n"), rsh_[:, :n], ALU.mult, ALU.mult)
                    P.dma(qn_o[h, :, c0:c0 + n], o_[:, :n])
                    P.stt(t64a[:, :n], pr[:64, :n], cvt[:64, CV_A["gq_r"]:CV_A["gq_r"] + 1], rsh_[:64, :n], ALU.mult, ALU.mult)
                    P.stt(t64b[:, :n], pS[:64, :n], cvt[:64, CV_A["gq_s"]:CV_A["gq_s"] + 1], rsh_[:64, :n], ALU.mult, ALU.mult)
                    o6 = ob64[h % 2]
                    rope_mix(o6[:, :n], t64a[:, :n], t64b[:, :n], n, kind)
                    P.dma(qr_o[h, :, c0:c0 + n], o6[:, :n])
            if g0 == 2560:
                for kc in range(2):
                    s_ = sqb[kc % 2]
                    P.act(s_[:, :n], ckt[:, kc, :n], AF.Square)
                    P.mm(pb[0][:, :n], ones[:], s_[:, :n], kc == 0, kc == 1)
                P.rstd(rs[:, :n], pb[0][:, :n], 1.0 / 256, epsb[:])
                for kc in range(2):
                    P.stt(ckn[:, kc, :n], ckt[:, kc, :n], C("gkva", kc), rs[:, :n], ALU.mult, ALU.mult)
                P.act(krsq[:, :n], krt[:, :n], AF.Square)
                P.ts(t64a[:, :n], krt[:, :n], cvt[:64, CV_A["gk_r"]:CV_A["gk_r"] + 1], None, ALU.mult)
                P.ts(t64b[:, :n], kst[:, :n], cvt[:64, CV_A["gk_s"]:CV_A["gk_s"] + 1], None, ALU.mult)
                if kind == 0:
                    P.tt(t64a[:, :n], t64a[:, :n], rc_t[:, :n], ALU.mult)
                    P.tt(t64b[:, :n], t64b[:, :n], rs_t[:, :n], ALU.mult)
                    P.tt(Rt[:, :n], t64a[:, :n], t64b[:, :n], ALU.add)
                else:
                    P.copy(Rt[:, :n], t64a[:, :n])
                def kproj(h):
                    pn = pb[1 + (h % 2)]
                    for kc in range(2):
                        P.mm(pn[:, :n], wuk[:, kc, h * 128:(h + 1) * 128], ckn[:, kc, :n], kc == 0, kc == 1)

                kproj(0)
                for h in range(NH):
                    pn = pb[1 + (h % 2)]
                    sqn_, rsh_ = sqn2[h % 2], rsh2[h % 2]
                    P.act(sqn_[:, :n], pn[:, :n], AF.Square)
                    if h + 1 < NH:
                        kproj(h + 1)
                    P.mm(pb[7][:, :n], ones[:], sqn_[:, :n], True, False)
                    P.mm(pb[7][:, :n], ones[:64, :], krsq[:, :n], False, True)
                    P.rstd(rsh_[:, :n], pb[7][:, :n], 1.0 / 192, epsb[:])
                    o_ = evb[h % 2]
                    P.stt(o_[:, :n], pn[:, :n], C("gk_n"), rsh_[:, :n], ALU.mult, ALU.mult)
                    P.dma(kn_o[h, :, c0:c0 + n], o_[:, :n])
                    o6 = ob64[h % 2]
                    P.tt(o6[:, :n], Rt[:, :n], rsh_[:64, :n], ALU.mult)
                    P.dma(kr_o[h, :, c0:c0 + n], o6[:, :n])
                for j in range(n // 128):
                    vt = vb[j % 2]
                    for hh in range(2):
                        pv = pb[3 + hh]
                        for kc in range(2):
                            P.mm(pv[:, :], ckn[:, kc, j * 128:(j + 1) * 128], wuv[:, kc, hh * 512:(hh + 1) * 512], kc == 0, kc == 1)
                        P.act(vt[:, hh * 512:(hh + 1) * 512], pv[:, :], AF.Copy)
                    P.dma(v_o[c0 + j * 128: c0 + (j + 1) * 128, :], vt[:])
    P.phase_end(mk)


def emit_A2(P, pb, G, l, T, NCX):
    mk = P.phase_begin()
    xr, gg, yr = G["xrT"], G["ggT"], G["yr"]
    cvt = P.sb("L_cvt", [128, 88])
    P.dma(cvt[:], G["cvl"][l])
    one = P.sb("L_one", [128, 1])
    P.memset(one[:], 1.0)
    cl = P.sb("L_cl", [128, 16])
    P.act(cl[:], cvt[:, 72:88], AF.Exp, scale=-1.0)
    P.act(cl[:], cl[:], AF.Ln, bias=one[:])
    P.ts(cl[:], cl[:], -8.0, None, ALU.mult)
    wab = P.sb("L_wab", [128, 2, 8, 128], BF16)
    wxb = P.sb("L_wxb", [128, 2, 8, 128], BF16)
    for d in range(2):
        P.dma(wab[:, d, :, :], G["lru_wa"][l, d].rearrange("g i j -> i g j"), q="pool")
        P.dma(wxb[:, d, :, :], G["lru_wx"][l, d].rearrange("g i j -> i g j"), q="pool")
    streams = [("C", T, NCX), ("L", 0, T)]
    tl = {}
    for s, _, n in streams:
        X = P.sb("L_X" + s, [128, n + 3])
        tl[s] = dict(X=X, u=P.sb("L_u" + s, [128, n]), ub=P.sb("L_ub" + s, [128, n], BF16),
                     ra=[P.sb("L_ra%d" % d + s, [128, n]) for d in range(2)],
                     ii=[P.sb("L_ii%d" % d + s, [128, n]) for d in range(2)],
                     sb=[P.sb("L_sb%d" % d + s, [128, n]) for d in range(2)],
                     hf=P.sb("L_hf" + s, [128, n]),
                     gg=P.sb("L_gg" + s, [128, n], BF16), yo=P.sb("L_yo" + s, [128, n], BF16))
    for g in range(8):
        rows = slice(g * 128, (g + 1) * 128)
        for s, s0, n in streams:
            t = tl[s]
            P.memset(t["X"][:, 0:2], 0.0)
            P.memset(t["X"][:, n + 2:n + 3], 0.0)
            P.dma(t["X"][:, 2:n + 2], xr[rows, s0:s0 + n])
            P.dma(t["gg"][:], gg[rows, s0:s0 + n])
            P.ts(t["u"][:], t["X"][:, 0:n], cvt[:, g * 4:g * 4 + 1], cvt[:, 32 + g:33 + g], ALU.mult, ALU.add)
            for k in range(1, 4):
                P.stt(t["u"][:], t["X"][:, k:k + n], cvt[:, g * 4 + k:g * 4 + k + 1], t["u"][:], ALU.mult, ALU.add)
            P.act(t["ub"][:], t["u"][:], AF.Copy)
        for d in range(2):
            for s, s0, n in streams:
                t = tl[s]
                ra, ii, sb = t["ra"][d], t["ii"][d], t["sb"][d]
                for ci, (c0, cn) in enumerate(chunks(n)):
                    pr, pi = pb[(ci % 2) * 2], pb[(ci % 2) * 2 + 1]
                    P.mm(pr[:, :cn], wab[:, d, g, :], t["ub"][:, c0:c0 + cn], True, True)
                    P.mm(pi[:, :cn], wxb[:, d, g, :], t["ub"][:, c0:c0 + cn], True, True)
                    P.act(ra[:, c0:c0 + cn], pr[:, :cn], AF.Sigmoid, bias=cvt[:, 40 + d * 8 + g:41 + d * 8 + g])
                    P.act(ii[:, c0:c0 + cn], pi[:, :cn], AF.Sigmoid, bias=cvt[:, 56 + d * 8 + g:57 + d * 8 + g])
                P.act(ra[:], ra[:], AF.Exp, scale=cl[:, d * 8 + g:d * 8 + g + 1])
                P.act(sb[:], ra[:], AF.Square)
                P.act(sb[:], sb[:], AF.Sqrt, bias=one[:], scale=-1.0)
        for d in range(2):
            for s, s0, n in streams:
                t = tl[s]
                ra, ii, sb = t["ra"][d], t["ii"][d], t["sb"][d]
                P.tt(ii[:], ii[:], t["u"][:], ALU.mult)
                P.tt(sb[:], sb[:], ii[:], ALU.mult)
                if d == 0:
                    init = 0.0 if s == "C" else tl["C"]["hf"][:, NCX - 1:NCX]
                    P.scan(t["hf"][:], ra[:], sb[:], init, ALU.mult, ALU.add)
                else:
                    hb = t["X"][:, 0:n]
                    init = 0.0 if s == "C" else tl["C"]["X"][:, 0:1]
                    P.scan(hb[:, ::-1], ra[:, ::-1], sb[:, ::-1], init, ALU.mult, ALU.add)
        for s, s0, n in streams:
            t = tl[s]
            P.tt(t["hf"][:], t["hf"][:], t["X"][:, 0:n], ALU.add)
            P.tt(t["yo"][:], t["hf"][:], t["gg"][:], ALU.mult)
            P.dma(yr[rows, s0:s0 + n], t["yo"][:])
    P.phase_end(mk)


def emit_B(P, pb, G, l, xT, T, NCX, do_ctx):
    NT = T + NCX
    NKT = NT // 128
    NI = T // 128
    modfm = G["modfm"]
    aff_sb = G["aff_sb"]
    HQ = min(2048, T)
    for qp in range(T // HQ):
        mk = P.phase_begin()
        qch = [(qp * HQ + c0, n, 0, c0) for c0, n in chunks(HQ)]
        NQP = HQ
        if do_ctx and qp == 0:
            qch += [(T + c0, n, 1, HQ + c0) for c0, n in chunks(NCX)]
            NQP = HQ + NCX
        cvt = P.sb("B_cvt", [128, CV_B["n"]])
        P.dma(cvt[:], G["cvB"][l])
        P.copy(cvt[:, 0:16], modfm[:, l, 32:48, 0])
        P.copy(cvt[:, 16:32], modfm[:, l, 32:48, 1])
        P.copy(cvt[:, 48:64], modfm[:, l, 48:64, 0])
        P.copy(cvt[:, 64:80], modfm[:, l, 64:80, 0])
        P.copy(cvt[:, 80:96], modfm[:, l, 48:64, 1])
        P.copy(cvt[:, 96:112], modfm[:, l, 64:80, 1])
        C = lambda name, j=0, w=1: cvt[:, CV_B[name] + j: CV_B[name] + j + w]
        epsb = P.sb("B_epsb", [128, 1])
        P.memset(epsb[:], EPS)
        ones = P.sb("B_ones", [128, 128])
        P.memset(ones[:], 1.0)
        onesb = P.sb("B_onesb", [128, 128], BF16)
        P.memset(onesb[:], 1.0)
        ident = P.sb("B_ident", [128, 128])
        P.dma(ident[:], G["ident"])
        A2m = P.sb("B_A2m", [128, 2, KC])
        for k, nm in enumerate(("sc2_l", "sc2_c")):
            P.ts(A2m[:, k, :], C(nm, 0, KC), 1.0, None, ALU.add)
            P.tt(A2m[:, k, :], A2m[:, k, :], C("n2", 0, KC), ALU.mult)
        rt = P.sb("B_rt", [128, KC, NE])
        P.dma(rt[:], G["router"][l].rearrange("(kc p) e -> p kc e", p=128))

        yatt = P.sb("B_yatt", [128, 8, NQP], BF16)
        yrv = G["yr"].rearrange("(c p) n -> p c n", p=128)
        kvq = [dict(knt=P.sb("B_knt%d" % i, [128, NT], BF16), krt=P.sb("B_krt%d" % i, [64, NT], BF16),
                    vt=P.sb("B_vt%d" % i, [128, NKT, 128], BF16), qnt=P.sb("B_qnt%d" % i, [128, NQP], BF16),
                    qrt=P.sb("B_qrt%d" % i, [64, NQP], BF16)) for i in range(2)]
        pt = [P.sb("B_pt%d" % i, [128, 512], BF16) for i in range(3)]
        rec = P.sb("B_rec", [128, 512])
        it = 0
        for h in range(NH):
            kq = kvq[h % 2]
            knt, krt, vt, qnt, qrt = kq["knt"], kq["krt"], kq["vt"], kq["qnt"], kq["qrt"]
            P.dma(knt[:], G["kn"][h])
            P.dma(krt[:], G["kr"][h])
            P.dma(vt[:], G["v"][:, h * 128:(h + 1) * 128].rearrange("(j p) d -> p j d", p=128))
            P.dma(qnt[:, 0:HQ], G["qn"][h, :, qp * HQ:(qp + 1) * HQ])
            P.dma(qrt[:, 0:HQ], G["qr"][h, :, qp * HQ:(qp + 1) * HQ])
            if NQP > HQ:
                P.dma(qnt[:, HQ:NQP], G["qn"][h, :, T:NT])
                P.dma(qrt[:, HQ:NQP], G["qr"][h, :, T:NT])
            for (ca, n, kind, c0) in qch:
                kts = list(range(NKT)) if kind == 0 else list(range(NI, NKT))
                SB = (pb[0], pb[1], pb[4], pb[5])

                def qk(ji):
                    S = SB[ji % 4]
                    j = kts[ji]
                    P.mm(S[:, :n], knt[:, j * 128:(j + 1) * 128], qnt[:, c0:c0 + n], True, False)
                    P.mm(S[:, :n], krt[:, j * 128:(j + 1) * 128], qrt[:, c0:c0 + n], False, True)

                for ji in range(min(2, len(kts))):
                    qk(ji)
                for ji, j in enumerate(kts):
                    if ji + 2 < len(kts):
                        qk(ji + 2)
                    p_ = pt[ji % 3]
                    P.act(p_[:, :n], SB[ji % 4][:, :n], AF.Exp, scale=ATT_SCALE)
                    P.mm(pb[2][:, :n], vt[:, j, :], p_[:, :n], ji == 0, ji == len(kts) - 1)
                    P.mm(pb[3][:, :n], onesb[:], p_[:, :n], ji == 0, ji == len(kts) - 1)
                P.recip(rec[:, :n], pb[3][:, :n])
                P.tt(yatt[:, h, c0:c0 + n], pb[2][:, :n], rec[:, :n], ALU.mult)

        xc = P.sb("B_xc", [128, KC, 512])
        yrc = P.sb("B_yrc", [128, 8, 512], BF16)
        wob = [P.sb("B_wob%d" % i, [128, KC, 256], BF16) for i in range(2)]
        sq = [P.sb("B_sq%d" % i, [128, 512], BF16) for i in range(2)]
        htm = [P.sb("B_htm%d" % i, [128, D], BF16) for i in range(4)]
        rs = P.sb("B_rs", [128, 512])
        mx = P.sb("B_mx", [128, 1])
        sm = P.sb("B_sm", [128, 1])
        ex = P.sb("B_ex", [128, NE])
        xv = xT.rearrange("(kc p) n -> p kc n", p=128)
        xnv = G["xn"].rearrange("(kc p) n -> p kc n", p=128)
        wov = G["w_out"][l].rearrange("(kc p) n -> p kc n", p=128)
        it = 0
        for (ca, n, kind, c0) in qch:
            nj = n // 128
            P.dma(xc[:, :, :n], xv[:, :, ca:ca + n], q="pool")
            P.dma(yrc[:, :, :n], yrv[:, :, ca:ca + n], q="pool")
            g1n = "g1_l" if kind == 0 else "g1_c"
            shn = "sh2_l" if kind == 0 else "sh2_c"
            for mg in range(8):
                wo = wob[it % 2]
                it += 1
                P.dma(wo[:], wov[:, :, mg * 256:(mg + 1) * 256], q="pool")
                for mi in range(2):
                    m = mg * 2 + mi
                    p_ = pb[m % 2]
                    for kc in range(KC):
                        rhs_ = yrc[:, kc, :n] if kc < 8 else yatt[:, kc - 8, c0:c0 + n]
                        P.mm(p_[:, :n], wo[:, kc, mi * 128:(mi + 1) * 128], rhs_, kc == 0, kc == KC - 1)
                    P.stt(xc[:, m, :n], p_[:, :n], C(g1n, m), xc[:, m, :n], ALU.mult, ALU.add)
            P.dma(xnv[:, :, ca:ca + n], xc[:, :, :n])
            for m in range(KC):
                P.act(sq[m % 2][:, :n], xc[:, m, :n], AF.Square)
                P.mm(pb[2][:, :n], onesb[:], sq[m % 2][:, :n], m == 0, m == KC - 1)
            P.rstd(rs[:, :n], pb[2][:, :n], 1.0 / D, epsb[:])
            for m in range(KC):
                P.tt(xc[:, m, :n], xc[:, m, :n], rs[:, :n], ALU.mult)
                P.ts(xc[:, m, :n], xc[:, m, :n], A2m[:, kind, m:m + 1], C(shn, m), ALU.mult, ALU.add)
                for j in range(nj):
                    P.emit("pe", lambda en, j=j, m=m: en.transpose(pb[4 + j][:, (m % 4) * 128:(m % 4 + 1) * 128],
                                                                 xc[:, m, j * 128:(j + 1) * 128], ident[:]),
                           [xc, ident], [pb[4 + j]])
                if m % 4 == 3:
                    for j in range(nj):
                        dst = htm[j][:, (m // 4) * 512:(m // 4 + 1) * 512]
                        if j % 2 == 0:
                            P.copy(dst, pb[4 + j][:, :])
                        else:
                            P.act(dst, pb[4 + j][:, :], AF.Copy)
            for j in range(nj):
                P.dma(G["h2tm"][ca + j * 128:ca + (j + 1) * 128, :], htm[j][:])
                for m in range(KC):
                    P.mm(pb[j][:, :NE], xc[:, m, j * 128:(j + 1) * 128], rt[:, m, :], m == 0, m == KC - 1)
                lg = pb[j][:, :NE]
                ti = (ca // 128) + j
                P.emit("dve", lambda en, lg=lg: en.tensor_reduce(mx[:], lg, AX.X, ALU.max), [lg], [mx])
                P.ts(mx[:], mx[:], -1.0, None, ALU.mult)
                P.act(ex[:], lg, AF.Exp, bias=mx[:])
                P.emit("dve", lambda en: en.tensor_reduce(sm[:], ex[:], AX.X, ALU.add), [ex], [sm])
                P.recip(sm[:], sm[:])
                P.ts(aff_sb[:, :, ti], ex[:], sm[:, 0:1], None, ALU.mult)
        P.phase_end(mk)


def emit_D(P, pb, G, l, T, NCX, do_ctx):
    mk = P.phase_begin()
    NI, NIC = T // 128, NCX // 128
    cap, capc = 2 * T // NE, 2 * NCX // NE
    NU = NE
    aff_sb = G["aff_sb"]
    ones = P.sb("D_ones", [128, 128])
    P.memset(ones[:], 1.0)
    onesb = P.sb("D_onesb", [128, 128], BF16)
    P.memset(onesb[:], 1.0)
    utf = P.sb("D_utf", [128, 128])
    P.dma(utf[:], G["ut"])
    utb = P.sb("D_utb", [128, 128], BF16)
    P.copy(utb[:], utf[:])
    iott = P.sb("D_iott", [128, 512])
    P.dma(iott[:], G["iot"])
    zer = P.sb("D_zer", [128, 64])
    P.memset(zer[:], 0.0)

    key_sb = G["key_sb"]
    streams = [("L", aff_sb[:, :, 0:NI], key_sb[:, :, 0:NI], NI, cap)]
    if do_ctx:
        streams.append(("C", aff_sb[:, :, NI:NI + NIC], key_sb[:, :, NI:NI + NIC], NIC, capc))
    NS = len(streams)
    lo = P.sb("D_lo", [128, NS, NU])
    mid = P.sb("D_mid", [128, NS, NU])
    cnt = P.sb("D_cnt", [128, NS, NU])
    capt = P.sb("D_capt", [128, NS, NU])
    ge = P.sb("D_ge", [128, NS, NU], I32)
    cms = [P.sb("D_cm" + tg, [128, NU, ni]) for (tg, _, _, ni, _) in streams]
    P.memset(lo[:], 0.0)
    for si, (_, _, _, _, cv_) in enumerate(streams):
        P.memset(capt[:, si, :], float(cv_))
    lof = lo[:].rearrange("p s u -> p (s u)")
    midf = mid[:].rearrange("p s u -> p (s u)")
    for k in range(N_BISECT):
        P.ts(midf, lof, float(2.0 ** -(k + 1)), None, ALU.add)
        for si, (_, af, _, ni, _) in enumerate(streams):
            P.tt(cms[si][:], af, mid[:, si, :].unsqueeze(2).to_broadcast([128, NU, ni]), ALU.is_ge)
            P.emit("dve", lambda en, si=si: en.tensor_reduce(cnt[:, si, :], cms[si][:], AX.X, ALU.add), [cms[si]], [cnt])
        P.mm(pb[0][:, :NS * NU], ones[:], cnt[:].rearrange("p s u -> p (s u)"), True, True)
        P.tt(ge[:].rearrange("p s u -> p (s u)"), pb[0][:, :NS * NU], capt[:].rearrange("p s u -> p (s u)"), ALU.is_ge)
        P.emit("dve", lambda en: en.copy_predicated(lof, ge[:].rearrange("p s u -> p (s u)"), midf), [ge, mid, lo], [lo])
    for si, (tag, af, pos, ni, _) in enumerate(streams):
        n3 = [128, NU, ni]
        cm = cms[si]
        P.tt(cm[:], af, lo[:, si, :].unsqueeze(2).to_broadcast(n3), ALU.is_ge)
        mb = P.sb("D_mb" + tag, [128, NU * ni], BF16)
        P.copy(mb[:], cm[:].rearrange("p u i -> p (u i)"))
        tot = P.sb("D_tot" + tag, n3)
        inc = P.sb("D_inc" + tag, n3)
        wit = P.sb("D_wit" + tag, n3)
        for c0, cn in chunks(NU * ni):
            P.mm(pb[1][:, :cn], utb[:], mb[:, c0:c0 + cn], True, True)
            P.mm(pb[2][:, :cn], onesb[:], mb[:, c0:c0 + cn], True, True)
            P.copy(wit[:].rearrange("p u i -> p (u i)")[:, c0:c0 + cn], pb[1][:, :cn])
            P.copy(tot[:].rearrange("p u i -> p (u i)")[:, c0:c0 + cn], pb[2][:, :cn])
        for u in range(NU):
            P.scan(inc[:, u, :], tot[:, u, :], zer[:, :ni], 0.0, ALU.add, ALU.add)
        P.tt(inc[:], inc[:], tot[:], ALU.subtract)
        P.tt(pos, inc[:], wit[:], ALU.add)
        P.tt(pos, pos, cm[:], ALU.mult)
        P.ts(pos, pos, -1.0, None, ALU.add)
    keyL, keyC = key_sb[:, :, 0:NI], key_sb[:, :, NI:NI + NIC]
    NT = T + NCX

    identf = P.sb("D_ident", [128, 128])
    P.dma(identf[:], G["ident"])
    trT = [P.sb("D_trT%d" % i, [128, 128]) for i in range(2)]
    ncol = NI + NIC
    ti = 0
    for src, dstD in ((key_sb, G["keyD"]), (aff_sb, G["gateD"])):
        flat = src[:].rearrange("p e i -> p (e i)")
        dst2 = dstD.rearrange("e (i p) -> (e i) p", p=128)
        for c0 in range(0, NE * ncol, 128):
            w = min(128, NE * ncol - c0)
            t_ = trT[ti % 2]
            ti += 1
            P.emit("pe", lambda en, c0=c0, w=w, flat=flat: en.transpose(pb[1][:w, :128], flat[:, c0:c0 + w], identf[:]),
                   [src, identf], [pb[1]])
            P.copy(t_[:w, :], pb[1][:w, :128])
            P.dma(dst2[c0:c0 + w, :], t_[:w, :])

    xg = P.sb("D_xg", [128, KC, cap], BF16)
    at = P.sb("D_at", [128, KC, cap], BF16)
    if do_ctx:
        xgc = P.sb("D_xgc", [128, KC, capc], BF16)
        atc = P.sb("D_atc", [128, KC, capc], BF16)
    selr = [P.sb("D_sel%d" % i, [128, 512], BF16) for i in range(3)]
    h2t = [P.sb("D_h2t%d" % i, [128, 1024], BF16) for i in range(3)]
    WG = 512
    wbuf = [P.sb("D_wbuf%d" % i, [128, KC, WG], BF16) for i in range(4)]
    sg = [P.sb("D_sg%d" % i, [128, 512]) for i in range(2)]
    yo = [P.sb("D_yo%d" % i, [128, 512], BF16) for i in range(2)]
    cnt_it = [0, 0, 0]
    h2tm = G["h2tm"]

    def gather(row0, key_t, u, ni, capv, dst):
        for half in range(2):
            for i in range(ni):
                s_ = selr[cnt_it[0] % 3]
                h_ = h2t[cnt_it[0] % 3]
                cnt_it[0] += 1
                P.ts(s_[:, :capv], iott[:, :capv], key_t[:, u, i:i + 1], None, ALU.is_equal)
                P.dma(h_[:], h2tm[row0 + i * 128:row0 + (i + 1) * 128, half * 1024:(half + 1) * 1024])
                for dc in range(8):
                    P.mm(pb[dc][:, :capv], h_[:, dc * 128:(dc + 1) * 128], s_[:, :capv], i == 0, i == ni - 1)
            for dc in range(8):
                if dc % 2 == 0:
                    P.copy(dst[:, half * 8 + dc, :capv], pb[dc][:, :capv])
                else:
                    P.act(dst[:, half * 8 + dc, :capv], pb[dc][:, :capv], AF.Copy)

    for e in range(NE):
        gather(0, keyL, e, NI, cap, xg)
        cks = [(xg, at, cap, G["ysl"])]
        if do_ctx:
            gather(T, keyC, e, NIC, capc, xgc)
            cks.append((xgc, atc, capc, G["ycsl"]))
        wgv = G["w_gate"][l, e].rearrange("(kc p) n -> p kc n", p=128)
        wuv = G["w_up"][l, e].rearrange("(kc p) n -> p kc n", p=128)
        wdv = G["w_down"][l, e].rearrange("(kc p) n -> p kc n", p=128)
        for fg in range(D // WG):
            wg_t = wbuf[(cnt_it[1] * 2) % 4]
            wu_t = wbuf[(cnt_it[1] * 2 + 1) % 4]
            cnt_it[1] += 1
            P.dma(wg_t[:], wgv[:, :, fg * WG:(fg + 1) * WG], q="pool")
            P.dma(wu_t[:], wuv[:, :, fg * WG:(fg + 1) * WG], q="pool")
            for (xs, as_, n, _) in cks:
                for fi in range(WG // 128):
                    f = fg * (WG // 128) + fi
                    pg, pu = pb[(f % 2) * 2], pb[(f % 2) * 2 + 1]
                    for kc in range(KC):
                        P.mm(pg[:, :n], wg_t[:, kc, fi * 128:(fi + 1) * 128], xs[:, kc, :n], kc == 0, kc == KC - 1)
                    for kc in range(KC):
                        P.mm(pu[:, :n], wu_t[:, kc, fi * 128:(fi + 1) * 128], xs[:, kc, :n], kc == 0, kc == KC - 1)
                    s_ = sg[f % 2]
                    P.act(s_[:, :n], pg[:, :n], AF.Silu)
                    P.tt(as_[:, f, :n], s_[:, :n], pu[:, :n], ALU.mult)
        for dg in range(D // WG):
            wd_t = wbuf[cnt_it[2] % 4]
            cnt_it[2] += 1
            P.dma(wd_t[:], wdv[:, :, dg * WG:(dg + 1) * WG], q="pool")
            for (xs, as_, n, ydst) in cks:
                for st in range((n + 127) // 128):
                    sw = min(128, n - st * 128)
                    py = pb[4 + (st % 2)]
                    for fc in range(KC):
                        P.mm(py[:sw, :WG], as_[:, fc, st * 128:st * 128 + sw], wd_t[:, fc, :], fc == 0, fc == KC - 1)
                    y_ = yo[st % 2]
                    P.act(y_[:sw, :WG], py[:sw, :WG], AF.Copy)
                    P.dma(ydst[e, st * 128:st * 128 + sw, dg * WG:(dg + 1) * WG], y_[:sw, :WG])
    P.phase_end(mk)


def emit_E(P, pb, G, l, T, NCX, do_ctx, xo):
    mk = P.phase_begin()
    cap, capc = 2 * T // NE, 2 * NCX // NE
    KL = min(128, cap)
    S = cap // KL
    modfm = G["modfm"]
    sidt = P.sb("E_sid", [128, 4])
    P.dma(sidt[:], G["sid"])
    Yt = P.sb("E_Yt", [KL, NE, S, 1024], BF16)
    if do_ctx:
        Yc = P.sb("E_Yc", [capc, NE, 1024], BF16)
    kgb = [P.sb("E_kgb%d" % i, [128, 2, 512]) for i in range(2)]
    selT = [P.sb("E_selT%d" % i, [128, 512], BF16) for i in range(3)]
    xs = [P.sb("E_xs%d" % i, [128, 512]) for i in range(2)]
    ot = [P.sb("E_ot%d" % i, [128, 512]) for i in range(2)]
    qch = [(c0, n, 0) for c0, n in chunks(T)]
    if do_ctx:
        qch += [(T + c0, n, 1) for c0, n in chunks(NCX)]
    it = 0
    for half in range(2):
        hs = slice(half * 1024, (half + 1) * 1024)
        for e in range(NE):
            P.dma(Yt[:, e, :, :], G["ysl"][e].rearrange("(s p) d -> p s d", p=KL)[:, :, hs], q="sp" if e % 2 == 0 else "pool")
        if do_ctx:
            P.dma(Yc[:], G["ycsl"].rearrange("e s d -> s e d")[:, :, hs])
        for (c0, n, kind) in qch:
            for e in range(NE):
                k_ = kgb[e % 2]
                ns, K = (S, KL) if kind == 0 else (1, capc)
                P.dma(k_[:, 0, :n], G["keyD"][e, c0:c0 + n].partition_broadcast(128))
                P.dma(k_[:, 1, :n], G["gateD"][e, c0:c0 + n].partition_broadcast(128), q="pool")
                for s in range(ns):
                    st = selT[it % 3]
                    it += 1
                    P.stt(st[:K, :n], k_[:K, 0, :n], sidt[:K, s:s + 1], k_[:K, 1, :n], ALU.is_equal, ALU.mult)
                    first = (e == 0 and s == 0)
                    last = (e == NE - 1 and s == ns - 1)
                    for dc in range(8):
                        lhs = Yt[:KL, e, s, dc * 128:(dc + 1) * 128] if kind == 0 else Yc[:, e, dc * 128:(dc + 1) * 128]
                        P.mm(pb[dc][:, :n], lhs, st[:K, :n], first, last)
            for dc in range(8):
                m = half * 8 + dc
                x_ = xs[dc % 2]
                o_ = ot[dc % 2]
                P.dma(x_[:, :n], G["xn"][m * 128:(m + 1) * 128, c0:c0 + n], q="pool")
                P.stt(o_[:, :n], pb[dc][:, :n], modfm[:, l, 80 + m, kind:kind + 1], x_[:, :n], ALU.mult, ALU.add)
                P.dma(xo[m * 128:(m + 1) * 128, c0:c0 + n], o_[:, :n])
    P.phase_end(mk)


_PROG_CACHE = {}


def build_fused(T, NCX):
    NT = T + NCX
    NI, NIC = T // 128, NCX // 128
    cap, capc = 2 * T // NE, 2 * NCX // NE
    P = Prog()
    G = {}
    f32in = dict(xT0=[D, NT], sT=[128, KC, 2], bmod=[128, 2, 96], w_mod=[2, D, 6 * D], cvA=[2, 128, CV_A["n"]],
                 w_in=[2, D, W_IN_EXT], w_uq=[2, 512, 2048], w_uk=[2, 256, 1024], w_uv=[2, 256, 1024],
                 ropec=[64, T], ropes=[64, T], cvl=[2, 128, 88], lru_wa=[2, 2, 8, 128, 128], lru_wx=[2, 2, 8, 128, 128],
                 cvB=[2, 128, CV_B["n"]], w_out=[2, D, D], router=[2, D, NE], ident=[128, 128], ut=[128, 128],
                 iot=[128, 512], sid=[128, 4], w_gate=[2, NE, D, D], w_up=[2, NE, D, D], w_down=[2, NE, D, D])
    for k, shp in f32in.items():
        G[k] = P.din(k, shp)
    out = P.dout("out", [D, T])
    G["hm_d"] = P.dscr("hm_d", [128, KC, NT], BF16, track=True)
    G["xrT"] = P.dscr("xrT", [1024, NT])
    G["ggT"] = P.dscr("ggT", [1024, NT], BF16)
    G["qn"] = P.dscr("qn", [NH, 128, NT], BF16)
    G["qr"] = P.dscr("qr", [NH, 64, NT], BF16)
    G["kn"] = P.dscr("kn", [NH, 128, NT], BF16)
    G["kr"] = P.dscr("kr", [NH, 64, NT], BF16)
    G["v"] = P.dscr("v", [NT, 1024], BF16)
    G["yr"] = P.dscr("yr", [1024, NT], BF16)
    G["xn"] = P.dscr("xn", [D, NT])
    G["h2tm"] = P.dscr("h2tm", [NT, D], BF16)
    G["ysl"] = P.dscr("ysl", [NE, cap, D], BF16)
    G["ycsl"] = P.dscr("ycsl", [NE, capc, D], BF16)
    G["keyD"] = P.dscr("keyD", [NE, NT])
    G["gateD"] = P.dscr("gateD", [NE, NT])
    x1 = P.dscr("x1", [D, NT])
    pb = [P.ps("pb%d" % i, [128, 512]) for i in range(8)]
    G["modfm"] = P.sb("modfm", [128, 2, 96, 2])
    G["aff_sb"] = P.sb("aff_sb", [128, NE, NI + NIC])
    G["key_sb"] = P.sb("key_sb", [128, NE, NI + NIC])
    P.memset(G["key_sb"][:], -1.0)
    emit_M(P, pb, G)
    xT = G["xT0"]
    for l in range(2):
        do_ctx = (l == 0)
        emit_A(P, pb, G, l, xT, T, NCX)
        emit_A2(P, pb, G, l, T, NCX)
        emit_B(P, pb, G, l, xT, T, NCX, do_ctx)
        emit_D(P, pb, G, l, T, NCX, do_ctx)
        emit_E(P, pb, G, l, T, NCX, do_ctx, x1 if l == 0 else out)
        xT = x1
    print("[build_fused] instructions:", P.n_ins)
    return P.finish()


def host_inputs(inp, b):
    x, ctx = inp["x"], inp["ctx"]
    T = x.shape[1]
    m = {}
    m["xT0"] = np.ascontiguousarray(np.concatenate([x[b].T, ctx[b].T], axis=1), dtype=np.float32)
    C2 = np.stack([inp["c"][b], inp["c_ctx"]], axis=0).astype(np.float32)
    m["sT"] = np.ascontiguousarray(C2.T.reshape(KC, 128, 2).transpose(1, 0, 2))
    m["bmod"] = np.ascontiguousarray(np.stack([fm(inp["b_mod"][l]) for l in range(2)], axis=1))
    m["w_mod"] = inp["w_mod"]
    z16 = np.zeros((128, 16), np.float32)
    cvA, cvl, cvB, w_in, w_uq, w_uk, w_uv = [], [], [], [], [], [], []
    for l in range(2):
        qn, kn = inp["q_norm"][l], inp["k_norm"][l]
        cvA.append(np.concatenate([fm(inp["norm1"][l]), z16, z16, z16, z16, fm(inp["q_a_norm"][l]), fm(inp["kv_a_norm"][l]),
                                   pad128(qn[:128]), pad128(qn[128:]), pad128(qn[128 + _SW]),
                                   pad128(kn[:128]), pad128(kn[128:]), pad128(kn[128 + _SW])], axis=1))
        a, b_, c_, d_ = a_weights(inp, l)
        w_in.append(a); w_uq.append(b_); w_uk.append(c_); w_uv.append(d_)
        cw, cb = inp["conv_w"][l], inp["conv_b"][l]
        cols = [cw.reshape(4, 8, 128).transpose(2, 1, 0).reshape(128, 32), cb.reshape(8, 128).T]
        for nm in ("lru_ba", "lru_bx", "lru_lambda"):
            cols.append(inp[nm][l].reshape(2, 8, 128).transpose(2, 0, 1).reshape(128, 16))
        cvl.append(np.concatenate(cols, axis=1))
        cvB.append(np.concatenate([z16, z16, fm(inp["norm2"][l]), z16, z16, z16, z16], axis=1))
    m["cvA"] = np.ascontiguousarray(np.stack(cvA).astype(np.float32))
    m["cvl"] = np.ascontiguousarray(np.stack(cvl).astype(np.float32))
    m["cvB"] = np.ascontiguousarray(np.stack(cvB).astype(np.float32))
    m["w_in"] = np.stack(w_in); m["w_uq"] = np.stack(w_uq); m["w_uk"] = np.stack(w_uk); m["w_uv"] = np.stack(w_uv)
    cos, sin = rope_tables(np.arange(T))
    m["ropec"], m["ropes"] = cos, sin
    m["lru_wa"], m["lru_wx"] = inp["lru_wa"], inp["lru_wx"]
    m["w_out"], m["router"] = inp["w_out"], inp["router"]
    m["ident"] = np.eye(128, dtype=np.float32)
    m["ut"] = np.triu(np.ones((128, 128), np.float32))
    m["iot"] = np.tile(np.arange(512, dtype=np.float32), (128, 1))
    m["sid"] = (np.arange(128, dtype=np.float32)[:, None] + 128.0 * np.arange(4, dtype=np.float32)[None, :]).astype(np.float32)
    m["w_gate"], m["w_up"], m["w_down"] = inp["w_gate"], inp["w_up"], inp["w_down"]
    return {k: np.ascontiguousarray(v, dtype=np.float32) for k, v in m.items()}


def kernel(**inputs):
    inp = {k: np.asarray(v) for k, v in inputs.items()}
    Bn, T, _ = inp["x"].shape
    NCX = inp["ctx"].shape[1]
    key = (T, NCX)
    if key not in _PROG_CACHE:
        _PROG_CACHE[key] = build_fused(T, NCX)
    nc = _PROG_CACHE[key]
    maps = [host_inputs(inp, b) for b in range(Bn)]
    res = run_bass_kernel_spmd(nc, maps, core_ids=list(range(Bn)))
    out = np.stack([np.ascontiguousarray(r["out"].T) for r in res.results])
    return out.astype(np.float32, copy=False)
```
